# Optimizing a Trainium2 kernel written in Bass

```python
import math
import jax, jax.numpy as jnp
from jax import lax
import numpy as np

D_MODEL = 1024
BATCH = 2
SEQ = 8192
DEPTH = 4

N_MIXERS = 2
N_HY_LAYERS = (DEPTH + 1) // 2
N_AT_LAYERS = DEPTH // 2
HEAD_DIM = 64
D_MIX = D_MODEL
MEM_LEN = 256
MEM_HEADS = 4
D_MEM = MEM_HEADS * HEAD_DIM
D_MAIN = D_MIX - D_MEM
N_Q_HEADS = D_MAIN // HEAD_DIM
N_KV_HEADS = 4
GQA_GROUP = N_Q_HEADS // N_KV_HEADS
Q_BLOCK = 128
GRID_W = 64
ROPE_THETA = 10000.0
ROPE_AXIS_DIM = HEAD_DIM // 2
HY_ORDER = 2
HY_SHORT = 3
HY_BANDS = 16
HY_EMB = 1 + 2 * HY_BANDS
HY_FILT = 64
HY_DECAY_TARGET = 1e-2
HY_FAST_PCT = 0.3
HY_SLOW_PCT = 1.5
N_EXPERTS = 16
EC_CAPACITY_FACTOR = 2
EXPERT_FF = 768
NORM_EPS = 1e-6
D_IN_HY = 3 * D_MAIN + D_MEM
D_IN_AT = D_MAIN + 2 * N_KV_HEADS * HEAD_DIM + D_MEM

kernel_name = 'hybrid_hyena_gqa_ec_moe_encoder'


def rms_norm(x, g):
    xf = x.astype(jnp.float32)
    y = xf * lax.rsqrt(jnp.mean(xf * xf, axis=-1, keepdims=True) + NORM_EPS)
    return (y * g.astype(jnp.float32)).astype(x.dtype)


def hyena_filters(L, w1, b1, w2, b2, w3, freq):
    f32 = jnp.float32
    t = jnp.linspace(0.0, 1.0, L, dtype=f32)[:, None]
    w = (2.0 * math.pi) * jnp.arange(L, dtype=f32)[:, None] / L
    bands = jnp.linspace(1e-4, HY_BANDS - 1, HY_BANDS, dtype=f32)[None, :]
    feats = jnp.concatenate([t, jnp.cos(bands * w), -jnp.sin(bands * w)], axis=-1)
    fr = freq.astype(f32)
    h = jnp.sin(fr * (feats @ w1.astype(f32) + b1.astype(f32)))
    h = jnp.sin(fr * (h @ w2.astype(f32) + b2.astype(f32)))
    h = (h @ w3.astype(f32)).reshape(L, HY_ORDER, 2, D_MAIN)
    max_decay = math.log(HY_DECAY_TARGET) / HY_FAST_PCT
    min_decay = math.log(HY_DECAY_TARGET) / HY_SLOW_PCT
    deltas = jnp.linspace(min_decay, max_decay, D_MAIN, dtype=f32)
    h = h * jnp.exp(-t[:, :, None, None] * jnp.abs(deltas))
    h = h / (jnp.sum(jnp.abs(h), axis=(0, 2), keepdims=True) + 1e-6)
    hf, hb = h[:, :, 0], h[:, :, 1]
    zero = jnp.zeros_like(hf[:1])
    return jnp.concatenate([hf[:1] + hb[:1], hf[1:], zero, hb[1:][::-1]], axis=0)


def long_conv(z, k_fft):
    T = z.shape[1]
    Z = jnp.fft.rfft(z, n=2 * T, axis=1)
    return jnp.fft.irfft(Z * k_fft[None], n=2 * T, axis=1)[:, :T]


def hyena_mixer(u, short_w, k_fft, skip):
    T = u.shape[1]
    pad = HY_SHORT // 2
    up = jnp.pad(u, ((0, 0), (pad, pad), (0, 0)))
    uc = sum(up[:, j:j + T] * short_w[j] for j in range(HY_SHORT))
    x1, x2, v = jnp.split(uc, 3, axis=-1)
    z = v.astype(jnp.float32)
    for o, gate in enumerate((x1, x2)):
        z = gate.astype(jnp.float32) * (long_conv(z, k_fft[:, o]) + skip[o].astype(jnp.float32) * z)
    return z.astype(u.dtype)


def axial_rope_tables(T):
    rows = T // GRID_W
    pos_row = jnp.repeat(jnp.arange(rows), GRID_W).astype(jnp.float32)
    pos_col = jnp.tile(jnp.arange(GRID_W), rows).astype(jnp.float32)
    inv = 1.0 / (ROPE_THETA ** (jnp.arange(0, ROPE_AXIS_DIM, 2, dtype=jnp.float32) / ROPE_AXIS_DIM))
    ang = jnp.stack([pos_row[:, None] * inv, pos_col[:, None] * inv], axis=1)
    return jnp.cos(ang), jnp.sin(ang)


def apply_axial_rope(x, cos, sin):
    B, T, H, Dh = x.shape
    nf = ROPE_AXIS_DIM // 2
    xr = x.astype(jnp.float32).reshape(B, T, H, 2, 2, nf)
    c = cos[None, :, None]
    s = sin[None, :, None]
    xa, xb = xr[..., 0, :], xr[..., 1, :]
    out = jnp.stack([xa * c - xb * s, xa * s + xb * c], axis=-2)
    return out.reshape(B, T, H, Dh).astype(x.dtype)


def blocked_gqa(q, k, v):
    B, T, Hq, Dh = q.shape
    nb = T // Q_BLOCK
    qb = q.reshape(B, nb, Q_BLOCK, N_KV_HEADS, GQA_GROUP, Dh).transpose(1, 0, 2, 3, 4, 5)
    scale = Dh ** -0.5

    def one_block(qblk):
        s = jnp.einsum('bqhgd,bkhd->bhgqk', qblk, k).astype(jnp.float32) * scale
        p = jax.nn.softmax(s, axis=-1).astype(v.dtype)
        return jnp.einsum('bhgqk,bkhd->bqhgd', p, v)

    o = lax.map(one_block, qb)
    return o.transpose(1, 0, 2, 3, 4, 5).reshape(B, T, Hq * Dh)


def memory_cross_attn(cq, mem_n, w_kv):
    B, T, _ = cq.shape
    M = mem_n.shape[1]
    q = cq.reshape(B, T, MEM_HEADS, HEAD_DIM)
    mk, mv = jnp.split(mem_n @ w_kv, 2, axis=-1)
    mk = mk.reshape(B, M, MEM_HEADS, HEAD_DIM)
    mv = mv.reshape(B, M, MEM_HEADS, HEAD_DIM)
    s = jnp.einsum('bqhd,bmhd->bhqm', q, mk).astype(jnp.float32) * (HEAD_DIM ** -0.5)
    p = jax.nn.softmax(s, axis=-1).astype(mv.dtype)
    return jnp.einsum('bhqm,bmhd->bqhd', p, mv).reshape(B, T, D_MEM)


def expert_choice_ffn(xn, w_router, w_gate, w_up, w_down):
    B, T, D = xn.shape
    cap = EC_CAPACITY_FACTOR * T // N_EXPERTS
    aff = jax.nn.softmax((xn @ w_router).astype(jnp.float32), axis=-1)
    g, idx = lax.top_k(jnp.swapaxes(aff, 1, 2), cap)
    bidx = jnp.arange(B)[:, None, None]
    xg = xn[bidx, idx]
    hid = jax.nn.silu(jnp.einsum('becd,edf->becf', xg, w_gate)) * jnp.einsum('becd,edf->becf', xg, w_up)
    y = jnp.einsum('becf,efd->becd', hid, w_down) * g[..., None].astype(xn.dtype)
    return jnp.zeros_like(xn).at[bidx, idx].add(y)


def setup_inputs(seed: int = 0) -> dict:
    key = jax.random.key(seed)
    ks = iter(jax.random.split(key, 32))

    def nrm(shape, scale):
        return jax.random.normal(next(ks), shape, jnp.float32) * scale

    def gain(shape):
        return 1.0 + nrm(shape, 0.02)

    D = D_MODEL
    return {
        'x': nrm((BATCH, SEQ, D), 1.0),
        'mem': nrm((BATCH, MEM_LEN, D), 1.0),
        'mix_norm_g': gain((DEPTH, D)),
        'ffn_norm_g': gain((DEPTH, D)),
        'mem_norm_g': gain((D,)),
        'final_norm_g': gain((D,)),
        'w_mem_kv': nrm((DEPTH, D, 2 * D_MEM), D ** -0.5),
        'w_out': nrm((DEPTH, D_MIX, D), D_MIX ** -0.5),
        'hy_w_in': nrm((N_HY_LAYERS, D, D_IN_HY), D ** -0.5),
        'hy_short_w': nrm((N_HY_LAYERS, HY_SHORT, 3 * D_MAIN), HY_SHORT ** -0.5),
        'hy_filt_w1': nrm((N_HY_LAYERS, HY_EMB, HY_FILT), HY_EMB ** -0.5),
        'hy_filt_b1': nrm((N_HY_LAYERS, HY_FILT), 0.02),
        'hy_filt_w2': nrm((N_HY_LAYERS, HY_FILT, HY_FILT), HY_FILT ** -0.5),
        'hy_filt_b2': nrm((N_HY_LAYERS, HY_FILT), 0.02),
        'hy_filt_w3': nrm((N_HY_LAYERS, HY_FILT, HY_ORDER * 2 * D_MAIN), HY_FILT ** -0.5),
        'hy_filt_freq': gain((N_HY_LAYERS, HY_FILT)),
        'hy_skip': nrm((N_HY_LAYERS, HY_ORDER, D_MAIN), 1.0),
        'at_w_in': nrm((N_AT_LAYERS, D, D_IN_AT), D ** -0.5),
        'at_q_norm_g': gain((N_AT_LAYERS, HEAD_DIM)),
        'at_k_norm_g': gain((N_AT_LAYERS, HEAD_DIM)),
        'router_w': nrm((DEPTH, D, N_EXPERTS), D ** -0.5),
        'exp_w_gate': nrm((DEPTH, N_EXPERTS, D, EXPERT_FF), D ** -0.5),
        'exp_w_up': nrm((DEPTH, N_EXPERTS, D, EXPERT_FF), D ** -0.5),
        'exp_w_down': nrm((DEPTH, N_EXPERTS, EXPERT_FF, D), EXPERT_FF ** -0.5),
    }


def reference(x, mem, mix_norm_g, ffn_norm_g, mem_norm_g, final_norm_g, w_mem_kv, w_out,
              hy_w_in, hy_short_w, hy_filt_w1, hy_filt_b1, hy_filt_w2, hy_filt_b2, hy_filt_w3,
              hy_filt_freq, hy_skip, at_w_in, at_q_norm_g, at_k_norm_g,
              router_w, exp_w_gate, exp_w_up, exp_w_down):
    B, T, _ = x.shape
    cos, sin = axial_rope_tables(T)
    mem_n = rms_norm(mem, mem_norm_g)
    h = x
    for i in range(DEPTH):
        j = i // N_MIXERS
        u = rms_norm(h, mix_norm_g[i])
        if i % N_MIXERS == 0:
            proj = u @ hy_w_in[j]
            main_in, cq = proj[..., :3 * D_MAIN], proj[..., 3 * D_MAIN:]
            k_taps = hyena_filters(T, hy_filt_w1[j], hy_filt_b1[j], hy_filt_w2[j], hy_filt_b2[j],
                                   hy_filt_w3[j], hy_filt_freq[j])
            k_fft = jnp.fft.rfft(k_taps, axis=0)
            main = hyena_mixer(main_in, hy_short_w[j], k_fft, hy_skip[j])
        else:
            proj = u @ at_w_in[j]
            o1 = D_MAIN
            o2 = o1 + N_KV_HEADS * HEAD_DIM
            o3 = o2 + N_KV_HEADS * HEAD_DIM
            q = proj[..., :o1].reshape(B, T, N_Q_HEADS, HEAD_DIM)
            kk = proj[..., o1:o2].reshape(B, T, N_KV_HEADS, HEAD_DIM)
            vv = proj[..., o2:o3].reshape(B, T, N_KV_HEADS, HEAD_DIM)
            cq = proj[..., o3:]
            q = apply_axial_rope(rms_norm(q, at_q_norm_g[j]), cos, sin)
            kk = apply_axial_rope(rms_norm(kk, at_k_norm_g[j]), cos, sin)
            main = blocked_gqa(q, kk, vv)
        cross = memory_cross_attn(cq, mem_n, w_mem_kv[i])
        h = h + jnp.concatenate([main, cross], axis=-1) @ w_out[i]
        h = h + expert_choice_ffn(rms_norm(h, ffn_norm_g[i]), router_w[i],
                                  exp_w_gate[i], exp_w_up[i], exp_w_down[i])
    return rms_norm(h, final_norm_g)
```

```python
from contextlib import ExitStack
import numpy as np
import concourse.bass as bass
import concourse.mybir as mybir
from concourse.bass_utils import run_bass_kernel_spmd

F32 = mybir.dt.float32
BF16 = mybir.dt.bfloat16
I32 = mybir.dt.int32
ALU = mybir.AluOpType
AF = mybir.ActivationFunctionType
AX = mybir.AxisListType

SEM_LIMIT = 16000
N_DMA_SLOTS = 6


def _box(ap):
    t = ap.tensor
    name = t.name
    off = int(ap.offset)
    dims = ap.ap
    space = str(ap.space)
    if space == "DRAM":
        ext = sum((c - 1) * abs(s) for s, c in dims)
        return (name, 0, 1, off, off + ext + 1)
    shp = t.shape
    pstride = 1
    for d in shp[1:]:
        pstride *= int(d)
    p_lo = off // pstride
    f_lo = off % pstride
    pc = 1
    fext = 0
    for s, c in dims:
        if s == pstride and c > 1:
            pc = c
        elif s >= pstride and c > 1:
            pc = max(pc, (c - 1) * (s // pstride) + 1)
        else:
            fext += (c - 1) * abs(s)
    return (name, p_lo, p_lo + pc, f_lo, f_lo + fext + 1)


def _overlap(a, b):
    return a[1] < b[2] and b[1] < a[2] and a[3] < b[4] and b[3] < a[4]


class Fw:
    ENGS = ("pe", "act", "dve", "pool", "sp")

    def __init__(self, name="k"):
        self.nc = bass.Bass("TRN2", target_bir_lowering=False)
        self.stack = ExitStack()
        self.prog = {e: [] for e in self.ENGS}
        self.cur = {}
        self.waited = {e: {} for e in self.ENGS}
        self.acc = {}
        self.nsem = 0
        self.sems = {}
        self.slots = {}
        self.slot_i = {}
        self.n_instr = 0
        self.out_tokens = []

    def dram_in(self, name, shape, dt=F32):
        return self.nc.dram_tensor(name, list(shape), dt, kind="ExternalInput").ap()

    def dram_out(self, name, shape, dt=F32):
        return self.nc.dram_tensor(name, list(shape), dt, kind="ExternalOutput").ap()

    def dram_tmp(self, name, shape, dt=F32):
        return self.nc.dram_tensor(name, list(shape), dt, kind="Internal").ap()

    def sb(self, name, shape, dt=F32):
        return self.stack.enter_context(self.nc.sbuf_tensor(name, list(shape), dt))

    def ps(self, name, shape, dt=F32):
        return self.stack.enter_context(self.nc.psum_tensor(name, list(shape), dt))

    def _newsem(self):
        self.nsem += 1
        s = self.stack.enter_context(self.nc.semaphore(f"s{self.nsem}"))
        self.sems[id(s)] = s
        return s

    def _deps(self, eng, reads, writes):
        toks = []
        for ap, is_w in [(a, False) for a in reads] + [(a, True) for a in writes]:
            b = _box(ap)
            for rec in self.acc.get(b[0], ()):
                tok, rw, rb, reng = rec
                if not (is_w or rw):
                    continue
                if not _overlap(b, rb):
                    continue
                if reng == "pe" and eng == "pe":
                    continue
                toks.append(tok)
        return toks

    def _record(self, eng, tok, reads, writes):
        for ap, is_w in [(a, False) for a in reads] + [(a, True) for a in writes]:
            b = _box(ap)
            lst = self.acc.setdefault(b[0], [])
            new = []
            for rec in lst:
                _, rw, rb, reng = rec
                if rb == b and reng == eng and rw == is_w and not eng.startswith("dma"):
                    continue
                if is_w and rb[1] >= b[1] and rb[2] <= b[2] and rb[3] >= b[3] and rb[4] <= b[4]:
                    continue
                new.append(rec)
            new.append((tok, is_w, b, eng))
            self.acc[b[0]] = new

    def _waits(self, eng, toks):
        w = {}
        for sem, val in toks:
            k = id(sem)
            if self.waited[eng].get(k, 0) >= val:
                continue
            if w.get(k, (None, 0))[1] < val:
                w[k] = (sem, val)
        for k, (sem, val) in w.items():
            self.waited[eng][k] = val
        return list(w.values())

    def _next_tok(self, eng):
        c = self.cur.get(eng)
        if c is None or c[1] >= SEM_LIMIT:
            c = [self._newsem(), 0]
            self.cur[eng] = c
        c[1] += 1
        return (c[0], c[1])

    def op(self, eng, fn, reads, writes):
        toks = self._deps(eng, reads, writes)
        waits = self._waits(eng, toks)
        tok = self._next_tok(eng)
        self.prog[eng].append((waits, fn, tok[0], 1))
        self._record(eng, tok, reads, writes)
        self.n_instr += 1
        return tok

    def dma(self, q, out, in_, is_output=False, **kw):
        toks = self._deps("dma" + q, [in_], [out])
        if q not in self.slots:
            self.slots[q] = [[self._newsem(), 0] for _ in range(N_DMA_SLOTS)]
        sl = self.slots[q]
        i = self.slot_i.get(q, 0)
        self.slot_i[q] = i + 1
        s = sl[i % N_DMA_SLOTS]
        if s[1] + 16 > SEM_LIMIT:
            toks.append((s[0], s[1]))
            s = [self._newsem(), 0]
            sl[i % N_DMA_SLOTS] = s
        if s[1] > 0:
            toks.append((s[0], s[1]))
        waits = self._waits(q, toks)
        s[1] += 16
        tok = (s[0], s[1])
        self.prog[q].append((waits, lambda e: e.dma_start(out=out, in_=in_, **kw), tok[0], 16))
        self._record("dma" + q, tok, [in_], [out])
        if is_output:
            self.out_tokens.append(tok)
        self.n_instr += 1
        return tok

    def mm(self, out, lhsT, rhs, start=True, stop=True):
        return self.op("pe", lambda e: e.matmul(out, lhsT, rhs, start=start, stop=stop), [lhsT, rhs], [out])

    def transpose(self, out, in_, ident):
        return self.op("pe", lambda e: e.transpose(out, in_, ident), [in_, ident], [out])

    def act(self, out, in_, func, bias=None, scale=None, accum_out=None, eng="act"):
        kw = {}
        rd = [in_]
        wr = [out]
        if bias is not None:
            kw["bias"] = bias
            if not isinstance(bias, (int, float)):
                rd.append(bias)
        if scale is not None:
            kw["scale"] = scale
            if not isinstance(scale, (int, float)):
                rd.append(scale)
        if accum_out is not None:
            kw["accum_out"] = accum_out
            wr.append(accum_out)
        return self.op("act", lambda e: e.activation(out, in_, func, **kw), rd, wr)

    def tt(self, eng, out, a, b, op):
        return self.op(eng, lambda e: e.tensor_tensor(out, a, b, op), [a, b], [out])

    def ts(self, eng, out, a, s1, s2=None, op0=ALU.mult, op1=None, accum_out=None):
        rd = [a]
        wr = [out]
        if not isinstance(s1, (int, float)):
            rd.append(s1)
        if s2 is not None and not isinstance(s2, (int, float)):
            rd.append(s2)
        kw = {}
        if op1 is not None:
            kw["op1"] = op1
        if accum_out is not None:
            kw["accum_out"] = accum_out
            wr.append(accum_out)
        return self.op(eng, lambda e: e.tensor_scalar(out, a, s1, s2, op0, **kw), rd, wr)

    def stt(self, eng, out, a, s, b, op0, op1):
        rd = [a, b]
        if not isinstance(s, (int, float)):
            rd.append(s)
        return self.op(eng, lambda e: e.scalar_tensor_tensor(out, a, s, b, op0, op1), rd, [out])

    def copy(self, eng, out, in_):
        if eng == "act":
            return self.op("act", lambda e: e.copy(out, in_), [in_], [out])
        return self.op(eng, lambda e: e.tensor_copy(out, in_), [in_], [out])

    def memset(self, eng, out, val):
        return self.op(eng, lambda e: e.memset(out, val), [], [out])

    def reduce(self, eng, out, in_, op, axis=AX.X):
        return self.op(eng, lambda e: e.tensor_reduce(out, in_, axis, op), [in_], [out])

    def finish(self):
        if self.out_tokens:
            waits = self._waits("sp", self.out_tokens)
            self.prog["sp"].append((waits, None, None, 0))
        nc = self.nc
        prog = self.prog

        def emit(e, lst):
            for waits, fn, sem, inc in lst:
                for s, v in waits:
                    e.wait_ge(s, v)
                if fn is not None:
                    fn(e).then_inc(sem, inc)

        with nc.Block() as block:
            @block.tensor
            def _(e):
                emit(e, prog["pe"])

            @block.scalar
            def _(e):
                emit(e, prog["act"])

            @block.vector
            def _(e):
                emit(e, prog["dve"])

            @block.gpsimd
            def _(e):
                emit(e, prog["pool"])

            @block.sync
            def _(e):
                emit(e, prog["sp"])
        self.stack.close()
        return nc


def run(fw, in_maps, n=8, trace=False):
    nc = fw.finish()
    res = run_bass_kernel_spmd(nc, in_maps, core_ids=list(range(n)), trace=trace)
    return res

D = 1024
B = 2
T = 8192
NT = 2048
TT = 512
EPS = 1e-6
NCORES = 8


def consts_common(fw):
    c = {}
    c["ones"] = fw.sb("ones", [128, 128], F32)
    fw.memset("dve", c["ones"][:], 1.0)
    c["eps"] = fw.sb("eps", [128, 1], F32)
    fw.memset("dve", c["eps"][:], EPS)
    return c


def emit_rmsnorm(fw, c, xT, g_sb, uT, t0, tw, sq, ps_ss, rstd, out_dt_scale=None):
    for ch in range(8):
        s = sq[ch % 2]
        eng = "act" if ch % 2 == 0 else "pool"
        if eng == "act":
            fw.act(s[:, :tw], xT[:, ch, t0:t0 + tw], AF.Square)
        else:
            fw.tt("pool", s[:, :tw], xT[:, ch, t0:t0 + tw], xT[:, ch, t0:t0 + tw], ALU.mult)
        fw.mm(ps_ss[:, :tw], c["ones"][:], s[:, :tw], start=(ch == 0), stop=(ch == 7))
    fw.act(rstd[:, :tw], ps_ss[:, :tw], AF.Sqrt, bias=c["eps"][:], scale=1.0 / D)
    fw.op("dve", lambda e: e.reciprocal(rstd[:, :tw], rstd[:, :tw]), [rstd[:, :tw]], [rstd[:, :tw]])
    for ch in range(8):
        fw.stt("dve", uT[:, ch, :tw], xT[:, ch, t0:t0 + tw], g_sb[:, ch:ch + 1], rstd[:, :tw], ALU.mult, ALU.mult)


def emit_proj(fw, uT, w_bf, Dp, out_dram, t0, tw, ps_list, stg_list, ctr):
    for j in range(Dp // 128):
        ps = ps_list[ctr[0] % len(ps_list)]
        stg = stg_list[ctr[0] % len(stg_list)]
        for ch in range(8):
            fw.mm(ps[:, :tw], w_bf[:, ch, j * 128:(j + 1) * 128], uT[:, ch, :tw], start=(ch == 0), stop=(ch == 7))
        fw.copy("act" if ctr[0] % 2 == 0 else "dve", stg[:, :tw], ps[:, :tw])
        fw.dma("sp", out_dram[j * 128:(j + 1) * 128, t0:t0 + tw], stg[:, :tw], is_output=True)
        ctr[0] += 1


def build_P(Dp):
    fw = Fw()
    hT = fw.dram_in("hT", [D, NT])
    g = fw.dram_in("g", [128, 8])
    w = fw.dram_in("w", [D, Dp])
    o = fw.dram_out("o", [Dp, NT])
    c = consts_common(fw)
    xT = fw.sb("xT", [128, 8, NT], F32)
    g_sb = fw.sb("g_sb", [128, 8], F32)
    w_bf = fw.sb("w_bf", [128, 8, Dp], BF16)
    sq = [fw.sb(f"sq{i}", [128, TT], F32) for i in range(2)]
    rstd = fw.sb("rstd", [128, TT], F32)
    uT = [fw.sb(f"uT{i}", [128, 8, TT], BF16) for i in range(2)]
    ps_ss = fw.ps("ps_ss", [128, TT], F32)
    ps_list = [fw.ps(f"ps{i}", [128, TT], F32) for i in range(4)]
    stg_list = [fw.sb(f"stg{i}", [128, TT], F32) for i in range(4)]
    fw.dma("sp", g_sb[:], g)
    for ch in range(8):
        fw.dma("sp", xT[:, ch, :], hT[ch * 128:(ch + 1) * 128, :])
    for ch in range(8):
        fw.dma("pool", w_bf[:, ch, :], w[ch * 128:(ch + 1) * 128, :])
    ctr = [0]
    for ti in range(NT // TT):
        u = uT[ti % 2]
        emit_rmsnorm(fw, c, xT, g_sb, u, ti * TT, TT, sq, ps_ss, rstd)
        emit_proj(fw, u, w_bf, Dp, o, ti * TT, TT, ps_list, stg_list, ctr)
    return fw


HG = 8
HNG = 12
HCH = 96
TWO_PI = 6.283185307179586
MAGIC = 12582912.0


def hy_host_consts():
    n = np.arange(128)
    F = np.exp(-2j * np.pi * np.outer(n, n) / 128.0)
    Tw = np.exp(-2j * np.pi * np.outer(n, n) / 16384.0)
    f32 = lambda a: np.ascontiguousarray(a, dtype=np.float32)
    c = {}
    c["F1a"] = f32(np.concatenate([F.real, F.imag], 1)[:64])
    c["F1b"] = f32(np.concatenate([-F.imag, F.real], 1)[:64])
    c["F2re"] = f32(F.real)
    c["F2im"] = f32(F.imag)
    c["nF2im"] = f32(-F.imag)
    c["TT"] = f32(np.concatenate([Tw.real] * 4, 1))
    c["Tim"] = f32(Tw.imag)
    c["nTim"] = f32(-Tw.imag)
    c["Gc"] = f32(np.concatenate([F.real, -F.imag], 1))
    c["Gd"] = f32(np.concatenate([F.imag, F.real], 1))
    c["iF1re"] = f32(F.real[:, :64] / 16384.0)
    c["iF1im"] = f32(F.imag[:, :64] / 16384.0)
    c["niF1im"] = f32(-F.imag[:, :64] / 16384.0)
    return c


HY_CONST_SHAPES = {"F1a": [64, 256], "F1b": [64, 256], "F2re": [128, 128], "F2im": [128, 128], "nF2im": [128, 128],
                   "TT": [128, 512], "Tim": [128, 128], "nTim": [128, 128], "Gc": [128, 256], "Gd": [128, 256],
                   "iF1re": [128, 64], "iF1im": [128, 64], "niF1im": [128, 64]}


def build_HY():
    fw = Fw()
    G, NG, CH = HG, HNG, HCH
    W = 2 * G * 128
    U = fw.dram_in("U", [NG, 3, 3, 64, W])
    featsT = fw.dram_in("featsT", [33, 8192])
    w1 = fw.dram_in("w1", [33, 64])
    w2 = fw.dram_in("w2", [64, 64])
    w3 = fw.dram_in("w3", [NG, 64, 4 * G])
    b1 = fw.dram_in("b1", [64, 1])
    b2 = fw.dram_in("b2", [64, 1])
    fr = fw.dram_in("fr", [64, 1])
    decay = fw.dram_in("decay", [NG, 64, G * 128])
    sw = fw.dram_in("sw", [64, 9 * CH])
    skip = fw.dram_in("skip", [64, 2 * CH])
    o = fw.dram_out("o", [NG, 64, W])
    cd = {k: fw.dram_in("c_" + k, shp) for k, shp in HY_CONST_SHAPES.items()}
    c = consts_common(fw)
    T1 = [fw.sb(f"T1_{i}", [128, 512], F32) for i in range(4)]
    T2 = [fw.sb(f"T2_{i}", [128, 512], F32) for i in range(4)]
    K = {}
    BF_CONSTS = ("F2re", "F2im", "nF2im", "Gc", "Gd", "iF1re", "iF1im", "niF1im")
    for n_, (k, shp) in enumerate(HY_CONST_SHAPES.items()):
        if k in BF_CONSTS:
            stg = T1[n_ % 4] if n_ % 2 == 0 else T2[n_ % 4]
            sv = stg[0:shp[0], 0:shp[1]]
            fw.dma("sp", sv, cd[k])
            K[k] = fw.sb("k_" + k, shp, BF16)
            fw.copy("dve", K[k][:], sv)
        else:
            K[k] = fw.sb("k_" + k, shp, F32)
            fw.dma("sp", K[k][:], cd[k])
    w1s = fw.sb("w1s", [33, 64]); fw.dma("sp", w1s[:], w1)
    w2s = fw.sb("w2s", [64, 64]); fw.dma("sp", w2s[:], w2)
    b1s = fw.sb("b1s", [64, 1]); fw.dma("sp", b1s[:], b1)
    b2s = fw.sb("b2s", [64, 1]); fw.dma("sp", b2s[:], b2)
    frs = fw.sb("frs", [64, 1]); fw.dma("sp", frs[:], fr)
    sws = fw.sb("sws", [64, 9 * CH]); fw.dma("sp", sws[:], sw)
    sks = fw.sb("sks", [64, 2 * CH]); fw.dma("sp", sks[:], skip)
    frb1 = fw.sb("frb1", [64, 1]); fw.tt("dve", frb1[:], frs[:], b1s[:], ALU.mult)
    frb2 = fw.sb("frb2", [64, 1]); fw.tt("dve", frb2[:], frs[:], b2s[:], ALU.mult)
    h2T = fw.sb("h2T", [64, 8192], F32)
    scr = fw.sb("scr", [64, 4096], F32)
    ft = [scr[0:33, 0:512], scr[0:33, 512:1024]]
    zt = scr[:, 1024:1536]
    rt = scr[:, 1536:2048]
    h1t = scr[:, 2048:2560]
    psR = [fw.ps(f"psR{i}", [128, 512], F32) for i in range(7)]
    psM = fw.ps("psM", [128, 512], F32)
    ring = [0]

    def nps():
        r = psR[ring[0] % 7]
        ring[0] += 1
        return r

    def sin_layer(ps, frb, dst):
        fw.ts("dve", zt, ps, frs[:, 0:1], frb[:, 0:1], op0=ALU.mult, op1=ALU.add)
        fw.ts("dve", rt, zt, 1.0 / TWO_PI, MAGIC, op0=ALU.mult, op1=ALU.add)
        fw.ts("dve", rt, rt, MAGIC, TWO_PI, op0=ALU.subtract, op1=ALU.mult)
        fw.tt("dve", zt, zt, rt, ALU.subtract)
        fw.act(dst, zt, AF.Sin, scale=1.0 - 1e-6)

    for ti in range(16):
        f = ft[ti % 2]
        fw.dma("sp", f, featsT[:, ti * 512:(ti + 1) * 512])
        fw.mm(psM[0:64, :], w1s[:], f)
        sin_layer(psM[0:64, :], frb1, h1t)
        fw.mm(psM[0:64, :], w2s[:], h1t)
        sin_layer(psM[0:64, :], frb2, h2T[:, ti * 512:(ti + 1) * 512])

    w3s = fw.sb("w3s", [64, 4 * G], F32)
    dec = fw.sb("dec", [64, G, 128], F32)
    hf = fw.sb("hf", [64, 4 * G, 128], F32)
    habs_v = scr[:].rearrange("p (a n) -> p a n", a=4 * G)
    rs = fw.sb("rs", [64, 4 * G], F32)
    dsum = fw.sb("dsum", [128, 4 * G], F32)
    rden = fw.sb("rden", [128, 2 * G], F32)
    kk = fw.sb("kk", [128, 2, G, 512], F32)
    Xs = [fw.sb(f"Xs{i}", [128, 512], F32) for i in range(2)]
    sd = [fw.sb(f"sd{i}", [128, 256], F32) for i in range(2)]
    ush2 = fw.sb("ush2", [64, 2, G, 128], F32)
    Ushv = [scr[:, 0:2048].rearrange("p (b c n) -> p b c n", b=2, c=G),
            scr[:, 2048:4096].rearrange("p (b c n) -> p b c n", b=2, c=G), ush2[:]]
    uc = [fw.sb(f"uc{s}", [64, 2, G, 128], F32) for s in range(3)]
    OPb = [fw.sb(f"OP{i}", [128, 512], BF16) for i in range(6)]
    gtl = [fw.sb(f"gt{i}", [64, 2, 2, 128], F32) for i in range(4)]
    cnt = {"t": 0, "op": 0, "x": 0, "g": 0}

    def vw(ap, lay):
        if lay == "crk":
            return ap.rearrange("p (c r k) -> p c r k", c=2, r=2)
        return ap.rearrange("p (r c k) -> p c r k", c=2, r=2)

    def cmul(ps, lin, mulP1, mulRe, mulIm, lout):
        a, b = T1[cnt["t"] % 4], T2[cnt["t"] % 4]
        cnt["t"] += 1
        dst = OPb[cnt["op"] % 6]
        cnt["op"] += 1
        p4 = vw(ps, lin)
        fw.tt("dve", vw(a[:], lout), p4, mulP1, ALU.mult)
        fw.tt("dve", vw(b[:], lout)[:, :, 0, :], p4[:, :, 1, :], mulRe, ALU.mult)
        fw.tt("dve", vw(b[:], lout)[:, :, 1, :], p4[:, :, 0, :], mulIm, ALU.mult)
        fw.tt("pool", dst[:], a[:], b[:], ALU.add)
        return dst

    TT4 = K["TT"][:].rearrange("p (c r k) -> p c r k", c=2, r=2)
    Tim_b = K["Tim"][:].unsqueeze(1).broadcast_to([128, 2, 128])
    nTim_b = K["nTim"][:].unsqueeze(1).broadcast_to([128, 2, 128])

    def fwd_s2(a):
        px = nps()
        fw.mm(px[:, 0:256], K["F2re"][:], a[:, 0:256], start=True, stop=False)
        fw.mm(px[:, 0:256], K["nF2im"][:], a[:, 256:512], start=False, stop=True)
        fw.mm(px[:, 256:512], K["F2re"][:], a[:, 256:512], start=True, stop=False)
        fw.mm(px[:, 256:512], K["F2im"][:], a[:, 0:256], start=False, stop=True)
        return px

    for g in range(NG):
        c0 = g * G
        fw.dma("sp", w3s[:], w3[g])
        fw.dma("sp", dec[:], decay[g].rearrange("p (c n) -> p c n", c=G))
        for nb in range(8):
            pl3 = nps()
            for i in range(16):
                n2 = nb * 16 + i
                fw.mm(pl3[0:64, i * 32:(i + 1) * 32], h2T[:, n2 * 64:(n2 + 1) * 64], w3s[:])
            pin = pl3[0:64, :].rearrange("p (n a c) -> p n a c", n=16, a=4)
            dv = dec[:, :, nb * 16:(nb + 1) * 16].rearrange("p c n -> p n c").unsqueeze(2).broadcast_to([64, 16, 4, G])
            ov = hf[:, :, nb * 16:(nb + 1) * 16].rearrange("p (a c) n -> p n a c", a=4)
            fw.tt("dve", ov, pin, dv, ALU.mult)
        fw.act(habs_v, hf[:], AF.Abs)
        fw.reduce("dve", rs[:], habs_v, ALU.add)
        fw.mm(psM[:, 0:4 * G], c["ones"][0:64, :], rs[:])
        fw.copy("act", dsum[:], psM[:, 0:4 * G])
        d4 = dsum[:].rearrange("p (o d c) -> p o d c", o=2, d=2)
        r3 = rden[:].rearrange("p (o c) -> p o c", o=2)
        fw.tt("dve", r3, d4[:, :, 0, :], d4[:, :, 1, :], ALU.add)
        fw.ts("dve", rden[:], rden[:], 1e-6, None, op0=ALU.add)
        fw.op("dve", lambda e: e.reciprocal(rden[:], rden[:]), [rden[:]], [rden[:]])
        for s in range(3):
            for j in range(3):
                fw.dma("sp", Ushv[j], U[g, s, j].rearrange("p (b c n) -> p b c n", b=2, c=G))
            def wsc(j, cc):
                col = (s * 3 + j) * CH + c0 + cc
                return sws[:, col:col + 1]
            for cc in range(G):
                fw.act(uc[s][:, :, cc, :], Ushv[0][:, :, cc, :], AF.Copy, scale=wsc(0, cc))
            for j in (1, 2):
                for cc in range(G):
                    acc = uc[s][:, :, cc, :]
                    fw.stt("dve", acc, Ushv[j][:, :, cc, :], wsc(j, cc), acc, ALU.mult, ALU.add)
        ocs = [(oo, cc) for oo in range(2) for cc in range(G)]
        for q0 in range(0, len(ocs), 4):
            wave = ocs[q0:q0 + 4]
            pas = []
            for (oo, cc) in wave:
                pa = nps()
                for d in range(2):
                    col = (oo * 2 + d) * G + cc
                    fw.mm(pa[:, d * 256:(d + 1) * 256], hf[:, col, :], K["F1a"][:])
                pas.append(pa)
            aps = [cmul(pa[:], "crk", TT4, nTim_b, Tim_b, "rck") for pa in pas]
            pxs = [fwd_s2(a) for a in aps]
            for (oo, cc), px in zip(wave, pxs):
                xs_, sd_ = Xs[cnt["x"] % 2], sd[cnt["x"] % 2]
                cnt["x"] += 1
                fw.copy("act", xs_[:], px[:])
                fw.tt("pool", sd_[:, 0:128], xs_[:, 0:128], xs_[:, 128:256], ALU.add)
                fw.tt("pool", sd_[:, 128:256], xs_[:, 256:384], xs_[:, 384:512], ALU.subtract)
                rsc = rden[:, oo * G + cc:oo * G + cc + 1]
                kv = kk[:, oo, cc, :]
                fw.ts("dve", kv[:, 0:256].rearrange("p (r k) -> p r k", r=2),
                      sd_[:, 0:128].unsqueeze(1).broadcast_to([128, 2, 128]), rsc, None, op0=ALU.mult)
                fw.ts("dve", kv[:, 384:512], sd_[:, 128:256], rsc, None, op0=ALU.mult)
                fw.ts("dve", kv[:, 256:384], sd_[:, 128:256], rsc, -1.0, op0=ALU.mult, op1=ALU.mult)
        for oo in range(2):
            zin = uc[2]
            gate = uc[0] if oo == 0 else uc[1]
            zout = uc[2]
            prs = list(range(G // 2))
            pas = []
            for p in prs:
                pa = nps()
                for ci in range(2):
                    cc = 2 * p + ci
                    fw.mm(pa[:, ci * 256:(ci + 1) * 256], zin[:, 0, cc, :], K["F1a"][:], start=True, stop=False)
                    fw.mm(pa[:, ci * 256:(ci + 1) * 256], zin[:, 1, cc, :], K["F1b"][:], start=False, stop=True)
                pas.append(pa)
            aps = [cmul(pa[:], "crk", TT4, nTim_b, Tim_b, "rck") for pa in pas]
            pxs = [fwd_s2(a) for a in aps]
            yps = []
            for p, px in zip(prs, pxs):
                kp = kk[:, oo, 2 * p:2 * p + 2, :]
                yps.append(cmul(px[:], "rck", kp[:, :, 0:256].rearrange("p c (r k) -> p c r k", r=2),
                                kp[:, :, 256:384], kp[:, :, 384:512], "crk"))
            pbs = []
            for yp in yps:
                pb = nps()
                y4 = vw(yp[:], "crk")
                for ci in range(2):
                    fw.mm(pb[:, ci * 256:(ci + 1) * 256], y4[:, ci, 0, :], K["Gc"][:], start=True, stop=False)
                    fw.mm(pb[:, ci * 256:(ci + 1) * 256], y4[:, ci, 1, :], K["Gd"][:], start=False, stop=True)
                pbs.append(pb)
            bps = [cmul(pb[:], "crk", TT4, Tim_b, nTim_b, "rck") for pb in pbs]
            pys = []
            for bp in bps:
                py = nps()
                yv = py[0:64, :].rearrange("p (b c n) -> p b c n", b=2, c=2)
                fw.mm(yv[:, 0, :, :], K["iF1re"][:], bp[:, 0:256], start=True, stop=False)
                fw.mm(yv[:, 0, :, :], K["iF1im"][:], bp[:, 256:512], start=False, stop=True)
                fw.mm(yv[:, 1, :, :], K["iF1re"][:], bp[:, 256:512], start=True, stop=False)
                fw.mm(yv[:, 1, :, :], K["niF1im"][:], bp[:, 0:256], start=False, stop=True)
                pys.append(py)
            for p, py in zip(prs, pys):
                yv = py[0:64, :].rearrange("p (b c n) -> p b c n", b=2, c=2)
                gt = gtl[cnt["g"] % 4]
                cnt["g"] += 1
                base = oo * CH + c0 + 2 * p
                for ci in range(2):
                    fw.act(gt[:, :, ci, :], zin[:, :, 2 * p + ci, :], AF.Copy, scale=sks[:, base + ci:base + ci + 1])
                fw.tt("dve", gt[:], yv, gt[:], ALU.add)
                fw.tt("pool", zout[:, :, 2 * p:2 * p + 2, :], gate[:, :, 2 * p:2 * p + 2, :], gt[:], ALU.mult)
        fw.dma("sp", o[g].rearrange("p (b c n) -> p b c n", b=2, c=G), uc[2][:], is_output=True)
    return fw


def hy_host_inputs(proj, hy_w, core):
    G, NG, CH = HG, HNG, HCH
    ch0 = core * CH
    Umat = np.empty((NG, 3, 3, 64, 2 * G * 128), np.float32)
    for s in range(3):
        a = proj[:, :, s * 768 + ch0: s * 768 + ch0 + CH]
        ap = np.pad(a, ((0, 0), (1, 1), (0, 0)))
        for j in range(3):
            sh = ap[:, j:j + T, :]
            x = sh.transpose(0, 2, 1).reshape(B, NG, G, 64, 128)
            Umat[:, s, j] = x.transpose(1, 3, 0, 2, 4).reshape(NG, 64, 2 * G * 128)
    d = {"U": Umat}
    d.update(hy_w[core])
    return d


def hy_host_weights(short_w, w1, b1, w2, b2, w3, freq, skip):
    G, NG, CH = HG, HNG, HCH
    L = T
    m = np.arange(L, dtype=np.float64)
    tt_ = m / (L - 1)
    wv = 2.0 * np.pi * m / L
    bands = np.linspace(1e-4, 15.0, 16)
    feats = np.concatenate([tt_[:, None], np.cos(bands[None] * wv[:, None]), -np.sin(bands[None] * wv[:, None])], 1)
    perm = (np.arange(64)[None, :] * 128 + np.arange(128)[:, None]).reshape(-1)
    featsT = np.ascontiguousarray(feats[perm].T, dtype=np.float32)
    max_decay = np.log(1e-2) / 0.3
    min_decay = np.log(1e-2) / 1.5
    deltas = np.abs(np.linspace(min_decay, max_decay, 768))
    dec_full = np.exp(-tt_[:, None] * deltas[None, :])
    consts = hy_host_consts()
    outs = []
    w3r = w3.reshape(64, 2, 2, 768)
    for core in range(NCORES):
        ch0 = core * CH
        d = {"featsT": featsT, "w1": np.ascontiguousarray(w1), "w2": np.ascontiguousarray(w2),
             "b1": np.ascontiguousarray(b1.reshape(64, 1)), "b2": np.ascontiguousarray(b2.reshape(64, 1)),
             "fr": np.ascontiguousarray(freq.reshape(64, 1))}
        w3c = w3r[:, :, :, ch0:ch0 + CH].reshape(64, 2, 2, NG, G)
        d["w3"] = np.ascontiguousarray(w3c.transpose(3, 0, 1, 2, 4).reshape(NG, 64, 4 * G))
        dc = dec_full[:, ch0:ch0 + CH].reshape(64, 128, NG, G)
        d["decay"] = np.ascontiguousarray(dc.transpose(2, 0, 3, 1).reshape(NG, 64, G * 128), dtype=np.float32)
        swc = short_w.reshape(3, 3, 768)[:, :, ch0:ch0 + CH]
        swl = swc.transpose(1, 0, 2).reshape(1, 9 * CH)
        d["sw"] = np.ascontiguousarray(np.broadcast_to(swl, (64, 9 * CH)))
        skl = skip[:, ch0:ch0 + CH].reshape(1, 2 * CH)
        d["skip"] = np.ascontiguousarray(np.broadcast_to(skl, (64, 2 * CH)))
        for k, v in consts.items():
            d["c_" + k] = v
        outs.append(d)
    return outs


def hy_host_gather(results):
    G, NG, CH = HG, HNG, HCH
    main = np.empty((B, T, 768), np.float32)
    for core, r in enumerate(results):
        x = r.reshape(NG, 64, B, G, 128)
        x = x.transpose(2, 1, 4, 0, 3).reshape(B, T, CH)
        main[:, :, core * CH:(core + 1) * CH] = x
    return main


def at_host_consts():
    rows = T // 64
    pos_row = np.repeat(np.arange(rows), 64).astype(np.float64)
    pos_col = np.tile(np.arange(64), rows).astype(np.float64)
    inv = 1.0 / (10000.0 ** (np.arange(0, 32, 2, dtype=np.float64) / 32))
    ang = np.stack([pos_row[:, None] * inv, pos_col[:, None] * inv], 1)
    d = np.arange(64)
    axis = d // 32
    f = d % 16
    cosT = np.cos(ang[:, axis, f]).T
    sinT = np.sin(ang[:, axis, f]).T
    R = np.zeros((64, 64))
    for a in range(2):
        for ff in range(16):
            i0 = a * 32 + ff
            i1 = a * 32 + 16 + ff
            R[i0, i1] = -1.0
            R[i1, i0] = 1.0
    f32 = lambda a: np.ascontiguousarray(a, dtype=np.float32)
    return {"cosT": f32(cosT), "sinT": f32(sinT), "rotT": f32(R.T)}


def build_AT():
    fw = Fw()
    qT = fw.dram_in("qT", [3, 64, T])
    kT = fw.dram_in("kT", [64, T])
    v = fw.dram_in("v", [T, 64])
    gq = fw.dram_in("gq", [64, 1])
    gk = fw.dram_in("gk", [64, 1])
    cosT = fw.dram_in("cosT", [64, T])
    sinT = fw.dram_in("sinT", [64, T])
    rotT = fw.dram_in("rotT", [64, 64])
    o = fw.dram_out("o", [3, 64, T])
    c = consts_common(fw)
    cs = fw.sb("cs", [64, T], F32); fw.dma("sp", cs[:], cosT)
    sn = fw.sb("sn", [64, T], F32); fw.dma("sp", sn[:], sinT)
    rot = fw.sb("rot", [64, 64], F32); fw.dma("sp", rot[:], rotT)
    gqs = fw.sb("gqs", [64, 1], F32); fw.dma("sp", gqs[:], gq)
    gks = fw.sb("gks", [64, 1], F32); fw.dma("sp", gks[:], gk)
    qb = [fw.sb(f"qb{h}", [128, T], BF16) for h in range(3)]
    kb = fw.sb("kb", [128, T], BF16)
    for t_ in qb + [kb]:
        fw.memset("pool", t_[64:128, :], 0.0)
    vst = fw.sb("vst", [128, 64, 64], F32)
    va = fw.sb("va", [128, 64, 128], BF16)
    fw.dma("sp", vst[:], v.rearrange("(c p) d -> p c d", p=128))
    fw.memset("pool", va[:, :, 64:128], 0.0)
    fw.copy("dve", va[:, :, 0:64], vst[:])
    fw.memset("dve", va[:, :, 64:65], 1.0)
    xin = [fw.sb(f"xin{i}", [64, TT], F32) for i in range(2)]
    sq = fw.sb("sqa", [64, TT], F32)
    rstd = fw.sb("rstda", [64, TT], F32)
    xn = fw.sb("xna", [64, TT], F32)
    ra = fw.sb("ra", [64, TT], F32)
    rb = fw.sb("rb", [64, TT], F32)
    psA = fw.ps("psA", [128, TT], F32)
    psB = fw.ps("psB", [128, TT], F32)
    psS = [fw.ps(f"psS{i}", [128, TT], F32) for i in range(3)]
    psO = [fw.ps(f"psO{i}", [128, TT], F32) for i in range(2)]
    cnt = 0
    for src, g, dst in [(kT, gks, kb)] + [(qT[h], gqs, qb[h]) for h in range(3)]:
        for ti in range(T // TT):
            x = xin[cnt % 2]
            cnt += 1
            sl = slice(ti * TT, (ti + 1) * TT)
            fw.dma("sp", x[:], src[:, sl])
            fw.act(sq[:], x[:], AF.Square)
            fw.mm(psA[0:64, :], c["ones"][0:64, 0:64], sq[:])
            fw.act(rstd[:], psA[0:64, :], AF.Sqrt, bias=c["eps"][0:64, :], scale=1.0 / 64)
            fw.op("dve", lambda e: e.reciprocal(rstd[:], rstd[:]), [rstd[:]], [rstd[:]])
            fw.stt("dve", xn[:], x[:], g[:, 0:1], rstd[:], ALU.mult, ALU.mult)
            fw.mm(psB[0:64, :], rot[:], xn[:])
            fw.tt("dve", rb[:], psB[0:64, :], sn[:, sl], ALU.mult)
            fw.tt("pool", ra[:], xn[:], cs[:, sl], ALU.mult)
            fw.tt("pool", dst[0:64, sl], ra[:], rb[:], ALU.add)
    pt = [fw.sb(f"pt{i}", [128, TT], BF16) for i in range(3)]
    lsb = fw.sb("lsb", [128, TT], F32)
    rec = fw.sb("rec", [64, TT], F32)
    ost = [fw.sb(f"ost{i}", [64, TT], F32) for i in range(2)]
    it = 0
    blk = 0
    for h in range(3):
        for qi in range(T // TT):
            qs = slice(qi * TT, (qi + 1) * TT)
            po = psO[blk % 2]
            for kc in range(2):
                fw.mm(psS[(it + kc) % 3][:], kb[:, kc * 128:(kc + 1) * 128], qb[h][:, qs])
            for kc in range(64):
                ps = psS[it % 3]
                p = pt[it % 3]
                fw.act(p[:], ps[:], AF.Exp, scale=0.125)
                if kc + 2 < 64:
                    fw.mm(psS[(it + 2) % 3][:], kb[:, (kc + 2) * 128:(kc + 3) * 128], qb[h][:, qs])
                fw.mm(po[:], va[:, kc, :], p[:], start=(kc == 0), stop=(kc == 63))
                it += 1
            fw.copy("act", lsb[64:65, :], po[64:65, :])
            fw.mm(psA[0:64, :], c["ones"][64:65, 0:64], lsb[64:65, :])
            fw.op("dve", lambda e: e.reciprocal(rec[:], psA[0:64, :]), [psA[0:64, :]], [rec[:]])
            os_ = ost[blk % 2]
            fw.tt("dve", os_[:], po[0:64, :], rec[:], ALU.mult)
            fw.dma("sp", o[h, :, qs], os_[:], is_output=True)
            blk += 1
    return fw


def build_O():
    fw = Fw()
    hT = fw.dram_in("hT", [D, NT])
    mainT = fw.dram_in("mainT", [768, NT])
    cqT = fw.dram_in("cqT", [4, 64, NT])
    memT = fw.dram_in("memT", [D, 256])
    g_mem = fw.dram_in("g_mem", [128, 8])
    w_kv = fw.dram_in("w_kv", [D, 512])
    w_out = fw.dram_in("w_out", [D, D])
    g_ffn = fw.dram_in("g_ffn", [128, 8])
    w_r = fw.dram_in("w_r", [D, 16])
    ident = fw.dram_in("ident", [128, 128])
    ho = fw.dram_out("ho", [D, NT])
    affo = fw.dram_out("aff", [NT, 16])
    c = consts_common(fw)
    xT = fw.sb("xT", [128, 8, NT], F32)
    for ch in range(8):
        fw.dma("sp", xT[:, ch, :], hT[ch * 128:(ch + 1) * 128, :])
    gm = fw.sb("gm", [128, 8], F32); fw.dma("sp", gm[:], g_mem)
    gf = fw.sb("gf", [128, 8], F32); fw.dma("sp", gf[:], g_ffn)
    wr = fw.sb("wr", [128, 8, 16], F32); fw.dma("sp", wr[:], w_r.rearrange("(c p) e -> p c e", p=128))
    idf = fw.sb("idf", [128, 128], F32); fw.dma("sp", idf[:], ident)
    idb = fw.sb("idb", [128, 128], BF16); fw.copy("dve", idb[:], idf[:])
    mT = fw.sb("mT", [128, 8, 256], F32); fw.dma("sp", mT[:], memT.rearrange("(c p) m -> p c m", p=128))
    wkv = fw.sb("wkv", [128, 8, 512], BF16); fw.dma("pool", wkv[:], w_kv.rearrange("(c p) f -> p c f", p=128))
    main_bf = fw.sb("main_bf", [128, 6, NT], BF16); fw.dma("pool", main_bf[:], mainT.rearrange("(c p) t -> p c t", p=128))
    cq_bf = fw.sb("cq_bf", [64, 4, NT], BF16); fw.dma("pool", cq_bf[:], cqT.rearrange("h p t -> p h t"))
    wo_bf = fw.sb("wo_bf", [128, 6, D], BF16); fw.dma("pool", wo_bf[:], w_out[0:768, :].rearrange("(c p) d -> p c d", p=128))
    woc_bf = fw.sb("woc_bf", [64, 4, D], BF16); fw.dma("pool", woc_bf[:], w_out[768:1024, :].rearrange("(h p) d -> p h d", p=64))
    cross_bf = fw.sb("cross_bf", [64, 4, NT], BF16)
    sq = [fw.sb(f"sq{i}", [128, TT], F32) for i in range(2)]
    rstd = fw.sb("rstd", [128, TT], F32)
    ps_ss = fw.ps("ps_ss", [128, TT], F32)
    psS = [fw.ps(f"psS{i}", [128, TT], F32) for i in range(2)]
    psT = [fw.ps(f"psT{i}", [128, 128], BF16) for i in range(2)]
    psO = fw.ps("psO", [128, TT], F32)
    psP = [fw.ps(f"psP{i}", [128, TT], F32) for i in range(2)]
    memn = fw.sb("memn", [128, 8, 256], BF16)
    emit_rmsnorm(fw, c, mT, gm, memn, 0, 256, sq, ps_ss, rstd)
    mkT = fw.sb("mkT", [64, 4, 256], BF16)
    mv = fw.sb("mv", [128, 2, 256], BF16)
    for h in range(4):
        for ch in range(8):
            fw.mm(psS[0][0:64, 0:256], wkv[:, ch, h * 64:(h + 1) * 64], memn[:, ch, :], start=(ch == 0), stop=(ch == 7))
        fw.copy("act", mkT[:, h, :], psS[0][0:64, 0:256])
    for mc in range(2):
        for ch in range(8):
            fw.mm(psS[1][:, 0:256], memn[:, ch, mc * 128:(mc + 1) * 128], wkv[:, ch, 256:512], start=(ch == 0), stop=(ch == 7))
        fw.copy("act", mv[:, mc, :], psS[1][:, 0:256])
    pe_ = [fw.sb(f"pe{i}", [128, 256], F32) for i in range(2)]
    pn = [fw.sb(f"pn{i}", [128, 256], BF16) for i in range(2)]
    pT = [fw.sb(f"pT{i}", [128, 128], BF16) for i in range(4)]
    st = [fw.sb(f"st{i}", [128, 4], F32) for i in range(2)]
    it = 0
    tcnt = 0
    for tile in range(NT // TT):
        for h in range(4):
            for qp in range(0, 4, 2):
                units = [(qp + u, u) for u in range(2)]
                for qb_, i in units:
                    q0 = tile * TT + qb_ * 128
                    fw.mm(psS[i][:, 0:256], cq_bf[:, h, q0:q0 + 128], mkT[:, h, :])
                for qb_, i in units:
                    fw.reduce("dve", st[i][:, 0:1], psS[i][:, 0:256], ALU.max)
                for qb_, i in units:
                    fw.ts("dve", st[i][:, 1:2], st[i][:, 0:1], -0.125, None, op0=ALU.mult)
                for qb_, i in units:
                    fw.act(pe_[i][:], psS[i][:, 0:256], AF.Exp, bias=st[i][:, 1:2], scale=0.125)
                for qb_, i in units:
                    fw.reduce("dve", st[i][:, 2:3], pe_[i][:], ALU.add)
                for qb_, i in units:
                    s_ = st[i]
                    fw.op("dve", lambda e, s_=s_: e.reciprocal(s_[:, 3:4], s_[:, 2:3]), [s_[:, 2:3]], [s_[:, 3:4]])
                for qb_, i in units:
                    fw.ts("dve", pn[i][:], pe_[i][:], st[i][:, 3:4], None, op0=ALU.mult)
                for mc in range(2):
                    for qb_, i in units:
                        fw.transpose(psT[i][:], pn[i][:, mc * 128:(mc + 1) * 128], idb[:])
                    for qb_, i in units:
                        fw.copy("act", pT[2 * mc + i][:], psT[i][:])
                for qb_, i in units:
                    for mc in range(2):
                        fw.mm(psO[0:64, qb_ * 128:(qb_ + 1) * 128], mv[:, mc, h * 64:(h + 1) * 64], pT[2 * mc + i][:], start=(mc == 0), stop=(mc == 1))
                it += 2
            fw.copy("act", cross_bf[:, h, tile * TT:(tile + 1) * TT], psO[0:64, :])
    xn = fw.sb("xn", [128, 8, TT], F32)
    lg = fw.sb("lg", [128, 16], F32)
    affs = fw.sb("affs", [128, 16, 16], F32)
    pc = 0
    for tile in range(NT // TT):
        ts_ = slice(tile * TT, (tile + 1) * TT)
        for j in range(8):
            ps = psP[pc % 2]
            pc += 1
            js = slice(j * 128, (j + 1) * 128)
            for cc in range(6):
                fw.mm(ps[:], wo_bf[:, cc, js], main_bf[:, cc, ts_], start=(cc == 0), stop=False)
            for h in range(4):
                fw.mm(ps[:], woc_bf[:, h, js], cross_bf[:, h, ts_], start=False, stop=(h == 3))
            fw.tt("dve", xT[:, j, ts_], xT[:, j, ts_], ps[:], ALU.add)
            fw.dma("sp", ho[js, ts_], xT[:, j, ts_], is_output=True)
        emit_rmsnorm(fw, c, xT, gf, xn, tile * TT, TT, sq, ps_ss, rstd)
        for b_ in range(4):
            blk = tile * 4 + b_
            s_ = st[it % 2]
            it += 1
            pl = psO[:, 0:16]
            for ch in range(8):
                fw.mm(pl, xn[:, ch, b_ * 128:(b_ + 1) * 128], wr[:, ch, :], start=(ch == 0), stop=(ch == 7))
            fw.reduce("dve", s_[:, 0:1], pl, ALU.max)
            fw.ts("dve", s_[:, 1:2], s_[:, 0:1], -1.0, None, op0=ALU.mult)
            fw.act(lg[:], pl, AF.Exp, bias=s_[:, 1:2], scale=1.0)
            fw.reduce("dve", s_[:, 2:3], lg[:], ALU.add)
            fw.op("dve", lambda e, s_=s_: e.reciprocal(s_[:, 3:4], s_[:, 2:3]), [s_[:, 2:3]], [s_[:, 3:4]])
            fw.ts("dve", affs[:, blk, :], lg[:], s_[:, 3:4], None, op0=ALU.mult)
    fw.dma("sp", affo.rearrange("(b p) e -> p b e", p=128), affs[:], is_output=True)
    return fw


N_BISECT = 40
CAP = 1024


def m_host_consts():
    p = np.arange(128)
    Gm = (p[:, None] // 8 == p[None, :] // 8).astype(np.float32)
    G16 = (p[:, None] // 8 == np.arange(16)[None, :]).astype(np.float32)
    selm = np.zeros((16, 16, 128), np.float32)
    for e in range(16):
        selm[e, e, :] = 1.0
    return {"Gm": Gm, "G16": G16, "selm": selm.reshape(16, 2048)}


def build_M():
    fw = Fw()
    hT = fw.dram_in("hT", [D, NT])
    g_ffn = fw.dram_in("g_ffn", [128, 8])
    affP = fw.dram_in("affP", [128, 1024])
    affT = fw.dram_in("affT", [16, NT])
    Gm_d = fw.dram_in("Gm", [128, 128])
    G16_d = fw.dram_in("G16", [128, 16])
    selm_d = fw.dram_in("selm", [16, 2048])
    wg = fw.dram_in("wg", [16, D, 768])
    wu = fw.dram_in("wu", [16, D, 768])
    wd = fw.dram_in("wd", [16, 768, D])
    ho = fw.dram_out("ho", [D, NT])
    c = consts_common(fw)
    xT = fw.sb("xT", [128, 8, NT], F32)
    for ch in range(8):
        fw.dma("sp", xT[:, ch, :], hT[ch * 128:(ch + 1) * 128, :])
    gf = fw.sb("gf", [128, 8], F32); fw.dma("sp", gf[:], g_ffn)
    aP = fw.sb("aP", [128, 1024], F32); fw.dma("sp", aP[:], affP)
    wT = fw.sb("wT", [16, NT], F32); fw.dma("sp", wT[:], affT)
    Gm = fw.sb("Gm_s", [128, 128], F32); fw.dma("sp", Gm[:], Gm_d)
    G16 = fw.sb("G16_s", [128, 16], F32); fw.dma("sp", G16[:], G16_d)
    selm = fw.sb("selm_s", [16, 2048], F32); fw.dma("sp", selm[:], selm_d)
    wgb = [fw.sb(f"wgb{i}", [128, 8, 384], BF16) for i in range(2)]
    wub = [fw.sb(f"wub{i}", [128, 8, 384], BF16) for i in range(2)]
    wdb = [fw.sb(f"wdb{i}", [128, 3, D], BF16) for i in range(2)]

    def load_w(e, half, i):
        fs = slice(half * 384, (half + 1) * 384)
        fw.dma("pool", wgb[i][:], wg[e][:, fs].rearrange("(c p) f -> p c f", p=128))
        fw.dma("pool", wub[i][:], wu[e][:, fs].rearrange("(c p) f -> p c f", p=128))
        fw.dma("pool", wdb[i][:], wd[e][fs, :].rearrange("(c p) d -> p c d", p=128))

    load_w(0, 0, 0)
    load_w(0, 1, 1)
    sq = [fw.sb(f"sq{i}", [128, TT], F32) for i in range(2)]
    rstd = fw.sb("rstd", [128, TT], F32)
    ps_ss = fw.ps("ps_ss", [128, TT], F32)
    psG = [fw.ps(f"psG{i}", [128, TT], F32) for i in range(2)]
    psU = [fw.ps(f"psU{i}", [128, TT], F32) for i in range(2)]
    psY = [fw.ps(f"psY{i}", [128, TT], F32) for i in range(2)]
    psW = fw.ps("psW", [128, TT], F32)
    xn = fw.sb("xn", [128, 8, NT], BF16)
    for tile in range(NT // TT):
        emit_rmsnorm(fw, c, xT, gf, xn[:, :, tile * TT:(tile + 1) * TT], tile * TT, TT, sq, ps_ss, rstd)
    cmp_ = fw.sb("cmp", [128, 1024], F32)
    cn = fw.sb("cn", [128, 1], F32)
    S = {}
    for L in (128, 16):
        S[L] = fw.sb(f"bs{L}", [L, 8], F32)
        fw.memset("dve", S[L][:, 0:1], 0.0)
        fw.memset("dve", S[L][:, 1:2], 1.5)
        fw.memset("dve", S[L][:, 2:3], 0.75)
    for itn in range(N_BISECT):
        fw.ts("dve", cmp_[:], aP[:], S[128][:, 2:3], None, op0=ALU.is_ge)
        fw.reduce("dve", cn[:], cmp_[:], ALU.add)
        fw.mm(psW[:, 0:1], Gm[:], cn[:])
        fw.mm(psW[0:16, 1:2], G16[:], cn[:])
        for L, pc in ((128, psW[:, 0:1]), (16, psW[0:16, 1:2])):
            s = S[L]
            lo, hi, mid, ge, t1, t2 = (s[:, k:k + 1] for k in range(6))
            fw.ts("dve", ge, pc, float(CAP) - 0.5, None, op0=ALU.is_ge)
            fw.tt("dve", t1, mid, lo, ALU.subtract)
            fw.tt("dve", t1, t1, ge, ALU.mult)
            fw.tt("dve", t2, hi, mid, ALU.subtract)
            fw.tt("dve", t2, t2, ge, ALU.mult)
            fw.tt("dve", lo, lo, t1, ALU.add)
            fw.tt("dve", hi, mid, t2, ALU.add)
            fw.tt("dve", t1, lo, hi, ALU.add)
            fw.ts("dve", mid, t1, 0.5, None, op0=ALU.mult)
    fw.stt("dve", wT[:], wT[:], S[16][:, 0:1], wT[:], ALU.is_ge, ALU.mult)
    sg = [fw.sb(f"sg{i}", [128, TT], F32) for i in range(2)]
    wbc = [fw.sb(f"wbc{i}", [128, TT], F32) for i in range(2)]
    hid = [fw.sb(f"hid{i}", [128, 3, TT], BF16) for i in range(2)]
    gi = [0]
    yi = [0]
    steps = [(k, tile) for k in range(32) for tile in range(NT // TT)]

    def gateup(sidx):
        k, tile = steps[sidx]
        e, i = k // 2, k % 2
        ts_ = slice(tile * TT, (tile + 1) * TT)
        wb = wbc[sidx % 2]
        fw.mm(psW[:], selm[:, e * 128:(e + 1) * 128], wT[:, ts_])
        fw.copy("act", wb[:], psW[:])
        hd = hid[sidx % 2]
        for f in range(3):
            pg = psG[gi[0] % 2]
            pu = psU[gi[0] % 2]
            s_ = sg[gi[0] % 2]
            gi[0] += 1
            fs = slice(f * 128, (f + 1) * 128)
            for ch in range(8):
                fw.mm(pg[:], wgb[i][:, ch, fs], xn[:, ch, ts_], start=(ch == 0), stop=(ch == 7))
            for ch in range(8):
                fw.mm(pu[:], wub[i][:, ch, fs], xn[:, ch, ts_], start=(ch == 0), stop=(ch == 7))
            fw.act(s_[:], pg[:], AF.Silu)
            fw.tt("dve", s_[:], s_[:], wb[:], ALU.mult)
            fw.tt("dve", hd[:, f, :], s_[:], pu[:], ALU.mult)

    def down(sidx):
        k, tile = steps[sidx]
        i = k % 2
        ts_ = slice(tile * TT, (tile + 1) * TT)
        hd = hid[sidx % 2]
        for j in range(8):
            py = psY[yi[0] % 2]
            yi[0] += 1
            for f in range(3):
                fw.mm(py[:], wdb[i][:, f, j * 128:(j + 1) * 128], hd[:, f, :], start=(f == 0), stop=(f == 2))
            fw.tt("dve", xT[:, j, ts_], xT[:, j, ts_], py[:], ALU.add)
        if tile == NT // TT - 1 and k + 2 < 32:
            load_w((k + 2) // 2, (k + 2) % 2, (k + 2) % 2)

    for sidx in range(len(steps)):
        gateup(sidx)
        if sidx > 0:
            down(sidx - 1)
    down(len(steps) - 1)
    for ch in range(8):
        fw.dma("sp", ho[ch * 128:(ch + 1) * 128, :], xT[:, ch, :], is_output=True)
    return fw


def build_F():
    fw = Fw()
    hT = fw.dram_in("hT", [D, NT])
    g = fw.dram_in("g", [128, 8])
    o = fw.dram_out("o", [D, NT])
    c = consts_common(fw)
    xT = fw.sb("xT", [128, 8, NT], F32)
    for ch in range(8):
        fw.dma("sp", xT[:, ch, :], hT[ch * 128:(ch + 1) * 128, :])
    gs = fw.sb("gs", [128, 8], F32); fw.dma("sp", gs[:], g)
    sq = [fw.sb(f"sq{i}", [128, TT], F32) for i in range(2)]
    rstd = fw.sb("rstd", [128, TT], F32)
    ps_ss = fw.ps("ps_ss", [128, TT], F32)
    un = [fw.sb(f"un{i}", [128, 8, TT], F32) for i in range(2)]
    for tile in range(NT // TT):
        u = un[tile % 2]
        emit_rmsnorm(fw, c, xT, gs, u, tile * TT, TT, sq, ps_ss, rstd)
        for ch in range(8):
            fw.dma("sp", o[ch * 128:(ch + 1) * 128, tile * TT:(tile + 1) * TT], u[:, ch, :], is_output=True)
    return fw


def _lay(g):
    return np.ascontiguousarray(np.asarray(g, np.float32).reshape(8, 128).T)


def _run(fw, in_maps):
    nc = fw.finish()
    res = run_bass_kernel_spmd(nc, in_maps, core_ids=list(range(NCORES)))
    return res.results


def kernel(x, mem, mix_norm_g, ffn_norm_g, mem_norm_g, final_norm_g, w_mem_kv, w_out,
           hy_w_in, hy_short_w, hy_filt_w1, hy_filt_b1, hy_filt_w2, hy_filt_b2, hy_filt_w3,
           hy_filt_freq, hy_skip, at_w_in, at_q_norm_g, at_k_norm_g,
           router_w, exp_w_gate, exp_w_up, exp_w_down):
    f32 = lambda a: np.ascontiguousarray(np.asarray(a), dtype=np.float32)
    x = f32(x); mem = f32(mem)
    h = x.reshape(B * T, D)
    hT = [np.ascontiguousarray(h[c * NT:(c + 1) * NT].T) for c in range(NCORES)]
    memT = [np.ascontiguousarray(mem[b].T) for b in range(B)]
    ident = np.eye(128, dtype=np.float32)
    mconst = m_host_consts()
    aconst = at_host_consts()
    for i in range(4):
        j = i // 2
        hyena = (i % 2 == 0)
        w_in = f32(hy_w_in[j]) if hyena else f32(at_w_in[j])
        Dp = w_in.shape[1]
        g_mix = _lay(mix_norm_g[i])
        res = _run(build_P(Dp), [{"hT": hT[c], "g": g_mix, "w": w_in} for c in range(NCORES)])
        proj = np.concatenate([r["o"].T for r in res], 0).reshape(B, T, Dp)
        if hyena:
            hw = hy_host_weights(f32(hy_short_w[j]), f32(hy_filt_w1[j]), f32(hy_filt_b1[j]), f32(hy_filt_w2[j]),
                                 f32(hy_filt_b2[j]), f32(hy_filt_w3[j]), f32(hy_filt_freq[j]), f32(hy_skip[j]))
            res = _run(build_HY(), [hy_host_inputs(proj, hw, c) for c in range(NCORES)])
            main = hy_host_gather([r["o"] for r in res])
        else:
            gq = f32(at_q_norm_g[j]).reshape(64, 1)
            gk = f32(at_k_norm_g[j]).reshape(64, 1)
            ims = []
            for c in range(NCORES):
                b, g = c // 4, c % 4
                q = proj[b, :, g * 192:(g + 1) * 192].reshape(T, 3, 64)
                d = {"qT": np.ascontiguousarray(q.transpose(1, 2, 0)),
                     "kT": np.ascontiguousarray(proj[b, :, 768 + g * 64:768 + (g + 1) * 64].T),
                     "v": np.ascontiguousarray(proj[b, :, 1024 + g * 64:1024 + (g + 1) * 64]),
                     "gq": gq, "gk": gk}
                d.update(aconst)
                ims.append(d)
            res = _run(build_AT(), ims)
            main = np.empty((B, T, 768), np.float32)
            for c in range(NCORES):
                b, g = c // 4, c % 4
                main[b, :, g * 192:(g + 1) * 192] = res[c]["o"].transpose(2, 0, 1).reshape(T, 192)
        mainf = main.reshape(B * T, 768)
        cqf = proj[:, :, Dp - 256:].reshape(B * T, 256)
        ims = []
        for c in range(NCORES):
            sl = slice(c * NT, (c + 1) * NT)
            ims.append({"hT": hT[c], "mainT": np.ascontiguousarray(mainf[sl].T),
                        "cqT": np.ascontiguousarray(cqf[sl].T.reshape(4, 64, NT)), "memT": memT[c // 4],
                        "g_mem": _lay(mem_norm_g), "w_kv": f32(w_mem_kv[i]), "w_out": f32(w_out[i]),
                        "g_ffn": _lay(ffn_norm_g[i]), "w_r": f32(router_w[i]), "ident": ident})
        res = _run(build_O(), ims)
        hT = [r["ho"] for r in res]
        aff = np.concatenate([r["aff"] for r in res], 0)
        wg_, wu_, wd_ = f32(exp_w_gate[i]), f32(exp_w_up[i]), f32(exp_w_down[i])
        ims = []
        for c in range(NCORES):
            b = c // 4
            ab = aff[b * T:(b + 1) * T]
            d = {"hT": hT[c], "g_ffn": _lay(ffn_norm_g[i]),
                 "affP": np.ascontiguousarray(ab.T.reshape(128, 1024)),
                 "affT": np.ascontiguousarray(aff[c * NT:(c + 1) * NT].T),
                 "wg": wg_, "wu": wu_, "wd": wd_}
            d.update(mconst)
            ims.append(d)
        res = _run(build_M(), ims)
        hT = [r["ho"] for r in res]
    res = _run(build_F(), [{"hT": hT[c], "g": _lay(final_norm_g)} for c in range(NCORES)])
    out = np.concatenate([r["o"].T for r in res], 0).reshape(B, T, D)
    return np.ascontiguousarray(out, dtype=np.float32)
```

```python
from contextlib import ExitStack
import numpy as np
import concourse.bass as bass
import concourse.mybir as mybir
from concourse.bass_utils import run_bass_kernel_spmd

F32 = mybir.dt.float32
BF16 = mybir.dt.bfloat16
I32 = mybir.dt.int32
ALU = mybir.AluOpType
AF = mybir.ActivationFunctionType
AX = mybir.AxisListType

SEM_LIMIT = 16000
N_DMA_SLOTS = 6


def _box(ap):
    t = ap.tensor
    name = t.name
    off = int(ap.offset)
    dims = ap.ap
    space = str(ap.space)
    if space == "DRAM":
        ext = sum((c - 1) * abs(s) for s, c in dims)
        return (name, 0, 1, off, off + ext + 1)
    shp = t.shape
    pstride = 1
    for d in shp[1:]:
        pstride *= int(d)
    p_lo = off // pstride
    f_lo = off % pstride
    pc = 1
    fext = 0
    for s, c in dims:
        if s == pstride and c > 1:
            pc = c
        elif s >= pstride and c > 1:
            pc = max(pc, (c - 1) * (s // pstride) + 1)
        else:
            fext += (c - 1) * abs(s)
    return (name, p_lo, p_lo + pc, f_lo, f_lo + fext + 1)


def _overlap(a, b):
    return a[1] < b[2] and b[1] < a[2] and a[3] < b[4] and b[3] < a[4]


class Fw:
    ENGS = ("pe", "act", "dve", "pool", "sp")

    def __init__(self, name="k"):
        self.nc = bass.Bass("TRN2", target_bir_lowering=False)
        self.stack = ExitStack()
        self.prog = {e: [] for e in self.ENGS}
        self.cur = {}
        self.waited = {e: {} for e in self.ENGS}
        self.acc = {}
        self.nsem = 0
        self.sems = {}
        self.slots = {}
        self.slot_i = {}
        self.n_instr = 0
        self.out_tokens = []

    def dram_in(self, name, shape, dt=F32):
        return self.nc.dram_tensor(name, list(shape), dt, kind="ExternalInput").ap()

    def dram_out(self, name, shape, dt=F32):
        return self.nc.dram_tensor(name, list(shape), dt, kind="ExternalOutput").ap()

    def dram_tmp(self, name, shape, dt=F32):
        return self.nc.dram_tensor(name, list(shape), dt, kind="Internal").ap()

    def sb(self, name, shape, dt=F32):
        return self.stack.enter_context(self.nc.sbuf_tensor(name, list(shape), dt))

    def ps(self, name, shape, dt=F32):
        return self.stack.enter_context(self.nc.psum_tensor(name, list(shape), dt))

    def _newsem(self):
        self.nsem += 1
        s = self.stack.enter_context(self.nc.semaphore(f"s{self.nsem}"))
        self.sems[id(s)] = s
        return s

    def _deps(self, eng, reads, writes):
        toks = []
        for ap, is_w in [(a, False) for a in reads] + [(a, True) for a in writes]:
            b = _box(ap)
            for rec in self.acc.get(b[0], ()):
                tok, rw, rb, reng = rec
                if not (is_w or rw):
                    continue
                if not _overlap(b, rb):
                    continue
                if reng == "pe" and eng == "pe":
                    continue
                toks.append(tok)
        return toks

    def _record(self, eng, tok, reads, writes):
        for ap, is_w in [(a, False) for a in reads] + [(a, True) for a in writes]:
            b = _box(ap)
            lst = self.acc.setdefault(b[0], [])
            new = []
            for rec in lst:
                _, rw, rb, reng = rec
                if rb == b and reng == eng and rw == is_w and not eng.startswith("dma"):
                    continue
                if is_w and rb[1] >= b[1] and rb[2] <= b[2] and rb[3] >= b[3] and rb[4] <= b[4]:
                    continue
                new.append(rec)
            new.append((tok, is_w, b, eng))
            self.acc[b[0]] = new

    def _waits(self, eng, toks):
        w = {}
        for sem, val in toks:
            k = id(sem)
            if self.waited[eng].get(k, 0) >= val:
                continue
            if w.get(k, (None, 0))[1] < val:
                w[k] = (sem, val)
        for k, (sem, val) in w.items():
            self.waited[eng][k] = val
        return list(w.values())

    def _next_tok(self, eng):
        c = self.cur.get(eng)
        if c is None or c[1] >= SEM_LIMIT:
            c = [self._newsem(), 0]
            self.cur[eng] = c
        c[1] += 1
        return (c[0], c[1])

    def op(self, eng, fn, reads, writes):
        toks = self._deps(eng, reads, writes)
        waits = self._waits(eng, toks)
        tok = self._next_tok(eng)
        self.prog[eng].append((waits, fn, tok[0], 1))
        self._record(eng, tok, reads, writes)
        self.n_instr += 1
        return tok

    def dma(self, q, out, in_, is_output=False, **kw):
        toks = self._deps("dma" + q, [in_], [out])
        if q not in self.slots:
            self.slots[q] = [[self._newsem(), 0] for _ in range(N_DMA_SLOTS)]
        sl = self.slots[q]
        i = self.slot_i.get(q, 0)
        self.slot_i[q] = i + 1
        s = sl[i % N_DMA_SLOTS]
        if s[1] + 16 > SEM_LIMIT:
            toks.append((s[0], s[1]))
            s = [self._newsem(), 0]
            sl[i % N_DMA_SLOTS] = s
        if s[1] > 0:
            toks.append((s[0], s[1]))
        waits = self._waits(q, toks)
        s[1] += 16
        tok = (s[0], s[1])
        self.prog[q].append((waits, lambda e: e.dma_start(out=out, in_=in_, **kw), tok[0], 16))
        self._record("dma" + q, tok, [in_], [out])
        if is_output:
            self.out_tokens.append(tok)
        self.n_instr += 1
        return tok

    def mm(self, out, lhsT, rhs, start=True, stop=True):
        return self.op("pe", lambda e: e.matmul(out, lhsT, rhs, start=start, stop=stop), [lhsT, rhs], [out])

    def transpose(self, out, in_, ident):
        return self.op("pe", lambda e: e.transpose(out, in_, ident), [in_, ident], [out])

    def act(self, out, in_, func, bias=None, scale=None, accum_out=None, eng="act"):
        kw = {}
        rd = [in_]
        wr = [out]
        if bias is not None:
            kw["bias"] = bias
            if not isinstance(bias, (int, float)):
                rd.append(bias)
        if scale is not None:
            kw["scale"] = scale
            if not isinstance(scale, (int, float)):
                rd.append(scale)
        if accum_out is not None:
            kw["accum_out"] = accum_out
            wr.append(accum_out)
        return self.op("act", lambda e: e.activation(out, in_, func, **kw), rd, wr)

    def tt(self, eng, out, a, b, op):
        return self.op(eng, lambda e: e.tensor_tensor(out, a, b, op), [a, b], [out])

    def ts(self, eng, out, a, s1, s2=None, op0=ALU.mult, op1=None, accum_out=None):
        rd = [a]
        wr = [out]
        if not isinstance(s1, (int, float)):
            rd.append(s1)
        if s2 is not None and not isinstance(s2, (int, float)):
            rd.append(s2)
        kw = {}
        if op1 is not None:
            kw["op1"] = op1
        if accum_out is not None:
            kw["accum_out"] = accum_out
            wr.append(accum_out)
        return self.op(eng, lambda e: e.tensor_scalar(out, a, s1, s2, op0, **kw), rd, wr)

    def stt(self, eng, out, a, s, b, op0, op1):
        rd = [a, b]
        if not isinstance(s, (int, float)):
            rd.append(s)
        return self.op(eng, lambda e: e.scalar_tensor_tensor(out, a, s, b, op0, op1), rd, [out])

    def copy(self, eng, out, in_):
        if eng == "act":
            return self.op("act", lambda e: e.copy(out, in_), [in_], [out])
        return self.op(eng, lambda e: e.tensor_copy(out, in_), [in_], [out])

    def memset(self, eng, out, val):
        return self.op(eng, lambda e: e.memset(out, val), [], [out])

    def reduce(self, eng, out, in_, op, axis=AX.X):
        return self.op(eng, lambda e: e.tensor_reduce(out, in_, axis, op), [in_], [out])

    def finish(self):
        if self.out_tokens:
            waits = self._waits("sp", self.out_tokens)
            self.prog["sp"].append((waits, None, None, 0))
        nc = self.nc
        prog = self.prog

        def emit(e, lst):
            for waits, fn, sem, inc in lst:
                for s, v in waits:
                    e.wait_ge(s, v)
                if fn is not None:
                    fn(e).then_inc(sem, inc)

        with nc.Block() as block:
            @block.tensor
            def _(e):
                emit(e, prog["pe"])

            @block.scalar
            def _(e):
                emit(e, prog["act"])

            @block.vector
            def _(e):
                emit(e, prog["dve"])

            @block.gpsimd
            def _(e):
                emit(e, prog["pool"])

            @block.sync
            def _(e):
                emit(e, prog["sp"])
        self.stack.close()
        return nc


def run(fw, in_maps, n=8, trace=False):
    nc = fw.finish()
    res = run_bass_kernel_spmd(nc, in_maps, core_ids=list(range(n)), trace=trace)
    return res

D = 1024
B = 2
T = 8192
NT = 2048
TT = 512
EPS = 1e-6
NCORES = 8


def consts_common(fw):
    c = {}
    c["ones"] = fw.sb("ones", [128, 128], F32)
    fw.memset("dve", c["ones"][:], 1.0)
    c["eps"] = fw.sb("eps", [128, 1], F32)
    fw.memset("dve", c["eps"][:], EPS)
    return c


def emit_rmsnorm(fw, c, xT, g_sb, uT, t0, tw, sq, ps_ss, rstd, out_dt_scale=None):
    for ch in range(8):
        s = sq[ch % 2]
        eng = "act" if ch % 2 == 0 else "pool"
        if eng == "act":
            fw.act(s[:, :tw], xT[:, ch, t0:t0 + tw], AF.Square)
        else:
            fw.tt("pool", s[:, :tw], xT[:, ch, t0:t0 + tw], xT[:, ch, t0:t0 + tw], ALU.mult)
        fw.mm(ps_ss[:, :tw], c["ones"][:], s[:, :tw], start=(ch == 0), stop=(ch == 7))
    fw.act(rstd[:, :tw], ps_ss[:, :tw], AF.Sqrt, bias=c["eps"][:], scale=1.0 / D)
    fw.op("dve", lambda e: e.reciprocal(rstd[:, :tw], rstd[:, :tw]), [rstd[:, :tw]], [rstd[:, :tw]])
    for ch in range(8):
        fw.stt("dve", uT[:, ch, :tw], xT[:, ch, t0:t0 + tw], g_sb[:, ch:ch + 1], rstd[:, :tw], ALU.mult, ALU.mult)


def emit_proj(fw, uT, w_bf, Dp, out_dram, t0, tw, ps_list, stg_list, ctr):
    for j in range(Dp // 128):
        ps = ps_list[ctr[0] % len(ps_list)]
        stg = stg_list[ctr[0] % len(stg_list)]
        for ch in range(8):
            fw.mm(ps[:, :tw], w_bf[:, ch, j * 128:(j + 1) * 128], uT[:, ch, :tw], start=(ch == 0), stop=(ch == 7))
        fw.copy("act" if ctr[0] % 2 == 0 else "dve", stg[:, :tw], ps[:, :tw])
        fw.dma("sp", out_dram[j * 128:(j + 1) * 128, t0:t0 + tw], stg[:, :tw], is_output=True)
        ctr[0] += 1


def build_P(Dp):
    fw = Fw()
    hT = fw.dram_in("hT", [D, NT])
    g = fw.dram_in("g", [128, 8])
    w = fw.dram_in("w", [D, Dp])
    o = fw.dram_out("o", [Dp, NT])
    c = consts_common(fw)
    xT = fw.sb("xT", [128, 8, NT], F32)
    g_sb = fw.sb("g_sb", [128, 8], F32)
    w_bf = fw.sb("w_bf", [128, 8, Dp], BF16)
    sq = [fw.sb(f"sq{i}", [128, TT], F32) for i in range(2)]
    rstd = fw.sb("rstd", [128, TT], F32)
    uT = [fw.sb(f"uT{i}", [128, 8, TT], BF16) for i in range(2)]
    ps_ss = fw.ps("ps_ss", [128, TT], F32)
    ps_list = [fw.ps(f"ps{i}", [128, TT], F32) for i in range(4)]
    stg_list = [fw.sb(f"stg{i}", [128, TT], F32) for i in range(4)]
    fw.dma("sp", g_sb[:], g)
    for ti in range(NT // TT):
        for ch in range(8):
            fw.dma("sp", xT[:, ch, ti * TT:(ti + 1) * TT], hT[ch * 128:(ch + 1) * 128, ti * TT:(ti + 1) * TT])
    for ch in range(8):
        fw.dma("pool", w_bf[:, ch, :], w[ch * 128:(ch + 1) * 128, :])
    ctr = [0]
    for ti in range(NT // TT):
        u = uT[ti % 2]
        emit_rmsnorm(fw, c, xT, g_sb, u, ti * TT, TT, sq, ps_ss, rstd)
        emit_proj(fw, u, w_bf, Dp, o, ti * TT, TT, ps_list, stg_list, ctr)
    return fw


HG = 8
HNG = 12
HCH = 96
TWO_PI = 6.283185307179586
MAGIC = 12582912.0


def hy_host_consts():
    n = np.arange(128)
    F = np.exp(-2j * np.pi * np.outer(n, n) / 128.0)
    Tw = np.exp(-2j * np.pi * np.outer(n, n) / 16384.0)
    f32 = lambda a: np.ascontiguousarray(a, dtype=np.float32)
    c = {}
    c["F1a"] = f32(np.concatenate([F.real, F.imag], 1)[:64])
    c["F1b"] = f32(np.concatenate([-F.imag, F.real], 1)[:64])
    c["F2re"] = f32(F.real)
    c["F2im"] = f32(F.imag)
    c["nF2im"] = f32(-F.imag)
    c["TT"] = f32(np.concatenate([Tw.real] * 4, 1))
    c["Tim"] = f32(Tw.imag)
    c["nTim"] = f32(-Tw.imag)
    c["Gc"] = f32(np.concatenate([F.real, -F.imag], 1))
    c["Gd"] = f32(np.concatenate([F.imag, F.real], 1))
    c["iF1re"] = f32(F.real[:, :64] / 16384.0)
    c["iF1im"] = f32(F.imag[:, :64] / 16384.0)
    c["niF1im"] = f32(-F.imag[:, :64] / 16384.0)
    return c


HY_CONST_SHAPES = {"F1a": [64, 256], "F1b": [64, 256], "F2re": [128, 128], "F2im": [128, 128], "nF2im": [128, 128],
                   "TT": [128, 512], "Tim": [128, 128], "nTim": [128, 128], "Gc": [128, 256], "Gd": [128, 256],
                   "iF1re": [128, 64], "iF1im": [128, 64], "niF1im": [128, 64]}


def build_HY():
    fw = Fw()
    G, NG, CH = HG, HNG, HCH
    W = 2 * G * 128
    U = fw.dram_in("U", [NG, 3, 3, 64, W])
    featsT = fw.dram_in("featsT", [33, 8192])
    w1 = fw.dram_in("w1", [33, 64])
    w2 = fw.dram_in("w2", [64, 64])
    w3 = fw.dram_in("w3", [NG, 64, 4 * G])
    b1 = fw.dram_in("b1", [64, 1])
    b2 = fw.dram_in("b2", [64, 1])
    fr = fw.dram_in("fr", [64, 1])
    decay = fw.dram_in("decay", [NG, 64, G * 128])
    sw = fw.dram_in("sw", [64, 9 * CH])
    skip = fw.dram_in("skip", [64, 2 * CH])
    o = fw.dram_out("o", [NG, 64, W])
    cd = {k: fw.dram_in("c_" + k, shp) for k, shp in HY_CONST_SHAPES.items()}
    c = consts_common(fw)
    T1 = [fw.sb(f"T1_{i}", [128, 512], F32) for i in range(4)]
    T2 = [fw.sb(f"T2_{i}", [128, 512], F32) for i in range(4)]
    K = {}
    BF_CONSTS = ("F2re", "F2im", "nF2im", "Gc", "Gd", "iF1re", "iF1im", "niF1im")
    for n_, (k, shp) in enumerate(HY_CONST_SHAPES.items()):
        if k in BF_CONSTS:
            stg = T1[n_ % 4] if n_ % 2 == 0 else T2[n_ % 4]
            sv = stg[0:shp[0], 0:shp[1]]
            fw.dma("sp", sv, cd[k])
            K[k] = fw.sb("k_" + k, shp, BF16)
            fw.copy("dve", K[k][:], sv)
        else:
            K[k] = fw.sb("k_" + k, shp, F32)
            fw.dma("sp", K[k][:], cd[k])
    w1s = fw.sb("w1s", [33, 64]); fw.dma("sp", w1s[:], w1)
    w2s = fw.sb("w2s", [64, 64]); fw.dma("sp", w2s[:], w2)
    b1s = fw.sb("b1s", [64, 1]); fw.dma("sp", b1s[:], b1)
    b2s = fw.sb("b2s", [64, 1]); fw.dma("sp", b2s[:], b2)
    frs = fw.sb("frs", [64, 1]); fw.dma("sp", frs[:], fr)
    sws = fw.sb("sws", [64, 9 * CH]); fw.dma("sp", sws[:], sw)
    sks = fw.sb("sks", [64, 2 * CH]); fw.dma("sp", sks[:], skip)
    frb1 = fw.sb("frb1", [64, 1]); fw.tt("dve", frb1[:], frs[:], b1s[:], ALU.mult)
    frb2 = fw.sb("frb2", [64, 1]); fw.tt("dve", frb2[:], frs[:], b2s[:], ALU.mult)
    h2T = fw.sb("h2T", [64, 8192], F32)
    scr = fw.sb("scr", [64, 4096], F32)
    ft = [scr[0:33, 0:512], scr[0:33, 512:1024]]
    zt = scr[:, 1024:1536]
    rt = scr[:, 1536:2048]
    h1t = scr[:, 2048:2560]
    psR = [fw.ps(f"psR{i}", [128, 512], F32) for i in range(7)]
    psM = fw.ps("psM", [128, 512], F32)
    ring = [0]

    def nps():
        r = psR[ring[0] % 7]
        ring[0] += 1
        return r

    def sin_layer(ps, frb, dst):
        fw.ts("dve", zt, ps, frs[:, 0:1], frb[:, 0:1], op0=ALU.mult, op1=ALU.add)
        fw.ts("dve", rt, zt, 1.0 / TWO_PI, MAGIC, op0=ALU.mult, op1=ALU.add)
        fw.ts("dve", rt, rt, MAGIC, TWO_PI, op0=ALU.subtract, op1=ALU.mult)
        fw.tt("dve", zt, zt, rt, ALU.subtract)
        fw.act(dst, zt, AF.Sin, scale=1.0 - 1e-6)

    for ti in range(16):
        f = ft[ti % 2]
        fw.dma("sp", f, featsT[:, ti * 512:(ti + 1) * 512])
        fw.mm(psM[0:64, :], w1s[:], f)
        sin_layer(psM[0:64, :], frb1, h1t)
        fw.mm(psM[0:64, :], w2s[:], h1t)
        sin_layer(psM[0:64, :], frb2, h2T[:, ti * 512:(ti + 1) * 512])

    w3s = fw.sb("w3s", [64, 4 * G], F32)
    dec = fw.sb("dec", [64, G, 128], F32)
    hf = fw.sb("hf", [64, 4 * G, 128], F32)
    habs_v = scr[:].rearrange("p (a n) -> p a n", a=4 * G)
    rs = fw.sb("rs", [64, 4 * G], F32)
    dsum = fw.sb("dsum", [128, 4 * G], F32)
    rden = fw.sb("rden", [128, 2 * G], F32)
    kk = fw.sb("kk", [128, 2, G, 512], F32)
    Xs = [fw.sb(f"Xs{i}", [128, 512], F32) for i in range(2)]
    sd = [fw.sb(f"sd{i}", [128, 256], F32) for i in range(2)]
    ush2 = fw.sb("ush2", [64, 2, G, 128], F32)
    Ushv = [scr[:, 0:2048].rearrange("p (b c n) -> p b c n", b=2, c=G),
            scr[:, 2048:4096].rearrange("p (b c n) -> p b c n", b=2, c=G), ush2[:]]
    uc = [fw.sb(f"uc{s}", [64, 2, G, 128], F32) for s in range(3)]
    OPb = [fw.sb(f"OP{i}", [128, 512], BF16) for i in range(6)]
    gtl = [fw.sb(f"gt{i}", [64, 2, 2, 128], F32) for i in range(4)]
    cnt = {"t": 0, "op": 0, "x": 0, "g": 0}

    def vw(ap, lay):
        if lay == "crk":
            return ap.rearrange("p (c r k) -> p c r k", c=2, r=2)
        return ap.rearrange("p (r c k) -> p c r k", c=2, r=2)

    def cmul(ps, lin, mulP1, mulRe, mulIm, lout):
        a, b = T1[cnt["t"] % 4], T2[cnt["t"] % 4]
        cnt["t"] += 1
        dst = OPb[cnt["op"] % 6]
        cnt["op"] += 1
        p4 = vw(ps, lin)
        fw.tt("dve", vw(a[:], lout), p4, mulP1, ALU.mult)
        fw.tt("dve", vw(b[:], lout)[:, :, 0, :], p4[:, :, 1, :], mulRe, ALU.mult)
        fw.tt("dve", vw(b[:], lout)[:, :, 1, :], p4[:, :, 0, :], mulIm, ALU.mult)
        fw.tt("pool", dst[:], a[:], b[:], ALU.add)
        return dst

    TT4 = K["TT"][:].rearrange("p (c r k) -> p c r k", c=2, r=2)
    Tim_b = K["Tim"][:].unsqueeze(1).broadcast_to([128, 2, 128])
    nTim_b = K["nTim"][:].unsqueeze(1).broadcast_to([128, 2, 128])

    def fwd_s2(a):
        px = nps()
        fw.mm(px[:, 0:256], K["F2re"][:], a[:, 0:256], start=True, stop=False)
        fw.mm(px[:, 0:256], K["nF2im"][:], a[:, 256:512], start=False, stop=True)
        fw.mm(px[:, 256:512], K["F2re"][:], a[:, 256:512], start=True, stop=False)
        fw.mm(px[:, 256:512], K["F2im"][:], a[:, 0:256], start=False, stop=True)
        return px

    for g in range(NG):
        c0 = g * G
        fw.dma("sp", w3s[:], w3[g])
        fw.dma("sp", dec[:], decay[g].rearrange("p (c n) -> p c n", c=G))
        for nb in range(8):
            pl3 = nps()
            for i in range(16):
                n2 = nb * 16 + i
                fw.mm(pl3[0:64, i * 32:(i + 1) * 32], h2T[:, n2 * 64:(n2 + 1) * 64], w3s[:])
            pin = pl3[0:64, :].rearrange("p (n a c) -> p n a c", n=16, a=4)
            dv = dec[:, :, nb * 16:(nb + 1) * 16].rearrange("p c n -> p n c").unsqueeze(2).broadcast_to([64, 16, 4, G])
            ov = hf[:, :, nb * 16:(nb + 1) * 16].rearrange("p (a c) n -> p n a c", a=4)
            fw.tt("dve", ov, pin, dv, ALU.mult)
        fw.act(habs_v, hf[:], AF.Abs)
        fw.reduce("dve", rs[:], habs_v, ALU.add)
        fw.mm(psM[:, 0:4 * G], c["ones"][0:64, :], rs[:])
        fw.copy("act", dsum[:], psM[:, 0:4 * G])
        d4 = dsum[:].rearrange("p (o d c) -> p o d c", o=2, d=2)
        r3 = rden[:].rearrange("p (o c) -> p o c", o=2)
        fw.tt("dve", r3, d4[:, :, 0, :], d4[:, :, 1, :], ALU.add)
        fw.ts("dve", rden[:], rden[:], 1e-6, None, op0=ALU.add)
        fw.op("dve", lambda e: e.reciprocal(rden[:], rden[:]), [rden[:]], [rden[:]])
        for s in range(3):
            for j in range(3):
                fw.dma("sp", Ushv[j], U[g, s, j].rearrange("p (b c n) -> p b c n", b=2, c=G))
            def wsc(j, cc):
                col = (s * 3 + j) * CH + c0 + cc
                return sws[:, col:col + 1]
            for cc in range(G):
                fw.act(uc[s][:, :, cc, :], Ushv[0][:, :, cc, :], AF.Copy, scale=wsc(0, cc))
            for j in (1, 2):
                for cc in range(G):
                    acc = uc[s][:, :, cc, :]
                    fw.stt("dve", acc, Ushv[j][:, :, cc, :], wsc(j, cc), acc, ALU.mult, ALU.add)
        ocs = [(oo, cc) for oo in range(2) for cc in range(G)]
        for q0 in range(0, len(ocs), 4):
            wave = ocs[q0:q0 + 4]
            pas = []
            for (oo, cc) in wave:
                pa = nps()
                for d in range(2):
                    col = (oo * 2 + d) * G + cc
                    fw.mm(pa[:, d * 256:(d + 1) * 256], hf[:, col, :], K["F1a"][:])
                pas.append(pa)
            aps = [cmul(pa[:], "crk", TT4, nTim_b, Tim_b, "rck") for pa in pas]
            pxs = [fwd_s2(a) for a in aps]
            for (oo, cc), px in zip(wave, pxs):
                xs_, sd_ = Xs[cnt["x"] % 2], sd[cnt["x"] % 2]
                cnt["x"] += 1
                fw.copy("act", xs_[:], px[:])
                fw.tt("pool", sd_[:, 0:128], xs_[:, 0:128], xs_[:, 128:256], ALU.add)
                fw.tt("pool", sd_[:, 128:256], xs_[:, 256:384], xs_[:, 384:512], ALU.subtract)
                rsc = rden[:, oo * G + cc:oo * G + cc + 1]
                kv = kk[:, oo, cc, :]
                fw.ts("dve", kv[:, 0:256].rearrange("p (r k) -> p r k", r=2),
                      sd_[:, 0:128].unsqueeze(1).broadcast_to([128, 2, 128]), rsc, None, op0=ALU.mult)
                fw.ts("dve", kv[:, 384:512], sd_[:, 128:256], rsc, None, op0=ALU.mult)
                fw.ts("dve", kv[:, 256:384], sd_[:, 128:256], rsc, -1.0, op0=ALU.mult, op1=ALU.mult)
        for oo in range(2):
            zin = uc[2]
            gate = uc[0] if oo == 0 else uc[1]
            zout = uc[2]
            prs = list(range(G // 2))
            pas = []
            for p in prs:
                pa = nps()
                for ci in range(2):
                    cc = 2 * p + ci
                    fw.mm(pa[:, ci * 256:(ci + 1) * 256], zin[:, 0, cc, :], K["F1a"][:], start=True, stop=False)
                    fw.mm(pa[:, ci * 256:(ci + 1) * 256], zin[:, 1, cc, :], K["F1b"][:], start=False, stop=True)
                pas.append(pa)
            aps = [cmul(pa[:], "crk", TT4, nTim_b, Tim_b, "rck") for pa in pas]
            pxs = [fwd_s2(a) for a in aps]
            yps = []
            for p, px in zip(prs, pxs):
                kp = kk[:, oo, 2 * p:2 * p + 2, :]
                yps.append(cmul(px[:], "rck", kp[:, :, 0:256].rearrange("p c (r k) -> p c r k", r=2),
                                kp[:, :, 256:384], kp[:, :, 384:512], "crk"))
            pbs = []
            for yp in yps:
                pb = nps()
                y4 = vw(yp[:], "crk")
                for ci in range(2):
                    fw.mm(pb[:, ci * 256:(ci + 1) * 256], y4[:, ci, 0, :], K["Gc"][:], start=True, stop=False)
                    fw.mm(pb[:, ci * 256:(ci + 1) * 256], y4[:, ci, 1, :], K["Gd"][:], start=False, stop=True)
                pbs.append(pb)
            bps = [cmul(pb[:], "crk", TT4, Tim_b, nTim_b, "rck") for pb in pbs]
            pys = []
            for bp in bps:
                py = nps()
                yv = py[0:64, :].rearrange("p (b c n) -> p b c n", b=2, c=2)
                fw.mm(yv[:, 0, :, :], K["iF1re"][:], bp[:, 0:256], start=True, stop=False)
                fw.mm(yv[:, 0, :, :], K["iF1im"][:], bp[:, 256:512], start=False, stop=True)
                fw.mm(yv[:, 1, :, :], K["iF1re"][:], bp[:, 256:512], start=True, stop=False)
                fw.mm(yv[:, 1, :, :], K["niF1im"][:], bp[:, 0:256], start=False, stop=True)
                pys.append(py)
            for p, py in zip(prs, pys):
                yv = py[0:64, :].rearrange("p (b c n) -> p b c n", b=2, c=2)
                gt = gtl[cnt["g"] % 4]
                cnt["g"] += 1
                base = oo * CH + c0 + 2 * p
                for ci in range(2):
                    fw.act(gt[:, :, ci, :], zin[:, :, 2 * p + ci, :], AF.Copy, scale=sks[:, base + ci:base + ci + 1])
                fw.tt("dve", gt[:], yv, gt[:], ALU.add)
                fw.tt("pool", zout[:, :, 2 * p:2 * p + 2, :], gate[:, :, 2 * p:2 * p + 2, :], gt[:], ALU.mult)
        fw.dma("sp", o[g].rearrange("p (b c n) -> p b c n", b=2, c=G), uc[2][:], is_output=True)
    return fw


def hy_host_inputs(proj, hy_w, core):
    G, NG, CH = HG, HNG, HCH
    ch0 = core * CH
    Umat = np.empty((NG, 3, 3, 64, 2 * G * 128), np.float32)
    for s in range(3):
        a = proj[:, :, s * 768 + ch0: s * 768 + ch0 + CH]
        ap = np.pad(a, ((0, 0), (1, 1), (0, 0)))
        for j in range(3):
            sh = ap[:, j:j + T, :]
            x = sh.transpose(0, 2, 1).reshape(B, NG, G, 64, 128)
            Umat[:, s, j] = x.transpose(1, 3, 0, 2, 4).reshape(NG, 64, 2 * G * 128)
    d = {"U": Umat}
    d.update(hy_w[core])
    return d


def hy_host_weights(short_w, w1, b1, w2, b2, w3, freq, skip):
    G, NG, CH = HG, HNG, HCH
    L = T
    m = np.arange(L, dtype=np.float64)
    tt_ = m / (L - 1)
    wv = 2.0 * np.pi * m / L
    bands = np.linspace(1e-4, 15.0, 16)
    feats = np.concatenate([tt_[:, None], np.cos(bands[None] * wv[:, None]), -np.sin(bands[None] * wv[:, None])], 1)
    perm = (np.arange(64)[None, :] * 128 + np.arange(128)[:, None]).reshape(-1)
    featsT = np.ascontiguousarray(feats[perm].T, dtype=np.float32)
    max_decay = np.log(1e-2) / 0.3
    min_decay = np.log(1e-2) / 1.5
    deltas = np.abs(np.linspace(min_decay, max_decay, 768))
    dec_full = np.exp(-tt_[:, None] * deltas[None, :])
    consts = hy_host_consts()
    outs = []
    w3r = w3.reshape(64, 2, 2, 768)
    for core in range(NCORES):
        ch0 = core * CH
        d = {"featsT": featsT, "w1": np.ascontiguousarray(w1), "w2": np.ascontiguousarray(w2),
             "b1": np.ascontiguousarray(b1.reshape(64, 1)), "b2": np.ascontiguousarray(b2.reshape(64, 1)),
             "fr": np.ascontiguousarray(freq.reshape(64, 1))}
        w3c = w3r[:, :, :, ch0:ch0 + CH].reshape(64, 2, 2, NG, G)
        d["w3"] = np.ascontiguousarray(w3c.transpose(3, 0, 1, 2, 4).reshape(NG, 64, 4 * G))
        dc = dec_full[:, ch0:ch0 + CH].reshape(64, 128, NG, G)
        d["decay"] = np.ascontiguousarray(dc.transpose(2, 0, 3, 1).reshape(NG, 64, G * 128), dtype=np.float32)
        swc = short_w.reshape(3, 3, 768)[:, :, ch0:ch0 + CH]
        swl = swc.transpose(1, 0, 2).reshape(1, 9 * CH)
        d["sw"] = np.ascontiguousarray(np.broadcast_to(swl, (64, 9 * CH)))
        skl = skip[:, ch0:ch0 + CH].reshape(1, 2 * CH)
        d["skip"] = np.ascontiguousarray(np.broadcast_to(skl, (64, 2 * CH)))
        for k, v in consts.items():
            d["c_" + k] = v
        outs.append(d)
    return outs


def hy_host_gather(results):
    G, NG, CH = HG, HNG, HCH
    main = np.empty((B, T, 768), np.float32)
    for core, r in enumerate(results):
        x = r.reshape(NG, 64, B, G, 128)
        x = x.transpose(2, 1, 4, 0, 3).reshape(B, T, CH)
        main[:, :, core * CH:(core + 1) * CH] = x
    return main


def at_host_consts():
    rows = T // 64
    pos_row = np.repeat(np.arange(rows), 64).astype(np.float64)
    pos_col = np.tile(np.arange(64), rows).astype(np.float64)
    inv = 1.0 / (10000.0 ** (np.arange(0, 32, 2, dtype=np.float64) / 32))
    ang = np.stack([pos_row[:, None] * inv, pos_col[:, None] * inv], 1)
    d = np.arange(64)
    axis = d // 32
    f = d % 16
    cosT = np.cos(ang[:, axis, f]).T
    sinT = np.sin(ang[:, axis, f]).T
    R = np.zeros((64, 64))
    for a in range(2):
        for ff in range(16):
            i0 = a * 32 + ff
            i1 = a * 32 + 16 + ff
            R[i0, i1] = -1.0
            R[i1, i0] = 1.0
    f32 = lambda a: np.ascontiguousarray(a, dtype=np.float32)
    return {"cosT": f32(cosT), "sinT": f32(sinT), "rotT": f32(R.T)}


def build_AT():
    fw = Fw()
    qT = fw.dram_in("qT", [3, 64, T])
    kT = fw.dram_in("kT", [64, T])
    v = fw.dram_in("v", [T, 64])
    gq = fw.dram_in("gq", [64, 1])
    gk = fw.dram_in("gk", [64, 1])
    cosT = fw.dram_in("cosT", [64, T])
    sinT = fw.dram_in("sinT", [64, T])
    rotT = fw.dram_in("rotT", [64, 64])
    o = fw.dram_out("o", [3, 64, T])
    c = consts_common(fw)
    cs = fw.sb("cs", [64, T], F32); fw.dma("sp", cs[:], cosT)
    sn = fw.sb("sn", [64, T], F32); fw.dma("sp", sn[:], sinT)
    rot = fw.sb("rot", [64, 64], F32); fw.dma("sp", rot[:], rotT)
    gqs = fw.sb("gqs", [64, 1], F32); fw.dma("sp", gqs[:], gq)
    gks = fw.sb("gks", [64, 1], F32); fw.dma("sp", gks[:], gk)
    qb = [fw.sb(f"qb{h}", [128, T], BF16) for h in range(3)]
    kb = fw.sb("kb", [128, T], BF16)
    for t_ in qb + [kb]:
        fw.memset("pool", t_[64:128, :], 0.0)
    vst = fw.sb("vst", [128, 64, 64], F32)
    va = fw.sb("va", [128, 64, 128], BF16)
    fw.dma("sp", vst[:], v.rearrange("(c p) d -> p c d", p=128))
    fw.memset("pool", va[:, :, 64:128], 0.0)
    fw.copy("dve", va[:, :, 0:64], vst[:])
    fw.memset("dve", va[:, :, 64:65], 1.0)
    xin = [fw.sb(f"xin{i}", [64, TT], F32) for i in range(2)]
    sq = fw.sb("sqa", [64, TT], F32)
    rstd = fw.sb("rstda", [64, TT], F32)
    xn = fw.sb("xna", [64, TT], F32)
    ra = fw.sb("ra", [64, TT], F32)
    rb = fw.sb("rb", [64, TT], F32)
    psA = fw.ps("psA", [128, TT], F32)
    psB = fw.ps("psB", [128, TT], F32)
    psS = [fw.ps(f"psS{i}", [128, TT], F32) for i in range(3)]
    psO = [fw.ps(f"psO{i}", [128, TT], F32) for i in range(2)]
    cnt = 0
    for src, g, dst in [(kT, gks, kb)] + [(qT[h], gqs, qb[h]) for h in range(3)]:
        for ti in range(T // TT):
            x = xin[cnt % 2]
            cnt += 1
            sl = slice(ti * TT, (ti + 1) * TT)
            fw.dma("sp", x[:], src[:, sl])
            fw.act(sq[:], x[:], AF.Square)
            fw.mm(psA[0:64, :], c["ones"][0:64, 0:64], sq[:])
            fw.act(rstd[:], psA[0:64, :], AF.Sqrt, bias=c["eps"][0:64, :], scale=1.0 / 64)
            fw.op("dve", lambda e: e.reciprocal(rstd[:], rstd[:]), [rstd[:]], [rstd[:]])
            fw.stt("dve", xn[:], x[:], g[:, 0:1], rstd[:], ALU.mult, ALU.mult)
            fw.mm(psB[0:64, :], rot[:], xn[:])
            fw.tt("dve", rb[:], psB[0:64, :], sn[:, sl], ALU.mult)
            fw.tt("pool", ra[:], xn[:], cs[:, sl], ALU.mult)
            fw.tt("pool", dst[0:64, sl], ra[:], rb[:], ALU.add)
    pt = [fw.sb(f"pt{i}", [128, TT], BF16) for i in range(3)]
    lsb = fw.sb("lsb", [128, TT], F32)
    rec = fw.sb("rec", [64, TT], F32)
    ost = [fw.sb(f"ost{i}", [64, TT], F32) for i in range(2)]
    it = 0
    blk = 0
    for h in range(3):
        for qi in range(T // TT):
            qs = slice(qi * TT, (qi + 1) * TT)
            po = psO[blk % 2]
            for kc in range(2):
                fw.mm(psS[(it + kc) % 3][:], kb[:, kc * 128:(kc + 1) * 128], qb[h][:, qs])
            for kc in range(64):
                ps = psS[it % 3]
                p = pt[it % 3]
                fw.act(p[:], ps[:], AF.Exp, scale=0.125)
                if kc + 2 < 64:
                    fw.mm(psS[(it + 2) % 3][:], kb[:, (kc + 2) * 128:(kc + 3) * 128], qb[h][:, qs])
                fw.mm(po[:], va[:, kc, :], p[:], start=(kc == 0), stop=(kc == 63))
                it += 1
            fw.copy("act", lsb[64:65, :], po[64:65, :])
            fw.mm(psA[0:64, :], c["ones"][64:65, 0:64], lsb[64:65, :])
            fw.op("dve", lambda e: e.reciprocal(rec[:], psA[0:64, :]), [psA[0:64, :]], [rec[:]])
            os_ = ost[blk % 2]
            fw.tt("dve", os_[:], po[0:64, :], rec[:], ALU.mult)
            fw.dma("sp", o[h, :, qs], os_[:], is_output=True)
            blk += 1
    return fw


def build_O():
    fw = Fw()
    hT = fw.dram_in("hT", [D, NT])
    mainT = fw.dram_in("mainT", [768, NT])
    cqT = fw.dram_in("cqT", [4, 64, NT])
    memT = fw.dram_in("memT", [D, 256])
    g_mem = fw.dram_in("g_mem", [128, 8])
    w_kv = fw.dram_in("w_kv", [D, 512])
    w_out = fw.dram_in("w_out", [D, D])
    g_ffn = fw.dram_in("g_ffn", [128, 8])
    w_r = fw.dram_in("w_r", [D, 16])
    ident = fw.dram_in("ident", [128, 128])
    ho = fw.dram_out("ho", [D, NT])
    affo = fw.dram_out("aff", [NT, 16])
    c = consts_common(fw)
    xT = fw.sb("xT", [128, 8, NT], F32)
    gm = fw.sb("gm", [128, 8], F32); fw.dma("sp", gm[:], g_mem)
    gf = fw.sb("gf", [128, 8], F32); fw.dma("sp", gf[:], g_ffn)
    wr = fw.sb("wr", [128, 8, 16], F32); fw.dma("sp", wr[:], w_r.rearrange("(c p) e -> p c e", p=128))
    idf = fw.sb("idf", [128, 128], F32); fw.dma("sp", idf[:], ident)
    idb = fw.sb("idb", [128, 128], BF16); fw.copy("dve", idb[:], idf[:])
    mT = fw.sb("mT", [128, 8, 256], F32); fw.dma("sp", mT[:], memT.rearrange("(c p) m -> p c m", p=128))
    wkv = fw.sb("wkv", [128, 8, 512], BF16); fw.dma("pool", wkv[:], w_kv.rearrange("(c p) f -> p c f", p=128))
    main_bf = fw.sb("main_bf", [128, 6, NT], BF16); fw.dma("pool", main_bf[:], mainT.rearrange("(c p) t -> p c t", p=128))
    cq_bf = fw.sb("cq_bf", [64, 4, NT], BF16); fw.dma("pool", cq_bf[:], cqT.rearrange("h p t -> p h t"))
    wo_bf = fw.sb("wo_bf", [128, 6, D], BF16); fw.dma("pool", wo_bf[:], w_out[0:768, :].rearrange("(c p) d -> p c d", p=128))
    woc_bf = fw.sb("woc_bf", [64, 4, D], BF16); fw.dma("pool", woc_bf[:], w_out[768:1024, :].rearrange("(h p) d -> p h d", p=64))
    cross_bf = fw.sb("cross_bf", [64, 4, NT], BF16)
    for ti in range(NT // TT):
        for ch in range(8):
            fw.dma("sp", xT[:, ch, ti * TT:(ti + 1) * TT], hT[ch * 128:(ch + 1) * 128, ti * TT:(ti + 1) * TT])
    sq = [fw.sb(f"sq{i}", [128, TT], F32) for i in range(2)]
    rstd = fw.sb("rstd", [128, TT], F32)
    ps_ss = fw.ps("ps_ss", [128, TT], F32)
    psS = [fw.ps(f"psS{i}", [128, TT], F32) for i in range(2)]
    psT = [fw.ps(f"psT{i}", [128, 128], BF16) for i in range(2)]
    psO = fw.ps("psO", [128, TT], F32)
    psP = [fw.ps(f"psP{i}", [128, TT], F32) for i in range(2)]
    memn = fw.sb("memn", [128, 8, 256], BF16)
    emit_rmsnorm(fw, c, mT, gm, memn, 0, 256, sq, ps_ss, rstd)
    mkT = fw.sb("mkT", [64, 4, 256], BF16)
    mv = fw.sb("mv", [128, 2, 256], BF16)
    for h in range(4):
        for ch in range(8):
            fw.mm(psS[0][0:64, 0:256], wkv[:, ch, h * 64:(h + 1) * 64], memn[:, ch, :], start=(ch == 0), stop=(ch == 7))
        fw.copy("act", mkT[:, h, :], psS[0][0:64, 0:256])
    for mc in range(2):
        for ch in range(8):
            fw.mm(psS[1][:, 0:256], memn[:, ch, mc * 128:(mc + 1) * 128], wkv[:, ch, 256:512], start=(ch == 0), stop=(ch == 7))
        fw.copy("act", mv[:, mc, :], psS[1][:, 0:256])
    pe_ = [fw.sb(f"pe{i}", [128, 256], F32) for i in range(2)]
    pn = [fw.sb(f"pn{i}", [128, 256], BF16) for i in range(2)]
    pT = [fw.sb(f"pT{i}", [128, 128], BF16) for i in range(4)]
    st = [fw.sb(f"st{i}", [128, 4], F32) for i in range(2)]
    it = 0
    tcnt = 0
    for tile in range(NT // TT):
        for h in range(4):
            for qp in range(0, 4, 2):
                units = [(qp + u, u) for u in range(2)]
                for qb_, i in units:
                    q0 = tile * TT + qb_ * 128
                    fw.mm(psS[i][:, 0:256], cq_bf[:, h, q0:q0 + 128], mkT[:, h, :])
                for qb_, i in units:
                    fw.reduce("dve", st[i][:, 0:1], psS[i][:, 0:256], ALU.max)
                for qb_, i in units:
                    fw.ts("dve", st[i][:, 1:2], st[i][:, 0:1], -0.125, None, op0=ALU.mult)
                for qb_, i in units:
                    fw.act(pe_[i][:], psS[i][:, 0:256], AF.Exp, bias=st[i][:, 1:2], scale=0.125)
                for qb_, i in units:
                    fw.reduce("dve", st[i][:, 2:3], pe_[i][:], ALU.add)
                for qb_, i in units:
                    s_ = st[i]
                    fw.op("dve", lambda e, s_=s_: e.reciprocal(s_[:, 3:4], s_[:, 2:3]), [s_[:, 2:3]], [s_[:, 3:4]])
                for qb_, i in units:
                    fw.ts("dve", pn[i][:], pe_[i][:], st[i][:, 3:4], None, op0=ALU.mult)
                for mc in range(2):
                    for qb_, i in units:
                        fw.transpose(psT[i][:], pn[i][:, mc * 128:(mc + 1) * 128], idb[:])
                    for qb_, i in units:
                        fw.copy("act", pT[2 * mc + i][:], psT[i][:])
                for qb_, i in units:
                    for mc in range(2):
                        fw.mm(psO[0:64, qb_ * 128:(qb_ + 1) * 128], mv[:, mc, h * 64:(h + 1) * 64], pT[2 * mc + i][:], start=(mc == 0), stop=(mc == 1))
                it += 2
            fw.copy("act", cross_bf[:, h, tile * TT:(tile + 1) * TT], psO[0:64, :])
    xn = fw.sb("xn", [128, 8, TT], F32)
    lg = fw.sb("lg", [128, 16], F32)
    affs = fw.sb("affs", [128, 16, 16], F32)
    pc = 0
    for tile in range(NT // TT):
        ts_ = slice(tile * TT, (tile + 1) * TT)
        for j in range(8):
            ps = psP[pc % 2]
            pc += 1
            js = slice(j * 128, (j + 1) * 128)
            for cc in range(6):
                fw.mm(ps[:], wo_bf[:, cc, js], main_bf[:, cc, ts_], start=(cc == 0), stop=False)
            for h in range(4):
                fw.mm(ps[:], woc_bf[:, h, js], cross_bf[:, h, ts_], start=False, stop=(h == 3))
            fw.tt("dve", xT[:, j, ts_], xT[:, j, ts_], ps[:], ALU.add)
            fw.dma("sp", ho[js, ts_], xT[:, j, ts_], is_output=True)
        emit_rmsnorm(fw, c, xT, gf, xn, tile * TT, TT, sq, ps_ss, rstd)
        for b_ in range(4):
            blk = tile * 4 + b_
            s_ = st[it % 2]
            it += 1
            pl = psO[:, 0:16]
            for ch in range(8):
                fw.mm(pl, xn[:, ch, b_:TT:4], wr[:, ch, :], start=(ch == 0), stop=(ch == 7))
            fw.reduce("dve", s_[:, 0:1], pl, ALU.max)
            fw.ts("dve", s_[:, 1:2], s_[:, 0:1], -1.0, None, op0=ALU.mult)
            fw.act(lg[:], pl, AF.Exp, bias=s_[:, 1:2], scale=1.0)
            fw.reduce("dve", s_[:, 2:3], lg[:], ALU.add)
            fw.op("dve", lambda e, s_=s_: e.reciprocal(s_[:, 3:4], s_[:, 2:3]), [s_[:, 2:3]], [s_[:, 3:4]])
            fw.ts("dve", affs[:, blk, :], lg[:], s_[:, 3:4], None, op0=ALU.mult)
        fw.dma("sp", affo[tile * TT:(tile + 1) * TT, :].rearrange("(p b) e -> p b e", b=4),
               affs[:, tile * 4:(tile + 1) * 4, :], is_output=True)
    return fw


N_BISECT = 34
CAP = 1024


def m_host_consts():
    p = np.arange(128)
    Gm = (p[:, None] // 8 == p[None, :] // 8).astype(np.float32)
    G16 = (p[:, None] // 8 == np.arange(16)[None, :]).astype(np.float32)
    selm = np.zeros((16, 16, 128), np.float32)
    for e in range(16):
        selm[e, e, :] = 1.0
    return {"Gm": Gm, "G16": G16, "selm": selm.reshape(16, 2048)}


def build_M():
    fw = Fw()
    hT = fw.dram_in("hT", [D, NT])
    g_ffn = fw.dram_in("g_ffn", [128, 8])
    affP = fw.dram_in("affP", [128, 1024])
    affT = fw.dram_in("affT", [16, NT])
    Gm_d = fw.dram_in("Gm", [128, 128])
    G16_d = fw.dram_in("G16", [128, 16])
    selm_d = fw.dram_in("selm", [16, 2048])
    wg = fw.dram_in("wg", [16, D, 768])
    wu = fw.dram_in("wu", [16, D, 768])
    wd = fw.dram_in("wd", [16, 768, D])
    ho = fw.dram_out("ho", [D, NT])
    c = consts_common(fw)
    xT = fw.sb("xT", [128, 8, NT], F32)
    gf = fw.sb("gf", [128, 8], F32); fw.dma("sp", gf[:], g_ffn)
    aP = fw.sb("aP", [128, 1024], F32); fw.dma("sp", aP[:], affP)
    wT = fw.sb("wT", [16, NT], F32); fw.dma("sp", wT[:], affT)
    Gm = fw.sb("Gm_s", [128, 128], F32); fw.dma("sp", Gm[:], Gm_d)
    selm = fw.sb("selm_s", [16, 2048], F32); fw.dma("sp", selm[:], selm_d)
    for ti in range(NT // TT):
        for ch in range(8):
            fw.dma("sp", xT[:, ch, ti * TT:(ti + 1) * TT], hT[ch * 128:(ch + 1) * 128, ti * TT:(ti + 1) * TT])
    wgb = [fw.sb(f"wgb{i}", [128, 8, 384], BF16) for i in range(2)]
    wub = [fw.sb(f"wub{i}", [128, 8, 384], BF16) for i in range(2)]
    wdb = [fw.sb(f"wdb{i}", [128, 3, D], BF16) for i in range(2)]

    def load_w(e, half, i):
        fs = slice(half * 384, (half + 1) * 384)
        fw.dma("pool", wgb[i][:], wg[e][:, fs].rearrange("(c p) f -> p c f", p=128))
        fw.dma("pool", wub[i][:], wu[e][:, fs].rearrange("(c p) f -> p c f", p=128))
        fw.dma("pool", wdb[i][:], wd[e][fs, :].rearrange("(c p) d -> p c d", p=128))

    load_w(0, 0, 0)
    load_w(0, 1, 1)
    sq = [fw.sb(f"sq{i}", [128, TT], F32) for i in range(2)]
    rstd = fw.sb("rstd", [128, TT], F32)
    ps_ss = fw.ps("ps_ss", [128, TT], F32)
    psG = [fw.ps(f"psG{i}", [128, TT], F32) for i in range(2)]
    psU = [fw.ps(f"psU{i}", [128, TT], F32) for i in range(2)]
    psY = [fw.ps(f"psY{i}", [128, TT], F32) for i in range(2)]
    psW = fw.ps("psW", [128, TT], F32)
    xn = fw.sb("xn", [128, 8, NT], BF16)
    for tile in range(NT // TT):
        emit_rmsnorm(fw, c, xT, gf, xn[:, :, tile * TT:(tile + 1) * TT], tile * TT, TT, sq, ps_ss, rstd)
    cmp_ = fw.sb("cmp", [128, 1024], F32)
    cn = fw.sb("cn", [128, 1], F32)
    bs = fw.sb("bs128", [128, 4], F32)
    lo, mid, ge = (bs[:, k:k + 1] for k in range(3))
    w = 0.75
    fw.memset("dve", lo, 0.0)
    fw.memset("dve", mid, w)
    for itn in range(N_BISECT):
        fw.ts("dve", cmp_[:], aP[:], mid, None, op0=ALU.is_ge)
        fw.reduce("dve", cn[:], cmp_[:], ALU.add)
        fw.mm(psW[:, 0:1], Gm[:], cn[:])
        fw.ts("dve", ge, psW[:, 0:1], float(CAP) - 0.5, None, op0=ALU.is_ge)
        fw.stt("dve", lo, ge, w, lo, ALU.mult, ALU.add)
        w = w * 0.5
        fw.ts("dve", mid, lo, w, None, op0=ALU.add)
    thr_d = fw.dram_tmp("thr_d", [128, 1])
    thr16 = fw.sb("thr16", [16, 1], F32)
    fw.dma("sp", thr_d, lo)
    fw.dma("sp", thr16[:], thr_d.rearrange("(e s) o -> e (s o)", s=8)[:, 0:1], allow_slow_non_contiguous=True)
    fw.stt("dve", wT[:], wT[:], thr16[:, 0:1], wT[:], ALU.is_ge, ALU.mult)
    sg = [fw.sb(f"sg{i}", [128, TT], F32) for i in range(2)]
    wbc = [fw.sb(f"wbc{i}", [128, TT], F32) for i in range(2)]
    hid = [fw.sb(f"hid{i}", [128, 3, TT], BF16) for i in range(2)]
    gi = [0]
    yi = [0]
    steps = [(k, tile) for k in range(32) for tile in range(NT // TT)]

    def gateup(sidx):
        k, tile = steps[sidx]
        e, i = k // 2, k % 2
        ts_ = slice(tile * TT, (tile + 1) * TT)
        wb = wbc[sidx % 2]
        fw.mm(psW[:], selm[:, e * 128:(e + 1) * 128], wT[:, ts_])
        fw.copy("act", wb[:], psW[:])
        hd = hid[sidx % 2]
        for f in range(3):
            pg = psG[gi[0] % 2]
            pu = psU[gi[0] % 2]
            s_ = sg[gi[0] % 2]
            gi[0] += 1
            fs = slice(f * 128, (f + 1) * 128)
            for ch in range(8):
                fw.mm(pg[:], wgb[i][:, ch, fs], xn[:, ch, ts_], start=(ch == 0), stop=(ch == 7))
            for ch in range(8):
                fw.mm(pu[:], wub[i][:, ch, fs], xn[:, ch, ts_], start=(ch == 0), stop=(ch == 7))
            fw.act(s_[:], pg[:], AF.Silu)
            fw.tt("dve", s_[:], s_[:], wb[:], ALU.mult)
            fw.tt("dve", hd[:, f, :], s_[:], pu[:], ALU.mult)

    def down(sidx):
        k, tile = steps[sidx]
        i = k % 2
        ts_ = slice(tile * TT, (tile + 1) * TT)
        hd = hid[sidx % 2]
        for j in range(8):
            py = psY[yi[0] % 2]
            yi[0] += 1
            for f in range(3):
                fw.mm(py[:], wdb[i][:, f, j * 128:(j + 1) * 128], hd[:, f, :], start=(f == 0), stop=(f == 2))
            fw.tt("dve", xT[:, j, ts_], xT[:, j, ts_], py[:], ALU.add)
        if tile == NT // TT - 1 and k + 2 < 32:
            load_w((k + 2) // 2, (k + 2) % 2, (k + 2) % 2)

    for sidx in range(len(steps)):
        gateup(sidx)
        if sidx > 0:
            down(sidx - 1)
    down(len(steps) - 1)
    for ch in range(8):
        fw.dma("sp", ho[ch * 128:(ch + 1) * 128, :], xT[:, ch, :], is_output=True)
    return fw


def build_F():
    fw = Fw()
    hT = fw.dram_in("hT", [D, NT])
    g = fw.dram_in("g", [128, 8])
    o = fw.dram_out("o", [D, NT])
    c = consts_common(fw)
    xT = fw.sb("xT", [128, 8, NT], F32)
    for ch in range(8):
        fw.dma("sp", xT[:, ch, :], hT[ch * 128:(ch + 1) * 128, :])
    gs = fw.sb("gs", [128, 8], F32); fw.dma("sp", gs[:], g)
    sq = [fw.sb(f"sq{i}", [128, TT], F32) for i in range(2)]
    rstd = fw.sb("rstd", [128, TT], F32)
    ps_ss = fw.ps("ps_ss", [128, TT], F32)
    un = [fw.sb(f"un{i}", [128, 8, TT], F32) for i in range(2)]
    for tile in range(NT // TT):
        u = un[tile % 2]
        emit_rmsnorm(fw, c, xT, gs, u, tile * TT, TT, sq, ps_ss, rstd)
        for ch in range(8):
            fw.dma("sp", o[ch * 128:(ch + 1) * 128, tile * TT:(tile + 1) * TT], u[:, ch, :], is_output=True)
    return fw


def _lay(g):
    return np.ascontiguousarray(np.asarray(g, np.float32).reshape(8, 128).T)


def _run(fw, in_maps):
    nc = fw.finish()
    res = run_bass_kernel_spmd(nc, in_maps, core_ids=list(range(NCORES)))
    return res.results


def kernel(x, mem, mix_norm_g, ffn_norm_g, mem_norm_g, final_norm_g, w_mem_kv, w_out,
           hy_w_in, hy_short_w, hy_filt_w1, hy_filt_b1, hy_filt_w2, hy_filt_b2, hy_filt_w3,
           hy_filt_freq, hy_skip, at_w_in, at_q_norm_g, at_k_norm_g,
           router_w, exp_w_gate, exp_w_up, exp_w_down):
    f32 = lambda a: np.ascontiguousarray(np.asarray(a), dtype=np.float32)
    x = f32(x); mem = f32(mem)
    h = x.reshape(B * T, D)
    hT = [np.ascontiguousarray(h[c * NT:(c + 1) * NT].T) for c in range(NCORES)]
    memT = [np.ascontiguousarray(mem[b].T) for b in range(B)]
    ident = np.eye(128, dtype=np.float32)
    mconst = m_host_consts()
    aconst = at_host_consts()
    for i in range(4):
        j = i // 2
        hyena = (i % 2 == 0)
        w_in = f32(hy_w_in[j]) if hyena else f32(at_w_in[j])
        Dp = w_in.shape[1]
        g_mix = _lay(mix_norm_g[i])
        res = _run(build_P(Dp), [{"hT": hT[c], "g": g_mix, "w": w_in} for c in range(NCORES)])
        proj = np.concatenate([r["o"].T for r in res], 0).reshape(B, T, Dp)
        if hyena:
            hw = hy_host_weights(f32(hy_short_w[j]), f32(hy_filt_w1[j]), f32(hy_filt_b1[j]), f32(hy_filt_w2[j]),
                                 f32(hy_filt_b2[j]), f32(hy_filt_w3[j]), f32(hy_filt_freq[j]), f32(hy_skip[j]))
            res = _run(build_HY(), [hy_host_inputs(proj, hw, c) for c in range(NCORES)])
            main = hy_host_gather([r["o"] for r in res])
        else:
            gq = f32(at_q_norm_g[j]).reshape(64, 1)
            gk = f32(at_k_norm_g[j]).reshape(64, 1)
            ims = []
            for c in range(NCORES):
                b, g = c // 4, c % 4
                q = proj[b, :, g * 192:(g + 1) * 192].reshape(T, 3, 64)
                d = {"qT": np.ascontiguousarray(q.transpose(1, 2, 0)),
                     "kT": np.ascontiguousarray(proj[b, :, 768 + g * 64:768 + (g + 1) * 64].T),
                     "v": np.ascontiguousarray(proj[b, :, 1024 + g * 64:1024 + (g + 1) * 64]),
                     "gq": gq, "gk": gk}
                d.update(aconst)
                ims.append(d)
            res = _run(build_AT(), ims)
            main = np.empty((B, T, 768), np.float32)
            for c in range(NCORES):
                b, g = c // 4, c % 4
                main[b, :, g * 192:(g + 1) * 192] = res[c]["o"].transpose(2, 0, 1).reshape(T, 192)
        mainf = main.reshape(B * T, 768)
        cqf = proj[:, :, Dp - 256:].reshape(B * T, 256)
        ims = []
        for c in range(NCORES):
            sl = slice(c * NT, (c + 1) * NT)
            ims.append({"hT": hT[c], "mainT": np.ascontiguousarray(mainf[sl].T),
                        "cqT": np.ascontiguousarray(cqf[sl].T.reshape(4, 64, NT)), "memT": memT[c // 4],
                        "g_mem": _lay(mem_norm_g), "w_kv": f32(w_mem_kv[i]), "w_out": f32(w_out[i]),
                        "g_ffn": _lay(ffn_norm_g[i]), "w_r": f32(router_w[i]), "ident": ident})
        res = _run(build_O(), ims)
        hT = [r["ho"] for r in res]
        aff = np.concatenate([r["aff"] for r in res], 0)
        wg_, wu_, wd_ = f32(exp_w_gate[i]), f32(exp_w_up[i]), f32(exp_w_down[i])
        ims = []
        for c in range(NCORES):
            b = c // 4
            ab = aff[b * T:(b + 1) * T]
            d = {"hT": hT[c], "g_ffn": _lay(ffn_norm_g[i]),
                 "affP": np.ascontiguousarray(ab.T.reshape(128, 1024)),
                 "affT": np.ascontiguousarray(aff[c * NT:(c + 1) * NT].T),
                 "wg": wg_, "wu": wu_, "wd": wd_}
            d.update(mconst)
            ims.append(d)
        res = _run(build_M(), ims)
        hT = [r["ho"] for r in res]
    res = _run(build_F(), [{"hT": hT[c], "g": _lay(final_norm_g)} for c in range(NCORES)])
    out = np.concatenate([r["o"].T for r in res], 0).reshape(B, T, D)
    return np.ascontiguousarray(out, dtype=np.float32)
```

```python
from contextlib import ExitStack
import numpy as np
import concourse.bass as bass
import concourse.mybir as mybir
from concourse.bass_utils import run_bass_kernel_spmd

F32 = mybir.dt.float32
BF16 = mybir.dt.bfloat16
I32 = mybir.dt.int32
ALU = mybir.AluOpType
AF = mybir.ActivationFunctionType
AX = mybir.AxisListType

SEM_LIMIT = 16000
N_DMA_SLOTS = 6


def _box(ap):
    t = ap.tensor
    name = t.name
    off = int(ap.offset)
    dims = ap.ap
    space = str(ap.space)
    if space == "DRAM":
        ext = sum((c - 1) * abs(s) for s, c in dims)
        return (name, 0, 1, off, off + ext + 1)
    shp = t.shape
    pstride = 1
    for d in shp[1:]:
        pstride *= int(d)
    p_lo = off // pstride
    f_lo = off % pstride
    pc = 1
    fext = 0
    for s, c in dims:
        if s == pstride and c > 1:
            pc = c
        elif s >= pstride and c > 1:
            pc = max(pc, (c - 1) * (s // pstride) + 1)
        else:
            fext += (c - 1) * abs(s)
    return (name, p_lo, p_lo + pc, f_lo, f_lo + fext + 1)


def _overlap(a, b):
    return a[1] < b[2] and b[1] < a[2] and a[3] < b[4] and b[3] < a[4]


class Fw:
    ENGS = ("pe", "act", "dve", "pool", "sp")

    def __init__(self, name="k"):
        self.nc = bass.Bass("TRN2", target_bir_lowering=False)
        self.stack = ExitStack()
        self.prog = {e: [] for e in self.ENGS}
        self.cur = {}
        self.waited = {e: {} for e in self.ENGS}
        self.acc = {}
        self.nsem = 0
        self.sems = {}
        self.slots = {}
        self.slot_i = {}
        self.n_instr = 0
        self.out_tokens = []

    def dram_in(self, name, shape, dt=F32):
        return self.nc.dram_tensor(name, list(shape), dt, kind="ExternalInput").ap()

    def dram_out(self, name, shape, dt=F32):
        return self.nc.dram_tensor(name, list(shape), dt, kind="ExternalOutput").ap()

    def dram_tmp(self, name, shape, dt=F32):
        return self.nc.dram_tensor(name, list(shape), dt, kind="Internal").ap()

    def sb(self, name, shape, dt=F32):
        return self.stack.enter_context(self.nc.sbuf_tensor(name, list(shape), dt))

    def ps(self, name, shape, dt=F32):
        return self.stack.enter_context(self.nc.psum_tensor(name, list(shape), dt))

    def _newsem(self):
        self.nsem += 1
        s = self.stack.enter_context(self.nc.semaphore(f"s{self.nsem}"))
        self.sems[id(s)] = s
        return s

    def _deps(self, eng, reads, writes):
        toks = []
        for ap, is_w in [(a, False) for a in reads] + [(a, True) for a in writes]:
            b = _box(ap)
            for rec in self.acc.get(b[0], ()):
                tok, rw, rb, reng = rec
                if not (is_w or rw):
                    continue
                if not _overlap(b, rb):
                    continue
                if reng == "pe" and eng == "pe":
                    continue
                toks.append(tok)
        return toks

    def _record(self, eng, tok, reads, writes):
        for ap, is_w in [(a, False) for a in reads] + [(a, True) for a in writes]:
            b = _box(ap)
            lst = self.acc.setdefault(b[0], [])
            new = []
            for rec in lst:
                _, rw, rb, reng = rec
                if rb == b and reng == eng and rw == is_w and not eng.startswith("dma"):
                    continue
                if is_w and rb[1] >= b[1] and rb[2] <= b[2] and rb[3] >= b[3] and rb[4] <= b[4]:
                    continue
                new.append(rec)
            new.append((tok, is_w, b, eng))
            self.acc[b[0]] = new

    def _waits(self, eng, toks):
        w = {}
        for sem, val in toks:
            k = id(sem)
            if self.waited[eng].get(k, 0) >= val:
                continue
            if w.get(k, (None, 0))[1] < val:
                w[k] = (sem, val)
        for k, (sem, val) in w.items():
            self.waited[eng][k] = val
        return list(w.values())

    def _next_tok(self, eng):
        c = self.cur.get(eng)
        if c is None or c[1] >= SEM_LIMIT:
            c = [self._newsem(), 0]
            self.cur[eng] = c
        c[1] += 1
        return (c[0], c[1])

    def op(self, eng, fn, reads, writes):
        toks = self._deps(eng, reads, writes)
        waits = self._waits(eng, toks)
        tok = self._next_tok(eng)
        self.prog[eng].append((waits, fn, tok[0], 1))
        self._record(eng, tok, reads, writes)
        self.n_instr += 1
        return tok

    def dma(self, q, out, in_, is_output=False, **kw):
        toks = self._deps("dma" + q, [in_], [out])
        if q not in self.slots:
            self.slots[q] = [[self._newsem(), 0] for _ in range(N_DMA_SLOTS)]
        sl = self.slots[q]
        i = self.slot_i.get(q, 0)
        self.slot_i[q] = i + 1
        s = sl[i % N_DMA_SLOTS]
        if s[1] + 16 > SEM_LIMIT:
            toks.append((s[0], s[1]))
            s = [self._newsem(), 0]
            sl[i % N_DMA_SLOTS] = s
        if s[1] > 0:
            toks.append((s[0], s[1]))
        waits = self._waits(q, toks)
        s[1] += 16
        tok = (s[0], s[1])
        self.prog[q].append((waits, lambda e: e.dma_start(out=out, in_=in_, **kw), tok[0], 16))
        self._record("dma" + q, tok, [in_], [out])
        if is_output:
            self.out_tokens.append(tok)
        self.n_instr += 1
        return tok

    def mm(self, out, lhsT, rhs, start=True, stop=True):
        return self.op("pe", lambda e: e.matmul(out, lhsT, rhs, start=start, stop=stop), [lhsT, rhs], [out])

    def transpose(self, out, in_, ident):
        return self.op("pe", lambda e: e.transpose(out, in_, ident), [in_, ident], [out])

    def act(self, out, in_, func, bias=None, scale=None, accum_out=None, eng="act"):
        kw = {}
        rd = [in_]
        wr = [out]
        if bias is not None:
            kw["bias"] = bias
            if not isinstance(bias, (int, float)):
                rd.append(bias)
        if scale is not None:
            kw["scale"] = scale
            if not isinstance(scale, (int, float)):
                rd.append(scale)
        if accum_out is not None:
            kw["accum_out"] = accum_out
            wr.append(accum_out)
        return self.op("act", lambda e: e.activation(out, in_, func, **kw), rd, wr)

    def tt(self, eng, out, a, b, op):
        return self.op(eng, lambda e: e.tensor_tensor(out, a, b, op), [a, b], [out])

    def ts(self, eng, out, a, s1, s2=None, op0=ALU.mult, op1=None, accum_out=None):
        rd = [a]
        wr = [out]
        if not isinstance(s1, (int, float)):
            rd.append(s1)
        if s2 is not None and not isinstance(s2, (int, float)):
            rd.append(s2)
        kw = {}
        if op1 is not None:
            kw["op1"] = op1
        if accum_out is not None:
            kw["accum_out"] = accum_out
            wr.append(accum_out)
        return self.op(eng, lambda e: e.tensor_scalar(out, a, s1, s2, op0, **kw), rd, wr)

    def stt(self, eng, out, a, s, b, op0, op1):
        rd = [a, b]
        if not isinstance(s, (int, float)):
            rd.append(s)
        return self.op(eng, lambda e: e.scalar_tensor_tensor(out, a, s, b, op0, op1), rd, [out])

    def copy(self, eng, out, in_):
        if eng == "act":
            return self.op("act", lambda e: e.copy(out, in_), [in_], [out])
        return self.op(eng, lambda e: e.tensor_copy(out, in_), [in_], [out])

    def memset(self, eng, out, val):
        return self.op(eng, lambda e: e.memset(out, val), [], [out])

    def reduce(self, eng, out, in_, op, axis=AX.X):
        return self.op(eng, lambda e: e.tensor_reduce(out, in_, axis, op), [in_], [out])

    def finish(self):
        if self.out_tokens:
            waits = self._waits("sp", self.out_tokens)
            self.prog["sp"].append((waits, None, None, 0))
        nc = self.nc
        prog = self.prog

        def emit(e, lst):
            for waits, fn, sem, inc in lst:
                for s, v in waits:
                    e.wait_ge(s, v)
                if fn is not None:
                    fn(e).then_inc(sem, inc)

        with nc.Block() as block:
            @block.tensor
            def _(e):
                emit(e, prog["pe"])

            @block.scalar
            def _(e):
                emit(e, prog["act"])

            @block.vector
            def _(e):
                emit(e, prog["dve"])

            @block.gpsimd
            def _(e):
                emit(e, prog["pool"])

            @block.sync
            def _(e):
                emit(e, prog["sp"])
        self.stack.close()
        return nc


def run(fw, in_maps, n=8, trace=False):
    nc = fw.finish()
    res = run_bass_kernel_spmd(nc, in_maps, core_ids=list(range(n)), trace=trace)
    return res

D = 1024
B = 2
T = 8192
NT = 2048
TT = 512
EPS = 1e-6
NCORES = 8


def consts_common(fw):
    c = {}
    c["ones"] = fw.sb("ones", [128, 128], F32)
    fw.memset("dve", c["ones"][:], 1.0)
    c["eps"] = fw.sb("eps", [128, 1], F32)
    fw.memset("dve", c["eps"][:], EPS)
    return c


def emit_rmsnorm(fw, c, xT, g_sb, uT, t0, tw, sq, ps_ss, rstd, out_dt_scale=None):
    for ch in range(8):
        s = sq[ch % 2]
        eng = "act" if ch % 2 == 0 else "pool"
        if eng == "act":
            fw.act(s[:, :tw], xT[:, ch, t0:t0 + tw], AF.Square)
        else:
            fw.tt("pool", s[:, :tw], xT[:, ch, t0:t0 + tw], xT[:, ch, t0:t0 + tw], ALU.mult)
        fw.mm(ps_ss[:, :tw], c["ones"][:], s[:, :tw], start=(ch == 0), stop=(ch == 7))
    fw.act(rstd[:, :tw], ps_ss[:, :tw], AF.Sqrt, bias=c["eps"][:], scale=1.0 / D)
    fw.op("dve", lambda e: e.reciprocal(rstd[:, :tw], rstd[:, :tw]), [rstd[:, :tw]], [rstd[:, :tw]])
    for ch in range(8):
        fw.stt("dve", uT[:, ch, :tw], xT[:, ch, t0:t0 + tw], g_sb[:, ch:ch + 1], rstd[:, :tw], ALU.mult, ALU.mult)


def rmsnorm_nodve_pieces(fw, c, xT, g_sb, uT, t0, tw, sq, ps_ss, rstd, tmp):
    def stats():
        for ch in range(8):
            s = sq[ch % 2]
            if ch % 2 == 0:
                fw.act(s[:, :tw], xT[:, ch, t0:t0 + tw], AF.Square)
            else:
                fw.tt("pool", s[:, :tw], xT[:, ch, t0:t0 + tw], xT[:, ch, t0:t0 + tw], ALU.mult)
            fw.mm(ps_ss[:, :tw], c["ones"][:], s[:, :tw], start=(ch == 0), stop=(ch == 7))
        fw.act(rstd[:, :tw], ps_ss[:, :tw], AF.Ln, bias=c["eps"][:], scale=1.0 / D)
        fw.act(rstd[:, :tw], rstd[:, :tw], AF.Exp, scale=-0.5)

    def apply():
        for ch in range(8):
            t_ = tmp[ch % 2]
            fw.tt("pool", t_[:, :tw], xT[:, ch, t0:t0 + tw], rstd[:, :tw], ALU.mult)
            fw.act(uT[:, ch, :tw], t_[:, :tw], AF.Copy, scale=g_sb[:, ch:ch + 1])
    return [stats, apply]


def emit_proj(fw, uT, w_bf, Dp, out_dram, t0, tw, ps_list, stg_list, ctr):
    for j in range(Dp // 128):
        ps = ps_list[ctr[0] % len(ps_list)]
        stg = stg_list[ctr[0] % len(stg_list)]
        for ch in range(8):
            fw.mm(ps[:, :tw], w_bf[:, ch, j * 128:(j + 1) * 128], uT[:, ch, :tw], start=(ch == 0), stop=(ch == 7))
        fw.copy("act" if ctr[0] % 2 == 0 else "dve", stg[:, :tw], ps[:, :tw])
        fw.dma("sp", out_dram[j * 128:(j + 1) * 128, t0:t0 + tw], stg[:, :tw], is_output=True)
        ctr[0] += 1


def build_P(Dp):
    fw = Fw()
    hT = fw.dram_in("hT", [D, NT])
    g = fw.dram_in("g", [128, 8])
    w = fw.dram_in("w", [D, Dp])
    o = fw.dram_out("o", [Dp, NT])
    c = consts_common(fw)
    xT = fw.sb("xT", [128, 8, NT], F32)
    g_sb = fw.sb("g_sb", [128, 8], F32)
    w_bf = fw.sb("w_bf", [128, 8, Dp], BF16)
    sq = [fw.sb(f"sq{i}", [128, TT], F32) for i in range(2)]
    rstd = fw.sb("rstd", [128, TT], F32)
    uT = [fw.sb(f"uT{i}", [128, 8, TT], BF16) for i in range(2)]
    ps_ss = fw.ps("ps_ss", [128, TT], F32)
    ps_list = [fw.ps(f"ps{i}", [128, TT], F32) for i in range(4)]
    stg_list = [fw.sb(f"stg{i}", [128, TT], F32) for i in range(4)]
    fw.dma("sp", g_sb[:], g)
    for ti in range(NT // TT):
        for ch in range(8):
            fw.dma("sp", xT[:, ch, ti * TT:(ti + 1) * TT], hT[ch * 128:(ch + 1) * 128, ti * TT:(ti + 1) * TT])
    for ch in range(8):
        fw.dma("pool", w_bf[:, ch, :], w[ch * 128:(ch + 1) * 128, :])
    ctr = [0]
    for ti in range(NT // TT):
        u = uT[ti % 2]
        emit_rmsnorm(fw, c, xT, g_sb, u, ti * TT, TT, sq, ps_ss, rstd)
        emit_proj(fw, u, w_bf, Dp, o, ti * TT, TT, ps_list, stg_list, ctr)
    return fw


HG = 8
HNG = 12
HCH = 96
TWO_PI = 6.283185307179586
MAGIC = 12582912.0


def hy_host_consts():
    n = np.arange(128)
    F = np.exp(-2j * np.pi * np.outer(n, n) / 128.0)
    Tw = np.exp(-2j * np.pi * np.outer(n, n) / 16384.0)
    f32 = lambda a: np.ascontiguousarray(a, dtype=np.float32)
    c = {}
    c["F1a"] = f32(np.concatenate([F.real, F.imag], 1)[:64])
    c["F1b"] = f32(np.concatenate([-F.imag, F.real], 1)[:64])
    c["F2re"] = f32(F.real)
    c["F2im"] = f32(F.imag)
    c["nF2im"] = f32(-F.imag)
    c["TT"] = f32(np.concatenate([Tw.real] * 4, 1))
    c["Tim"] = f32(Tw.imag)
    c["nTim"] = f32(-Tw.imag)
    c["Gc"] = f32(np.concatenate([F.real, -F.imag], 1))
    c["Gd"] = f32(np.concatenate([F.imag, F.real], 1))
    c["iF1re"] = f32(F.real[:, :64] / 16384.0)
    c["iF1im"] = f32(F.imag[:, :64] / 16384.0)
    c["niF1im"] = f32(-F.imag[:, :64] / 16384.0)
    return c


HY_CONST_SHAPES = {"F1a": [64, 256], "F1b": [64, 256], "F2re": [128, 128], "F2im": [128, 128], "nF2im": [128, 128],
                   "TT": [128, 512], "Tim": [128, 128], "nTim": [128, 128], "Gc": [128, 256], "Gd": [128, 256],
                   "iF1re": [128, 64], "iF1im": [128, 64], "niF1im": [128, 64]}


def build_HY():
    fw = Fw()
    G, NG, CH = HG, HNG, HCH
    W = 2 * G * 128
    U = fw.dram_in("U", [NG, 3, 3, 64, W])
    featsT = fw.dram_in("featsT", [33, 8192])
    w1 = fw.dram_in("w1", [33, 64])
    w2 = fw.dram_in("w2", [64, 64])
    w3 = fw.dram_in("w3", [NG, 64, 4 * G])
    b1 = fw.dram_in("b1", [64, 1])
    b2 = fw.dram_in("b2", [64, 1])
    fr = fw.dram_in("fr", [64, 1])
    decay = fw.dram_in("decay", [NG, 64, G * 128])
    sw = fw.dram_in("sw", [64, 9 * CH])
    skip = fw.dram_in("skip", [64, 2 * CH])
    o = fw.dram_out("o", [NG, 64, W])
    cd = {k: fw.dram_in("c_" + k, shp) for k, shp in HY_CONST_SHAPES.items()}
    c = consts_common(fw)
    T1 = [fw.sb(f"T1_{i}", [128, 512], F32) for i in range(4)]
    T2 = [fw.sb(f"T2_{i}", [128, 512], F32) for i in range(4)]
    K = {}
    BF_CONSTS = ("F2re", "F2im", "nF2im", "Gc", "Gd", "iF1re", "iF1im", "niF1im")
    for n_, (k, shp) in enumerate(HY_CONST_SHAPES.items()):
        if k in BF_CONSTS:
            stg = T1[n_ % 4] if n_ % 2 == 0 else T2[n_ % 4]
            sv = stg[0:shp[0], 0:shp[1]]
            fw.dma("sp", sv, cd[k])
            K[k] = fw.sb("k_" + k, shp, BF16)
            fw.copy("dve", K[k][:], sv)
        else:
            K[k] = fw.sb("k_" + k, shp, F32)
            fw.dma("sp", K[k][:], cd[k])
    w1s = fw.sb("w1s", [33, 64]); fw.dma("sp", w1s[:], w1)
    w2s = fw.sb("w2s", [64, 64]); fw.dma("sp", w2s[:], w2)
    b1s = fw.sb("b1s", [64, 1]); fw.dma("sp", b1s[:], b1)
    b2s = fw.sb("b2s", [64, 1]); fw.dma("sp", b2s[:], b2)
    frs = fw.sb("frs", [64, 1]); fw.dma("sp", frs[:], fr)
    sws = fw.sb("sws", [64, 9 * CH]); fw.dma("sp", sws[:], sw)
    sks = fw.sb("sks", [64, 2 * CH]); fw.dma("sp", sks[:], skip)
    frb1 = fw.sb("frb1", [64, 1]); fw.tt("dve", frb1[:], frs[:], b1s[:], ALU.mult)
    frb2 = fw.sb("frb2", [64, 1]); fw.tt("dve", frb2[:], frs[:], b2s[:], ALU.mult)
    h2T = fw.sb("h2T", [64, 8192], F32)
    scr = fw.sb("scr", [64, 4096], F32)
    ft = [scr[0:33, 0:512], scr[0:33, 512:1024]]
    zt = scr[:, 1024:1536]
    rt = scr[:, 1536:2048]
    h1t = scr[:, 2048:2560]
    psR = [fw.ps(f"psR{i}", [128, 512], F32) for i in range(7)]
    psM = fw.ps("psM", [128, 512], F32)
    ring = [0]

    def nps():
        r = psR[ring[0] % 7]
        ring[0] += 1
        return r

    def sin_layer(ps, frb, dst):
        fw.ts("dve", zt, ps, frs[:, 0:1], frb[:, 0:1], op0=ALU.mult, op1=ALU.add)
        fw.ts("dve", rt, zt, 1.0 / TWO_PI, MAGIC, op0=ALU.mult, op1=ALU.add)
        fw.ts("dve", rt, rt, MAGIC, TWO_PI, op0=ALU.subtract, op1=ALU.mult)
        fw.tt("dve", zt, zt, rt, ALU.subtract)
        fw.act(dst, zt, AF.Sin, scale=1.0 - 1e-6)

    for ti in range(16):
        f = ft[ti % 2]
        fw.dma("sp", f, featsT[:, ti * 512:(ti + 1) * 512])
        fw.mm(psM[0:64, :], w1s[:], f)
        sin_layer(psM[0:64, :], frb1, h1t)
        fw.mm(psM[0:64, :], w2s[:], h1t)
        sin_layer(psM[0:64, :], frb2, h2T[:, ti * 512:(ti + 1) * 512])

    w3s = fw.sb("w3s", [64, 4 * G], F32)
    dec = fw.sb("dec", [64, G, 128], F32)
    hf = fw.sb("hf", [64, 4 * G, 128], F32)
    habs_v = scr[:].rearrange("p (a n) -> p a n", a=4 * G)
    rs = fw.sb("rs", [64, 4 * G], F32)
    dsum = fw.sb("dsum", [128, 4 * G], F32)
    rden = fw.sb("rden", [128, 2 * G], F32)
    kk = fw.sb("kk", [128, 2, G, 512], F32)
    Xs = [fw.sb(f"Xs{i}", [128, 512], F32) for i in range(2)]
    sd = [fw.sb(f"sd{i}", [128, 256], F32) for i in range(2)]
    ush2 = fw.sb("ush2", [64, 2, G, 128], F32)
    Ushv = [scr[:, 0:2048].rearrange("p (b c n) -> p b c n", b=2, c=G),
            scr[:, 2048:4096].rearrange("p (b c n) -> p b c n", b=2, c=G), ush2[:]]
    uc = [fw.sb(f"uc{s}", [64, 2, G, 128], F32) for s in range(3)]
    OPb = [fw.sb(f"OP{i}", [128, 512], BF16) for i in range(6)]
    gtl = [fw.sb(f"gt{i}", [64, 2, 2, 128], F32) for i in range(4)]
    cnt = {"t": 0, "op": 0, "x": 0, "g": 0}

    def vw(ap, lay):
        if lay == "crk":
            return ap.rearrange("p (c r k) -> p c r k", c=2, r=2)
        return ap.rearrange("p (r c k) -> p c r k", c=2, r=2)

    def cmul(ps, lin, mulP1, mulRe, mulIm, lout):
        a, b = T1[cnt["t"] % 4], T2[cnt["t"] % 4]
        cnt["t"] += 1
        dst = OPb[cnt["op"] % 6]
        cnt["op"] += 1
        p4 = vw(ps, lin)
        fw.tt("dve", vw(a[:], lout), p4, mulP1, ALU.mult)
        fw.tt("dve", vw(b[:], lout)[:, :, 0, :], p4[:, :, 1, :], mulRe, ALU.mult)
        fw.tt("dve", vw(b[:], lout)[:, :, 1, :], p4[:, :, 0, :], mulIm, ALU.mult)
        fw.tt("pool", dst[:], a[:], b[:], ALU.add)
        return dst

    TT4 = K["TT"][:].rearrange("p (c r k) -> p c r k", c=2, r=2)
    Tim_b = K["Tim"][:].unsqueeze(1).broadcast_to([128, 2, 128])
    nTim_b = K["nTim"][:].unsqueeze(1).broadcast_to([128, 2, 128])

    def fwd_s2(a):
        px = nps()
        fw.mm(px[:, 0:256], K["F2re"][:], a[:, 0:256], start=True, stop=False)
        fw.mm(px[:, 0:256], K["nF2im"][:], a[:, 256:512], start=False, stop=True)
        fw.mm(px[:, 256:512], K["F2re"][:], a[:, 256:512], start=True, stop=False)
        fw.mm(px[:, 256:512], K["F2im"][:], a[:, 0:256], start=False, stop=True)
        return px

    for g in range(NG):
        c0 = g * G
        fw.dma("sp", w3s[:], w3[g])
        fw.dma("sp", dec[:], decay[g].rearrange("p (c n) -> p c n", c=G))
        for nb in range(8):
            pl3 = nps()
            for i in range(16):
                n2 = nb * 16 + i
                fw.mm(pl3[0:64, i * 32:(i + 1) * 32], h2T[:, n2 * 64:(n2 + 1) * 64], w3s[:])
            pin = pl3[0:64, :].rearrange("p (n a c) -> p n a c", n=16, a=4)
            dv = dec[:, :, nb * 16:(nb + 1) * 16].rearrange("p c n -> p n c").unsqueeze(2).broadcast_to([64, 16, 4, G])
            ov = hf[:, :, nb * 16:(nb + 1) * 16].rearrange("p (a c) n -> p n a c", a=4)
            fw.tt("dve", ov, pin, dv, ALU.mult)
        fw.act(habs_v, hf[:], AF.Abs)
        fw.reduce("dve", rs[:], habs_v, ALU.add)
        fw.mm(psM[:, 0:4 * G], c["ones"][0:64, :], rs[:])
        fw.copy("act", dsum[:], psM[:, 0:4 * G])
        d4 = dsum[:].rearrange("p (o d c) -> p o d c", o=2, d=2)
        r3 = rden[:].rearrange("p (o c) -> p o c", o=2)
        fw.tt("dve", r3, d4[:, :, 0, :], d4[:, :, 1, :], ALU.add)
        fw.ts("dve", rden[:], rden[:], 1e-6, None, op0=ALU.add)
        fw.op("dve", lambda e: e.reciprocal(rden[:], rden[:]), [rden[:]], [rden[:]])
        for s in range(3):
            for j in range(3):
                fw.dma("sp", Ushv[j], U[g, s, j].rearrange("p (b c n) -> p b c n", b=2, c=G))
            def wsc(j, cc):
                col = (s * 3 + j) * CH + c0 + cc
                return sws[:, col:col + 1]
            for cc in range(G):
                fw.act(uc[s][:, :, cc, :], Ushv[0][:, :, cc, :], AF.Copy, scale=wsc(0, cc))
            for j in (1, 2):
                for cc in range(G):
                    acc = uc[s][:, :, cc, :]
                    fw.stt("dve", acc, Ushv[j][:, :, cc, :], wsc(j, cc), acc, ALU.mult, ALU.add)
        ocs = [(oo, cc) for oo in range(2) for cc in range(G)]
        for q0 in range(0, len(ocs), 4):
            wave = ocs[q0:q0 + 4]
            pas = []
            for (oo, cc) in wave:
                pa = nps()
                for d in range(2):
                    col = (oo * 2 + d) * G + cc
                    fw.mm(pa[:, d * 256:(d + 1) * 256], hf[:, col, :], K["F1a"][:])
                pas.append(pa)
            aps = [cmul(pa[:], "crk", TT4, nTim_b, Tim_b, "rck") for pa in pas]
            pxs = [fwd_s2(a) for a in aps]
            for (oo, cc), px in zip(wave, pxs):
                xs_, sd_ = Xs[cnt["x"] % 2], sd[cnt["x"] % 2]
                cnt["x"] += 1
                fw.copy("act", xs_[:], px[:])
                fw.tt("pool", sd_[:, 0:128], xs_[:, 0:128], xs_[:, 128:256], ALU.add)
                fw.tt("pool", sd_[:, 128:256], xs_[:, 256:384], xs_[:, 384:512], ALU.subtract)
                rsc = rden[:, oo * G + cc:oo * G + cc + 1]
                kv = kk[:, oo, cc, :]
                fw.ts("dve", kv[:, 0:256].rearrange("p (r k) -> p r k", r=2),
                      sd_[:, 0:128].unsqueeze(1).broadcast_to([128, 2, 128]), rsc, None, op0=ALU.mult)
                fw.ts("dve", kv[:, 384:512], sd_[:, 128:256], rsc, None, op0=ALU.mult)
                fw.ts("dve", kv[:, 256:384], sd_[:, 128:256], rsc, -1.0, op0=ALU.mult, op1=ALU.mult)
        for oo in range(2):
            zin = uc[2]
            gate = uc[0] if oo == 0 else uc[1]
            zout = uc[2]
            prs = list(range(G // 2))
            pas = []
            for p in prs:
                pa = nps()
                for ci in range(2):
                    cc = 2 * p + ci
                    fw.mm(pa[:, ci * 256:(ci + 1) * 256], zin[:, 0, cc, :], K["F1a"][:], start=True, stop=False)
                    fw.mm(pa[:, ci * 256:(ci + 1) * 256], zin[:, 1, cc, :], K["F1b"][:], start=False, stop=True)
                pas.append(pa)
            aps = [cmul(pa[:], "crk", TT4, nTim_b, Tim_b, "rck") for pa in pas]
            pxs = [fwd_s2(a) for a in aps]
            yps = []
            for p, px in zip(prs, pxs):
                kp = kk[:, oo, 2 * p:2 * p + 2, :]
                yps.append(cmul(px[:], "rck", kp[:, :, 0:256].rearrange("p c (r k) -> p c r k", r=2),
                                kp[:, :, 256:384], kp[:, :, 384:512], "crk"))
            pbs = []
            for yp in yps:
                pb = nps()
                y4 = vw(yp[:], "crk")
                for ci in range(2):
                    fw.mm(pb[:, ci * 256:(ci + 1) * 256], y4[:, ci, 0, :], K["Gc"][:], start=True, stop=False)
                    fw.mm(pb[:, ci * 256:(ci + 1) * 256], y4[:, ci, 1, :], K["Gd"][:], start=False, stop=True)
                pbs.append(pb)
            bps = [cmul(pb[:], "crk", TT4, Tim_b, nTim_b, "rck") for pb in pbs]
            pys = []
            for bp in bps:
                py = nps()
                yv = py[0:64, :].rearrange("p (b c n) -> p b c n", b=2, c=2)
                fw.mm(yv[:, 0, :, :], K["iF1re"][:], bp[:, 0:256], start=True, stop=False)
                fw.mm(yv[:, 0, :, :], K["iF1im"][:], bp[:, 256:512], start=False, stop=True)
                fw.mm(yv[:, 1, :, :], K["iF1re"][:], bp[:, 256:512], start=True, stop=False)
                fw.mm(yv[:, 1, :, :], K["niF1im"][:], bp[:, 0:256], start=False, stop=True)
                pys.append(py)
            for p, py in zip(prs, pys):
                yv = py[0:64, :].rearrange("p (b c n) -> p b c n", b=2, c=2)
                gt = gtl[cnt["g"] % 4]
                cnt["g"] += 1
                base = oo * CH + c0 + 2 * p
                for ci in range(2):
                    fw.act(gt[:, :, ci, :], zin[:, :, 2 * p + ci, :], AF.Copy, scale=sks[:, base + ci:base + ci + 1])
                fw.tt("dve", gt[:], yv, gt[:], ALU.add)
                fw.tt("pool", zout[:, :, 2 * p:2 * p + 2, :], gate[:, :, 2 * p:2 * p + 2, :], gt[:], ALU.mult)
        fw.dma("sp", o[g].rearrange("p (b c n) -> p b c n", b=2, c=G), uc[2][:], is_output=True)
    return fw


def hy_host_inputs(proj, hy_w, core):
    G, NG, CH = HG, HNG, HCH
    ch0 = core * CH
    Umat = np.empty((NG, 3, 3, 64, 2 * G * 128), np.float32)
    for s in range(3):
        a = proj[:, :, s * 768 + ch0: s * 768 + ch0 + CH]
        ap = np.pad(a, ((0, 0), (1, 1), (0, 0)))
        for j in range(3):
            sh = ap[:, j:j + T, :]
            x = sh.transpose(0, 2, 1).reshape(B, NG, G, 64, 128)
            Umat[:, s, j] = x.transpose(1, 3, 0, 2, 4).reshape(NG, 64, 2 * G * 128)
    d = {"U": Umat}
    d.update(hy_w[core])
    return d


def hy_host_weights(short_w, w1, b1, w2, b2, w3, freq, skip):
    G, NG, CH = HG, HNG, HCH
    L = T
    m = np.arange(L, dtype=np.float64)
    tt_ = m / (L - 1)
    wv = 2.0 * np.pi * m / L
    bands = np.linspace(1e-4, 15.0, 16)
    feats = np.concatenate([tt_[:, None], np.cos(bands[None] * wv[:, None]), -np.sin(bands[None] * wv[:, None])], 1)
    perm = (np.arange(64)[None, :] * 128 + np.arange(128)[:, None]).reshape(-1)
    featsT = np.ascontiguousarray(feats[perm].T, dtype=np.float32)
    max_decay = np.log(1e-2) / 0.3
    min_decay = np.log(1e-2) / 1.5
    deltas = np.abs(np.linspace(min_decay, max_decay, 768))
    dec_full = np.exp(-tt_[:, None] * deltas[None, :])
    consts = hy_host_consts()
    outs = []
    w3r = w3.reshape(64, 2, 2, 768)
    for core in range(NCORES):
        ch0 = core * CH
        d = {"featsT": featsT, "w1": np.ascontiguousarray(w1), "w2": np.ascontiguousarray(w2),
             "b1": np.ascontiguousarray(b1.reshape(64, 1)), "b2": np.ascontiguousarray(b2.reshape(64, 1)),
             "fr": np.ascontiguousarray(freq.reshape(64, 1))}
        w3c = w3r[:, :, :, ch0:ch0 + CH].reshape(64, 2, 2, NG, G)
        d["w3"] = np.ascontiguousarray(w3c.transpose(3, 0, 1, 2, 4).reshape(NG, 64, 4 * G))
        dc = dec_full[:, ch0:ch0 + CH].reshape(64, 128, NG, G)
        d["decay"] = np.ascontiguousarray(dc.transpose(2, 0, 3, 1).reshape(NG, 64, G * 128), dtype=np.float32)
        swc = short_w.reshape(3, 3, 768)[:, :, ch0:ch0 + CH]
        swl = swc.transpose(1, 0, 2).reshape(1, 9 * CH)
        d["sw"] = np.ascontiguousarray(np.broadcast_to(swl, (64, 9 * CH)))
        skl = skip[:, ch0:ch0 + CH].reshape(1, 2 * CH)
        d["skip"] = np.ascontiguousarray(np.broadcast_to(skl, (64, 2 * CH)))
        for k, v in consts.items():
            d["c_" + k] = v
        outs.append(d)
    return outs


def hy_host_gather(results):
    G, NG, CH = HG, HNG, HCH
    main = np.empty((B, T, 768), np.float32)
    for core, r in enumerate(results):
        x = r.reshape(NG, 64, B, G, 128)
        x = x.transpose(2, 1, 4, 0, 3).reshape(B, T, CH)
        main[:, :, core * CH:(core + 1) * CH] = x
    return main


def at_host_consts():
    rows = T // 64
    pos_row = np.repeat(np.arange(rows), 64).astype(np.float64)
    pos_col = np.tile(np.arange(64), rows).astype(np.float64)
    inv = 1.0 / (10000.0 ** (np.arange(0, 32, 2, dtype=np.float64) / 32))
    ang = np.stack([pos_row[:, None] * inv, pos_col[:, None] * inv], 1)
    d = np.arange(64)
    axis = d // 32
    f = d % 16
    cosT = np.cos(ang[:, axis, f]).T
    sinT = np.sin(ang[:, axis, f]).T
    R = np.zeros((64, 64))
    for a in range(2):
        for ff in range(16):
            i0 = a * 32 + ff
            i1 = a * 32 + 16 + ff
            R[i0, i1] = -1.0
            R[i1, i0] = 1.0
    f32 = lambda a: np.ascontiguousarray(a, dtype=np.float32)
    return {"cosT": f32(cosT), "sinT": f32(sinT), "rotT": f32(R.T)}


def build_AT():
    fw = Fw()
    qT = fw.dram_in("qT", [3, 64, T])
    kT = fw.dram_in("kT", [64, T])
    v = fw.dram_in("v", [T, 64])
    gq = fw.dram_in("gq", [64, 1])
    gk = fw.dram_in("gk", [64, 1])
    cosT = fw.dram_in("cosT", [64, T])
    sinT = fw.dram_in("sinT", [64, T])
    rotT = fw.dram_in("rotT", [64, 64])
    o = fw.dram_out("o", [3, 64, T])
    c = consts_common(fw)
    cs = fw.sb("cs", [64, T], F32); fw.dma("sp", cs[:], cosT)
    sn = fw.sb("sn", [64, T], F32); fw.dma("sp", sn[:], sinT)
    rot = fw.sb("rot", [64, 64], F32); fw.dma("sp", rot[:], rotT)
    gqs = fw.sb("gqs", [64, 1], F32); fw.dma("sp", gqs[:], gq)
    gks = fw.sb("gks", [64, 1], F32); fw.dma("sp", gks[:], gk)
    qb = [fw.sb(f"qb{h}", [128, T], BF16) for h in range(3)]
    kb = fw.sb("kb", [128, T], BF16)
    for t_ in qb + [kb]:
        fw.memset("pool", t_[64:128, :], 0.0)
    vst = fw.sb("vst", [128, 64, 64], F32)
    va = fw.sb("va", [128, 64, 128], BF16)
    fw.dma("sp", vst[:], v.rearrange("(c p) d -> p c d", p=128))
    fw.memset("pool", va[:, :, 64:128], 0.0)
    fw.copy("dve", va[:, :, 0:64], vst[:])
    fw.memset("dve", va[:, :, 64:65], 1.0)
    xin = [fw.sb(f"xin{i}", [64, TT], F32) for i in range(2)]
    sq = fw.sb("sqa", [64, TT], F32)
    rstd = fw.sb("rstda", [64, TT], F32)
    xn = fw.sb("xna", [64, TT], F32)
    ra = fw.sb("ra", [64, TT], F32)
    rb = fw.sb("rb", [64, TT], F32)
    psA = fw.ps("psA", [128, TT], F32)
    psB = fw.ps("psB", [128, TT], F32)
    psS = [fw.ps(f"psS{i}", [128, TT], F32) for i in range(3)]
    psO = [fw.ps(f"psO{i}", [128, TT], F32) for i in range(2)]
    cnt = 0
    for src, g, dst in [(kT, gks, kb)] + [(qT[h], gqs, qb[h]) for h in range(3)]:
        for ti in range(T // TT):
            x = xin[cnt % 2]
            cnt += 1
            sl = slice(ti * TT, (ti + 1) * TT)
            fw.dma("sp", x[:], src[:, sl])
            fw.act(sq[:], x[:], AF.Square)
            fw.mm(psA[0:64, :], c["ones"][0:64, 0:64], sq[:])
            fw.act(rstd[:], psA[0:64, :], AF.Sqrt, bias=c["eps"][0:64, :], scale=1.0 / 64)
            fw.op("dve", lambda e: e.reciprocal(rstd[:], rstd[:]), [rstd[:]], [rstd[:]])
            fw.stt("dve", xn[:], x[:], g[:, 0:1], rstd[:], ALU.mult, ALU.mult)
            fw.mm(psB[0:64, :], rot[:], xn[:])
            fw.tt("dve", rb[:], psB[0:64, :], sn[:, sl], ALU.mult)
            fw.tt("pool", ra[:], xn[:], cs[:, sl], ALU.mult)
            fw.tt("pool", dst[0:64, sl], ra[:], rb[:], ALU.add)
    pt = [fw.sb(f"pt{i}", [128, TT], BF16) for i in range(3)]
    lsb = fw.sb("lsb", [128, TT], F32)
    rec = fw.sb("rec", [64, TT], F32)
    ost = [fw.sb(f"ost{i}", [64, TT], F32) for i in range(2)]
    it = 0
    blk = 0
    for h in range(3):
        for qi in range(T // TT):
            qs = slice(qi * TT, (qi + 1) * TT)
            po = psO[blk % 2]
            for kc in range(2):
                fw.mm(psS[(it + kc) % 3][:], kb[:, kc * 128:(kc + 1) * 128], qb[h][:, qs])
            for kc in range(64):
                ps = psS[it % 3]
                p = pt[it % 3]
                fw.act(p[:], ps[:], AF.Exp, scale=0.125)
                if kc + 2 < 64:
                    fw.mm(psS[(it + 2) % 3][:], kb[:, (kc + 2) * 128:(kc + 3) * 128], qb[h][:, qs])
                fw.mm(po[:], va[:, kc, :], p[:], start=(kc == 0), stop=(kc == 63))
                it += 1
            fw.copy("act", lsb[64:65, :], po[64:65, :])
            fw.mm(psA[0:64, :], c["ones"][64:65, 0:64], lsb[64:65, :])
            fw.op("dve", lambda e: e.reciprocal(rec[:], psA[0:64, :]), [psA[0:64, :]], [rec[:]])
            os_ = ost[blk % 2]
            fw.tt("dve", os_[:], po[0:64, :], rec[:], ALU.mult)
            fw.dma("sp", o[h, :, qs], os_[:], is_output=True)
            blk += 1
    return fw


def build_O():
    fw = Fw()
    hT = fw.dram_in("hT", [D, NT])
    mainT = fw.dram_in("mainT", [768, NT])
    cqT = fw.dram_in("cqT", [4, 64, NT])
    memT = fw.dram_in("memT", [D, 256])
    g_mem = fw.dram_in("g_mem", [128, 8])
    w_kv = fw.dram_in("w_kv", [D, 512])
    w_out = fw.dram_in("w_out", [D, D])
    g_ffn = fw.dram_in("g_ffn", [128, 8])
    w_r = fw.dram_in("w_r", [D, 16])
    ident = fw.dram_in("ident", [128, 128])
    ho = fw.dram_out("ho", [D, NT])
    affo = fw.dram_out("aff", [NT, 16])
    c = consts_common(fw)
    xT = fw.sb("xT", [128, 8, NT], F32)
    gm = fw.sb("gm", [128, 8], F32); fw.dma("sp", gm[:], g_mem)
    gf = fw.sb("gf", [128, 8], F32); fw.dma("sp", gf[:], g_ffn)
    wr = fw.sb("wr", [128, 8, 16], F32); fw.dma("sp", wr[:], w_r.rearrange("(c p) e -> p c e", p=128))
    idf = fw.sb("idf", [128, 128], F32); fw.dma("sp", idf[:], ident)
    idb = fw.sb("idb", [128, 128], BF16); fw.copy("dve", idb[:], idf[:])
    mT = fw.sb("mT", [128, 8, 256], F32); fw.dma("sp", mT[:], memT.rearrange("(c p) m -> p c m", p=128))
    wkv = fw.sb("wkv", [128, 8, 512], BF16); fw.dma("pool", wkv[:], w_kv.rearrange("(c p) f -> p c f", p=128))
    main_bf = fw.sb("main_bf", [128, 6, NT], BF16); fw.dma("pool", main_bf[:], mainT.rearrange("(c p) t -> p c t", p=128))
    cq_bf = fw.sb("cq_bf", [64, 4, NT], BF16); fw.dma("pool", cq_bf[:], cqT.rearrange("h p t -> p h t"))
    wo_bf = fw.sb("wo_bf", [128, 6, D], BF16); fw.dma("pool", wo_bf[:], w_out[0:768, :].rearrange("(c p) d -> p c d", p=128))
    woc_bf = fw.sb("woc_bf", [64, 4, D], BF16); fw.dma("pool", woc_bf[:], w_out[768:1024, :].rearrange("(h p) d -> p h d", p=64))
    cross_bf = fw.sb("cross_bf", [64, 4, NT], BF16)
    for ti in range(NT // TT):
        for ch in range(8):
            fw.dma("sp", xT[:, ch, ti * TT:(ti + 1) * TT], hT[ch * 128:(ch + 1) * 128, ti * TT:(ti + 1) * TT])
    sq = [fw.sb(f"sq{i}", [128, TT], F32) for i in range(2)]
    rstd = fw.sb("rstd", [128, TT], F32)
    ps_ss = fw.ps("ps_ss", [128, TT], F32)
    psS = [fw.ps(f"psS{i}", [128, TT], F32) for i in range(2)]
    psT = [fw.ps(f"psT{i}", [128, 128], BF16) for i in range(2)]
    psO = fw.ps("psO", [128, TT], F32)
    psP = [fw.ps(f"psP{i}", [128, TT], F32) for i in range(2)]
    memn = fw.sb("memn", [128, 8, 256], BF16)
    emit_rmsnorm(fw, c, mT, gm, memn, 0, 256, sq, ps_ss, rstd)
    mkT = fw.sb("mkT", [64, 4, 256], BF16)
    mv = fw.sb("mv", [128, 2, 256], BF16)
    for h in range(4):
        for ch in range(8):
            fw.mm(psS[0][0:64, 0:256], wkv[:, ch, h * 64:(h + 1) * 64], memn[:, ch, :], start=(ch == 0), stop=(ch == 7))
        fw.copy("act", mkT[:, h, :], psS[0][0:64, 0:256])
    for mc in range(2):
        for ch in range(8):
            fw.mm(psS[1][:, 0:256], memn[:, ch, mc * 128:(mc + 1) * 128], wkv[:, ch, 256:512], start=(ch == 0), stop=(ch == 7))
        fw.copy("act", mv[:, mc, :], psS[1][:, 0:256])
    pe_ = [fw.sb(f"pe{i}", [128, 256], F32) for i in range(2)]
    pn = [fw.sb(f"pn{i}", [128, 256], BF16) for i in range(2)]
    pT = [fw.sb(f"pT{i}", [128, 128], BF16) for i in range(4)]
    st = [fw.sb(f"st{i}", [128, 4], F32) for i in range(2)]
    it = 0
    tcnt = 0
    for tile in range(NT // TT):
        for h in range(4):
            for qp in range(0, 4, 2):
                units = [(qp + u, u) for u in range(2)]
                for qb_, i in units:
                    q0 = tile * TT + qb_ * 128
                    fw.mm(psS[i][:, 0:256], cq_bf[:, h, q0:q0 + 128], mkT[:, h, :])
                for qb_, i in units:
                    fw.reduce("dve", st[i][:, 0:1], psS[i][:, 0:256], ALU.max)
                for qb_, i in units:
                    fw.ts("dve", st[i][:, 1:2], st[i][:, 0:1], -0.125, None, op0=ALU.mult)
                for qb_, i in units:
                    fw.act(pe_[i][:], psS[i][:, 0:256], AF.Exp, bias=st[i][:, 1:2], scale=0.125)
                for qb_, i in units:
                    fw.reduce("dve", st[i][:, 2:3], pe_[i][:], ALU.add)
                for qb_, i in units:
                    s_ = st[i]
                    fw.op("dve", lambda e, s_=s_: e.reciprocal(s_[:, 3:4], s_[:, 2:3]), [s_[:, 2:3]], [s_[:, 3:4]])
                for qb_, i in units:
                    fw.ts("dve", pn[i][:], pe_[i][:], st[i][:, 3:4], None, op0=ALU.mult)
                for mc in range(2):
                    for qb_, i in units:
                        fw.transpose(psT[i][:], pn[i][:, mc * 128:(mc + 1) * 128], idb[:])
                    for qb_, i in units:
                        fw.copy("act", pT[2 * mc + i][:], psT[i][:])
                for qb_, i in units:
                    for mc in range(2):
                        fw.mm(psO[0:64, qb_ * 128:(qb_ + 1) * 128], mv[:, mc, h * 64:(h + 1) * 64], pT[2 * mc + i][:], start=(mc == 0), stop=(mc == 1))
                it += 2
            fw.copy("act", cross_bf[:, h, tile * TT:(tile + 1) * TT], psO[0:64, :])
    xn = fw.sb("xn", [128, 8, TT], F32)
    lg = fw.sb("lg", [128, 16], F32)
    affs = fw.sb("affs", [128, 16, 16], F32)
    pc = 0
    for tile in range(NT // TT):
        ts_ = slice(tile * TT, (tile + 1) * TT)
        for j in range(8):
            ps = psP[pc % 2]
            pc += 1
            js = slice(j * 128, (j + 1) * 128)
            for cc in range(6):
                fw.mm(ps[:], wo_bf[:, cc, js], main_bf[:, cc, ts_], start=(cc == 0), stop=False)
            for h in range(4):
                fw.mm(ps[:], woc_bf[:, h, js], cross_bf[:, h, ts_], start=False, stop=(h == 3))
            fw.tt("dve", xT[:, j, ts_], xT[:, j, ts_], ps[:], ALU.add)
            fw.dma("sp", ho[js, ts_], xT[:, j, ts_], is_output=True)
        emit_rmsnorm(fw, c, xT, gf, xn, tile * TT, TT, sq, ps_ss, rstd)
        for b_ in range(4):
            blk = tile * 4 + b_
            s_ = st[it % 2]
            it += 1
            pl = psO[:, 0:16]
            for ch in range(8):
                fw.mm(pl, xn[:, ch, b_:TT:4], wr[:, ch, :], start=(ch == 0), stop=(ch == 7))
            fw.reduce("dve", s_[:, 0:1], pl, ALU.max)
            fw.ts("dve", s_[:, 1:2], s_[:, 0:1], -1.0, None, op0=ALU.mult)
            fw.act(lg[:], pl, AF.Exp, bias=s_[:, 1:2], scale=1.0)
            fw.reduce("dve", s_[:, 2:3], lg[:], ALU.add)
            fw.op("dve", lambda e, s_=s_: e.reciprocal(s_[:, 3:4], s_[:, 2:3]), [s_[:, 2:3]], [s_[:, 3:4]])
            fw.ts("dve", affs[:, blk, :], lg[:], s_[:, 3:4], None, op0=ALU.mult)
        fw.dma("sp", affo[tile * TT:(tile + 1) * TT, :].rearrange("(p b) e -> p b e", b=4),
               affs[:, tile * 4:(tile + 1) * 4, :], is_output=True)
    return fw


N_BISECT = 34
CAP = 1024


def m_host_consts():
    p = np.arange(128)
    Gm = (p[:, None] // 8 == p[None, :] // 8).astype(np.float32)
    G16 = (p[:, None] // 8 == np.arange(16)[None, :]).astype(np.float32)
    selm = np.zeros((16, 16, 128), np.float32)
    for e in range(16):
        selm[e, e, :] = 1.0
    return {"Gm": Gm, "G16": G16, "selm": selm.reshape(16, 2048)}


def build_M():
    fw = Fw()
    hT = fw.dram_in("hT", [D, NT])
    g_ffn = fw.dram_in("g_ffn", [128, 8])
    affP = fw.dram_in("affP", [128, 1024])
    affT = fw.dram_in("affT", [16, NT])
    Gm_d = fw.dram_in("Gm", [128, 128])
    G16_d = fw.dram_in("G16", [128, 16])
    selm_d = fw.dram_in("selm", [16, 2048])
    wg = fw.dram_in("wg", [16, D, 768])
    wu = fw.dram_in("wu", [16, D, 768])
    wd = fw.dram_in("wd", [16, 768, D])
    ho = fw.dram_out("ho", [D, NT])
    c = consts_common(fw)
    xT = fw.sb("xT", [128, 8, NT], F32)
    gf = fw.sb("gf", [128, 8], F32); fw.dma("sp", gf[:], g_ffn)
    aP = fw.sb("aP", [128, 1024], F32); fw.dma("sp", aP[:], affP)
    wT = fw.sb("wT", [16, NT], F32); fw.dma("sp", wT[:], affT)
    Gm = fw.sb("Gm_s", [128, 128], F32); fw.dma("sp", Gm[:], Gm_d)
    selm = fw.sb("selm_s", [16, 2048], F32); fw.dma("sp", selm[:], selm_d)
    for ti in range(NT // TT):
        for ch in range(8):
            fw.dma("sp", xT[:, ch, ti * TT:(ti + 1) * TT], hT[ch * 128:(ch + 1) * 128, ti * TT:(ti + 1) * TT])
    wgb = [fw.sb(f"wgb{i}", [128, 8, 384], BF16) for i in range(2)]
    wub = [fw.sb(f"wub{i}", [128, 8, 384], BF16) for i in range(2)]
    wdb = [fw.sb(f"wdb{i}", [128, 3, D], BF16) for i in range(2)]

    def load_w(e, half, i):
        fs = slice(half * 384, (half + 1) * 384)
        fw.dma("pool", wgb[i][:], wg[e][:, fs].rearrange("(c p) f -> p c f", p=128))
        fw.dma("pool", wub[i][:], wu[e][:, fs].rearrange("(c p) f -> p c f", p=128))
        fw.dma("pool", wdb[i][:], wd[e][fs, :].rearrange("(c p) d -> p c d", p=128))

    load_w(0, 0, 0)
    load_w(0, 1, 1)
    sq = [fw.sb(f"sq{i}", [128, TT], F32) for i in range(2)]
    rstd = fw.sb("rstd", [128, TT], F32)
    ps_ss = fw.ps("ps_ss", [128, TT], F32)
    psG = [fw.ps(f"psG{i}", [128, TT], F32) for i in range(2)]
    psU = [fw.ps(f"psU{i}", [128, TT], F32) for i in range(2)]
    psY = [fw.ps(f"psY{i}", [128, TT], F32) for i in range(2)]
    psW = fw.ps("psW", [128, TT], F32)
    cmp_ = fw.sb("cmp", [128, 1024], F32)
    cn = fw.sb("cn", [128, 1], F32)
    bs = fw.sb("bs128", [128, 4], F32)
    lo, mid, ge = (bs[:, k:k + 1] for k in range(3))
    w = 0.75
    fw.memset("dve", lo, 0.0)
    fw.memset("dve", mid, w)
    xn = fw.sb("xn", [128, 8, NT], BF16)
    sg = [fw.sb(f"sg{i}", [128, TT], F32) for i in range(2)]
    pieces = []
    for tile in range(NT // TT):
        pieces += rmsnorm_nodve_pieces(fw, c, xT, gf, xn[:, :, tile * TT:(tile + 1) * TT], tile * TT, TT, sq, ps_ss, rstd, sg)
    for itn in range(N_BISECT):
        if itn % 4 == 1 and pieces:
            pieces.pop(0)()
        fw.ts("dve", cmp_[:], aP[:], mid, None, op0=ALU.is_ge)
        fw.reduce("dve", cn[:], cmp_[:], ALU.add)
        fw.mm(psW[:, 0:1], Gm[:], cn[:])
        fw.ts("dve", ge, psW[:, 0:1], float(CAP) - 0.5, None, op0=ALU.is_ge)
        fw.stt("dve", lo, ge, w, lo, ALU.mult, ALU.add)
        w = w * 0.5
        fw.ts("dve", mid, lo, w, None, op0=ALU.add)
    thr_d = fw.dram_tmp("thr_d", [128, 1])
    thr16 = fw.sb("thr16", [16, 1], F32)
    fw.dma("sp", thr_d, lo)
    fw.dma("sp", thr16[:], thr_d.rearrange("(e s) o -> e (s o)", s=8)[:, 0:1], allow_slow_non_contiguous=True)
    fw.stt("dve", wT[:], wT[:], thr16[:, 0:1], wT[:], ALU.is_ge, ALU.mult)
    while pieces:
        pieces.pop(0)()
    wbc = [fw.sb(f"wbc{i}", [128, TT], F32) for i in range(2)]
    hid = [fw.sb(f"hid{i}", [128, 3, TT], BF16) for i in range(2)]
    gi = [0]
    yi = [0]
    steps = [(k, tile) for k in range(32) for tile in range(NT // TT)]

    def gateup(sidx):
        k, tile = steps[sidx]
        e, i = k // 2, k % 2
        ts_ = slice(tile * TT, (tile + 1) * TT)
        wb = wbc[sidx % 2]
        fw.mm(psW[:], selm[:, e * 128:(e + 1) * 128], wT[:, ts_])
        fw.copy("act", wb[:], psW[:])
        hd = hid[sidx % 2]
        for f in range(3):
            pg = psG[gi[0] % 2]
            pu = psU[gi[0] % 2]
            s_ = sg[gi[0] % 2]
            gi[0] += 1
            fs = slice(f * 128, (f + 1) * 128)
            for ch in range(8):
                fw.mm(pg[:], wgb[i][:, ch, fs], xn[:, ch, ts_], start=(ch == 0), stop=(ch == 7))
            for ch in range(8):
                fw.mm(pu[:], wub[i][:, ch, fs], xn[:, ch, ts_], start=(ch == 0), stop=(ch == 7))
            fw.act(s_[:], pg[:], AF.Silu)
            fw.tt("dve", s_[:], s_[:], wb[:], ALU.mult)
            fw.tt("dve", hd[:, f, :], s_[:], pu[:], ALU.mult)

    def down(sidx):
        k, tile = steps[sidx]
        i = k % 2
        ts_ = slice(tile * TT, (tile + 1) * TT)
        hd = hid[sidx % 2]
        for j in range(8):
            py = psY[yi[0] % 2]
            yi[0] += 1
            for f in range(3):
                fw.mm(py[:], wdb[i][:, f, j * 128:(j + 1) * 128], hd[:, f, :], start=(f == 0), stop=(f == 2))
            fw.tt("dve", xT[:, j, ts_], xT[:, j, ts_], py[:], ALU.add)
        if tile == NT // TT - 1 and k + 2 < 32:
            load_w((k + 2) // 2, (k + 2) % 2, (k + 2) % 2)

    for sidx in range(len(steps)):
        gateup(sidx)
        if sidx > 0:
            down(sidx - 1)
    down(len(steps) - 1)
    for ch in range(8):
        fw.dma("sp", ho[ch * 128:(ch + 1) * 128, :], xT[:, ch, :], is_output=True)
    return fw


def build_F():
    fw = Fw()
    hT = fw.dram_in("hT", [D, NT])
    g = fw.dram_in("g", [128, 8])
    o = fw.dram_out("o", [D, NT])
    c = consts_common(fw)
    xT = fw.sb("xT", [128, 8, NT], F32)
    for ch in range(8):
        fw.dma("sp", xT[:, ch, :], hT[ch * 128:(ch + 1) * 128, :])
    gs = fw.sb("gs", [128, 8], F32); fw.dma("sp", gs[:], g)
    sq = [fw.sb(f"sq{i}", [128, TT], F32) for i in range(2)]
    rstd = fw.sb("rstd", [128, TT], F32)
    ps_ss = fw.ps("ps_ss", [128, TT], F32)
    un = [fw.sb(f"un{i}", [128, 8, TT], F32) for i in range(2)]
    for tile in range(NT // TT):
        u = un[tile % 2]
        emit_rmsnorm(fw, c, xT, gs, u, tile * TT, TT, sq, ps_ss, rstd)
        for ch in range(8):
            fw.dma("sp", o[ch * 128:(ch + 1) * 128, tile * TT:(tile + 1) * TT], u[:, ch, :], is_output=True)
    return fw


def _lay(g):
    return np.ascontiguousarray(np.asarray(g, np.float32).reshape(8, 128).T)


def _run(fw, in_maps):
    nc = fw.finish()
    res = run_bass_kernel_spmd(nc, in_maps, core_ids=list(range(NCORES)))
    return res.results


def kernel(x, mem, mix_norm_g, ffn_norm_g, mem_norm_g, final_norm_g, w_mem_kv, w_out,
           hy_w_in, hy_short_w, hy_filt_w1, hy_filt_b1, hy_filt_w2, hy_filt_b2, hy_filt_w3,
           hy_filt_freq, hy_skip, at_w_in, at_q_norm_g, at_k_norm_g,
           router_w, exp_w_gate, exp_w_up, exp_w_down):
    f32 = lambda a: np.ascontiguousarray(np.asarray(a), dtype=np.float32)
    x = f32(x); mem = f32(mem)
    h = x.reshape(B * T, D)
    hT = [np.ascontiguousarray(h[c * NT:(c + 1) * NT].T) for c in range(NCORES)]
    memT = [np.ascontiguousarray(mem[b].T) for b in range(B)]
    ident = np.eye(128, dtype=np.float32)
    mconst = m_host_consts()
    aconst = at_host_consts()
    for i in range(4):
        j = i // 2
        hyena = (i % 2 == 0)
        w_in = f32(hy_w_in[j]) if hyena else f32(at_w_in[j])
        Dp = w_in.shape[1]
        g_mix = _lay(mix_norm_g[i])
        res = _run(build_P(Dp), [{"hT": hT[c], "g": g_mix, "w": w_in} for c in range(NCORES)])
        proj = np.concatenate([r["o"].T for r in res], 0).reshape(B, T, Dp)
        if hyena:
            hw = hy_host_weights(f32(hy_short_w[j]), f32(hy_filt_w1[j]), f32(hy_filt_b1[j]), f32(hy_filt_w2[j]),
                                 f32(hy_filt_b2[j]), f32(hy_filt_w3[j]), f32(hy_filt_freq[j]), f32(hy_skip[j]))
            res = _run(build_HY(), [hy_host_inputs(proj, hw, c) for c in range(NCORES)])
            main = hy_host_gather([r["o"] for r in res])
        else:
            gq = f32(at_q_norm_g[j]).reshape(64, 1)
            gk = f32(at_k_norm_g[j]).reshape(64, 1)
            ims = []
            for c in range(NCORES):
                b, g = c // 4, c % 4
                q = proj[b, :, g * 192:(g + 1) * 192].reshape(T, 3, 64)
                d = {"qT": np.ascontiguousarray(q.transpose(1, 2, 0)),
                     "kT": np.ascontiguousarray(proj[b, :, 768 + g * 64:768 + (g + 1) * 64].T),
                     "v": np.ascontiguousarray(proj[b, :, 1024 + g * 64:1024 + (g + 1) * 64]),
                     "gq": gq, "gk": gk}
                d.update(aconst)
                ims.append(d)
            res = _run(build_AT(), ims)
            main = np.empty((B, T, 768), np.float32)
            for c in range(NCORES):
                b, g = c // 4, c % 4
                main[b, :, g * 192:(g + 1) * 192] = res[c]["o"].transpose(2, 0, 1).reshape(T, 192)
        mainf = main.reshape(B * T, 768)
        cqf = proj[:, :, Dp - 256:].reshape(B * T, 256)
        ims = []
        for c in range(NCORES):
            sl = slice(c * NT, (c + 1) * NT)
            ims.append({"hT": hT[c], "mainT": np.ascontiguousarray(mainf[sl].T),
                        "cqT": np.ascontiguousarray(cqf[sl].T.reshape(4, 64, NT)), "memT": memT[c // 4],
                        "g_mem": _lay(mem_norm_g), "w_kv": f32(w_mem_kv[i]), "w_out": f32(w_out[i]),
                        "g_ffn": _lay(ffn_norm_g[i]), "w_r": f32(router_w[i]), "ident": ident})
        res = _run(build_O(), ims)
        hT = [r["ho"] for r in res]
        aff = np.concatenate([r["aff"] for r in res], 0)
        wg_, wu_, wd_ = f32(exp_w_gate[i]), f32(exp_w_up[i]), f32(exp_w_down[i])
        ims = []
        for c in range(NCORES):
            b = c // 4
            ab = aff[b * T:(b + 1) * T]
            d = {"hT": hT[c], "g_ffn": _lay(ffn_norm_g[i]),
                 "affP": np.ascontiguousarray(ab.T.reshape(128, 1024)),
                 "affT": np.ascontiguousarray(aff[c * NT:(c + 1) * NT].T),
                 "wg": wg_, "wu": wu_, "wd": wd_}
            d.update(mconst)
            ims.append(d)
        res = _run(build_M(), ims)
        hT = [r["ho"] for r in res]
    res = _run(build_F(), [{"hT": hT[c], "g": _lay(final_norm_g)} for c in range(NCORES)])
    out = np.concatenate([r["o"].T for r in res], 0).reshape(B, T, D)
    return np.ascontiguousarray(out, dtype=np.float32)
```

```python
from contextlib import ExitStack
import numpy as np
import concourse.bass as bass
import concourse.mybir as mybir
from concourse.bass_utils import run_bass_kernel_spmd

F32 = mybir.dt.float32
BF16 = mybir.dt.bfloat16
I32 = mybir.dt.int32
ALU = mybir.AluOpType
AF = mybir.ActivationFunctionType
AX = mybir.AxisListType

SEM_LIMIT = 16000
N_DMA_SLOTS = 6


def _box(ap):
    t = ap.tensor
    name = t.name
    off = int(ap.offset)
    dims = ap.ap
    space = str(ap.space)
    if space == "DRAM":
        ext = sum((c - 1) * abs(s) for s, c in dims)
        return (name, 0, 1, off, off + ext + 1)
    shp = t.shape
    pstride = 1
    for d in shp[1:]:
        pstride *= int(d)
    p_lo = off // pstride
    f_lo = off % pstride
    pc = 1
    fext = 0
    for s, c in dims:
        if s == pstride and c > 1:
            pc = c
        elif s >= pstride and c > 1:
            pc = max(pc, (c - 1) * (s // pstride) + 1)
        else:
            fext += (c - 1) * abs(s)
    return (name, p_lo, p_lo + pc, f_lo, f_lo + fext + 1)


def _overlap(a, b):
    return a[1] < b[2] and b[1] < a[2] and a[3] < b[4] and b[3] < a[4]


class Fw:
    ENGS = ("pe", "act", "dve", "pool", "sp")

    def __init__(self, name="k"):
        self.nc = bass.Bass("TRN2", target_bir_lowering=False)
        self.stack = ExitStack()
        self.prog = {e: [] for e in self.ENGS}
        self.cur = {}
        self.waited = {e: {} for e in self.ENGS}
        self.acc = {}
        self.nsem = 0
        self.sems = {}
        self.slots = {}
        self.slot_i = {}
        self.n_instr = 0
        self.out_tokens = []

    def dram_in(self, name, shape, dt=F32):
        return self.nc.dram_tensor(name, list(shape), dt, kind="ExternalInput").ap()

    def dram_out(self, name, shape, dt=F32):
        return self.nc.dram_tensor(name, list(shape), dt, kind="ExternalOutput").ap()

    def dram_tmp(self, name, shape, dt=F32):
        return self.nc.dram_tensor(name, list(shape), dt, kind="Internal").ap()

    def sb(self, name, shape, dt=F32):
        return self.stack.enter_context(self.nc.sbuf_tensor(name, list(shape), dt))

    def ps(self, name, shape, dt=F32):
        return self.stack.enter_context(self.nc.psum_tensor(name, list(shape), dt))

    def _newsem(self):
        self.nsem += 1
        s = self.stack.enter_context(self.nc.semaphore(f"s{self.nsem}"))
        self.sems[id(s)] = s
        return s

    def _deps(self, eng, reads, writes):
        toks = []
        for ap, is_w in [(a, False) for a in reads] + [(a, True) for a in writes]:
            b = _box(ap)
            for rec in self.acc.get(b[0], ()):
                tok, rw, rb, reng = rec
                if not (is_w or rw):
                    continue
                if not _overlap(b, rb):
                    continue
                if reng == "pe" and eng == "pe":
                    continue
                toks.append(tok)
        return toks

    def _record(self, eng, tok, reads, writes):
        for ap, is_w in [(a, False) for a in reads] + [(a, True) for a in writes]:
            b = _box(ap)
            lst = self.acc.setdefault(b[0], [])
            new = []
            for rec in lst:
                _, rw, rb, reng = rec
                if rb == b and reng == eng and rw == is_w and not eng.startswith("dma"):
                    continue
                if is_w and rb[1] >= b[1] and rb[2] <= b[2] and rb[3] >= b[3] and rb[4] <= b[4]:
                    continue
                new.append(rec)
            new.append((tok, is_w, b, eng))
            self.acc[b[0]] = new

    def _waits(self, eng, toks):
        w = {}
        for sem, val in toks:
            k = id(sem)
            if self.waited[eng].get(k, 0) >= val:
                continue
            if w.get(k, (None, 0))[1] < val:
                w[k] = (sem, val)
        for k, (sem, val) in w.items():
            self.waited[eng][k] = val
        return list(w.values())

    def _next_tok(self, eng):
        c = self.cur.get(eng)
        if c is None or c[1] >= SEM_LIMIT:
            c = [self._newsem(), 0]
            self.cur[eng] = c
        c[1] += 1
        return (c[0], c[1])

    def op(self, eng, fn, reads, writes):
        toks = self._deps(eng, reads, writes)
        waits = self._waits(eng, toks)
        tok = self._next_tok(eng)
        self.prog[eng].append((waits, fn, tok[0], 1))
        self._record(eng, tok, reads, writes)
        self.n_instr += 1
        return tok

    def dma(self, q, out, in_, is_output=False, **kw):
        toks = self._deps("dma" + q, [in_], [out])
        if q not in self.slots:
            self.slots[q] = [[self._newsem(), 0] for _ in range(N_DMA_SLOTS)]
        sl = self.slots[q]
        i = self.slot_i.get(q, 0)
        self.slot_i[q] = i + 1
        s = sl[i % N_DMA_SLOTS]
        if s[1] + 16 > SEM_LIMIT:
            toks.append((s[0], s[1]))
            s = [self._newsem(), 0]
            sl[i % N_DMA_SLOTS] = s
        if s[1] > 0:
            toks.append((s[0], s[1]))
        waits = self._waits(q, toks)
        s[1] += 16
        tok = (s[0], s[1])
        self.prog[q].append((waits, lambda e: e.dma_start(out=out, in_=in_, **kw), tok[0], 16))
        self._record("dma" + q, tok, [in_], [out])
        if is_output:
            self.out_tokens.append(tok)
        self.n_instr += 1
        return tok

    def mm(self, out, lhsT, rhs, start=True, stop=True):
        return self.op("pe", lambda e: e.matmul(out, lhsT, rhs, start=start, stop=stop), [lhsT, rhs], [out])

    def transpose(self, out, in_, ident):
        return self.op("pe", lambda e: e.transpose(out, in_, ident), [in_, ident], [out])

    def act(self, out, in_, func, bias=None, scale=None, accum_out=None, eng="act"):
        kw = {}
        rd = [in_]
        wr = [out]
        if bias is not None:
            kw["bias"] = bias
            if not isinstance(bias, (int, float)):
                rd.append(bias)
        if scale is not None:
            kw["scale"] = scale
            if not isinstance(scale, (int, float)):
                rd.append(scale)
        if accum_out is not None:
            kw["accum_out"] = accum_out
            wr.append(accum_out)
        return self.op("act", lambda e: e.activation(out, in_, func, **kw), rd, wr)

    def tt(self, eng, out, a, b, op):
        return self.op(eng, lambda e: e.tensor_tensor(out, a, b, op), [a, b], [out])

    def ts(self, eng, out, a, s1, s2=None, op0=ALU.mult, op1=None, accum_out=None):
        rd = [a]
        wr = [out]
        if not isinstance(s1, (int, float)):
            rd.append(s1)
        if s2 is not None and not isinstance(s2, (int, float)):
            rd.append(s2)
        kw = {}
        if op1 is not None:
            kw["op1"] = op1
        if accum_out is not None:
            kw["accum_out"] = accum_out
            wr.append(accum_out)
        return self.op(eng, lambda e: e.tensor_scalar(out, a, s1, s2, op0, **kw), rd, wr)

    def stt(self, eng, out, a, s, b, op0, op1):
        rd = [a, b]
        if not isinstance(s, (int, float)):
            rd.append(s)
        return self.op(eng, lambda e: e.scalar_tensor_tensor(out, a, s, b, op0, op1), rd, [out])

    def copy(self, eng, out, in_):
        if eng == "act":
            return self.op("act", lambda e: e.copy(out, in_), [in_], [out])
        return self.op(eng, lambda e: e.tensor_copy(out, in_), [in_], [out])

    def memset(self, eng, out, val):
        return self.op(eng, lambda e: e.memset(out, val), [], [out])

    def reduce(self, eng, out, in_, op, axis=AX.X):
        return self.op(eng, lambda e: e.tensor_reduce(out, in_, axis, op), [in_], [out])

    def finish(self):
        if self.out_tokens:
            waits = self._waits("sp", self.out_tokens)
            self.prog["sp"].append((waits, None, None, 0))
        nc = self.nc
        prog = self.prog

        def emit(e, lst):
            for waits, fn, sem, inc in lst:
                for s, v in waits:
                    e.wait_ge(s, v)
                if fn is not None:
                    fn(e).then_inc(sem, inc)

        with nc.Block() as block:
            @block.tensor
            def _(e):
                emit(e, prog["pe"])

            @block.scalar
            def _(e):
                emit(e, prog["act"])

            @block.vector
            def _(e):
                emit(e, prog["dve"])

            @block.gpsimd
            def _(e):
                emit(e, prog["pool"])

            @block.sync
            def _(e):
                emit(e, prog["sp"])
        self.stack.close()
        return nc


def run(fw, in_maps, n=8, trace=False):
    nc = fw.finish()
    res = run_bass_kernel_spmd(nc, in_maps, core_ids=list(range(n)), trace=trace)
    return res

D = 1024
B = 2
T = 8192
NT = 2048
TT = 512
EPS = 1e-6
NCORES = 8


def consts_common(fw):
    c = {}
    c["ones"] = fw.sb("ones", [128, 128], F32)
    fw.memset("dve", c["ones"][:], 1.0)
    c["eps"] = fw.sb("eps", [128, 1], F32)
    fw.memset("dve", c["eps"][:], EPS)
    return c


def emit_rmsnorm(fw, c, xT, g_sb, uT, t0, tw, sq, ps_ss, rstd, out_dt_scale=None):
    for ch in range(8):
        s = sq[ch % 2]
        eng = "act" if ch % 2 == 0 else "pool"
        if eng == "act":
            fw.act(s[:, :tw], xT[:, ch, t0:t0 + tw], AF.Square)
        else:
            fw.tt("pool", s[:, :tw], xT[:, ch, t0:t0 + tw], xT[:, ch, t0:t0 + tw], ALU.mult)
        fw.mm(ps_ss[:, :tw], c["ones"][:], s[:, :tw], start=(ch == 0), stop=(ch == 7))
    fw.act(rstd[:, :tw], ps_ss[:, :tw], AF.Sqrt, bias=c["eps"][:], scale=1.0 / D)
    fw.op("dve", lambda e: e.reciprocal(rstd[:, :tw], rstd[:, :tw]), [rstd[:, :tw]], [rstd[:, :tw]])
    for ch in range(8):
        fw.stt("dve", uT[:, ch, :tw], xT[:, ch, t0:t0 + tw], g_sb[:, ch:ch + 1], rstd[:, :tw], ALU.mult, ALU.mult)


def rmsnorm_nodve_pieces(fw, c, xT, g_sb, uT, t0, tw, sq, ps_ss, rstd, tmp):
    def stats():
        for ch in range(8):
            s = sq[ch % 2]
            if ch % 2 == 0:
                fw.act(s[:, :tw], xT[:, ch, t0:t0 + tw], AF.Square)
            else:
                fw.tt("pool", s[:, :tw], xT[:, ch, t0:t0 + tw], xT[:, ch, t0:t0 + tw], ALU.mult)
            fw.mm(ps_ss[:, :tw], c["ones"][:], s[:, :tw], start=(ch == 0), stop=(ch == 7))
        fw.act(rstd[:, :tw], ps_ss[:, :tw], AF.Ln, bias=c["eps"][:], scale=1.0 / D)
        fw.act(rstd[:, :tw], rstd[:, :tw], AF.Exp, scale=-0.5)

    def apply():
        for ch in range(8):
            t_ = tmp[ch % 2]
            fw.tt("pool", t_[:, :tw], xT[:, ch, t0:t0 + tw], rstd[:, :tw], ALU.mult)
            fw.act(uT[:, ch, :tw], t_[:, :tw], AF.Copy, scale=g_sb[:, ch:ch + 1])
    return [stats, apply]


def emit_proj(fw, uT, w_bf, Dp, out_dram, t0, tw, ps_list, stg_list, ctr):
    for j in range(Dp // 128):
        ps = ps_list[ctr[0] % len(ps_list)]
        stg = stg_list[ctr[0] % len(stg_list)]
        for ch in range(8):
            fw.mm(ps[:, :tw], w_bf[:, ch, j * 128:(j + 1) * 128], uT[:, ch, :tw], start=(ch == 0), stop=(ch == 7))
        fw.copy("act" if ctr[0] % 2 == 0 else "dve", stg[:, :tw], ps[:, :tw])
        fw.dma("sp", out_dram[j * 128:(j + 1) * 128, t0:t0 + tw], stg[:, :tw], is_output=True)
        ctr[0] += 1


def build_P(Dp):
    fw = Fw()
    hT = fw.dram_in("hT", [D, NT])
    g = fw.dram_in("g", [128, 8])
    w = fw.dram_in("w", [D, Dp])
    o = fw.dram_out("o", [Dp, NT])
    c = consts_common(fw)
    xT = fw.sb("xT", [128, 8, NT], F32)
    g_sb = fw.sb("g_sb", [128, 8], F32)
    w_bf = fw.sb("w_bf", [128, 8, Dp], BF16)
    sq = [fw.sb(f"sq{i}", [128, TT], F32) for i in range(2)]
    rstd = fw.sb("rstd", [128, TT], F32)
    uT = [fw.sb(f"uT{i}", [128, 8, TT], BF16) for i in range(2)]
    ps_ss = fw.ps("ps_ss", [128, TT], F32)
    ps_list = [fw.ps(f"ps{i}", [128, TT], F32) for i in range(4)]
    stg_list = [fw.sb(f"stg{i}", [128, TT], F32) for i in range(4)]
    fw.dma("sp", g_sb[:], g)
    for ti in range(NT // TT):
        for ch in range(8):
            fw.dma("sp", xT[:, ch, ti * TT:(ti + 1) * TT], hT[ch * 128:(ch + 1) * 128, ti * TT:(ti + 1) * TT])
    for ch in range(8):
        fw.dma("pool", w_bf[:, ch, :], w[ch * 128:(ch + 1) * 128, :])
    ctr = [0]
    for ti in range(NT // TT):
        u = uT[ti % 2]
        emit_rmsnorm(fw, c, xT, g_sb, u, ti * TT, TT, sq, ps_ss, rstd)
        emit_proj(fw, u, w_bf, Dp, o, ti * TT, TT, ps_list, stg_list, ctr)
    return fw


HG = 8
HNG = 12
HCH = 96
TWO_PI = 6.283185307179586
MAGIC = 12582912.0


def hy_host_consts():
    n = np.arange(128)
    F = np.exp(-2j * np.pi * np.outer(n, n) / 128.0)
    Tw = np.exp(-2j * np.pi * np.outer(n, n) / 16384.0)
    f32 = lambda a: np.ascontiguousarray(a, dtype=np.float32)
    c = {}
    c["F1a"] = f32(np.concatenate([F.real, F.imag], 1)[:64])
    c["F1b"] = f32(np.concatenate([-F.imag, F.real], 1)[:64])
    c["F2re"] = f32(F.real)
    c["F2im"] = f32(F.imag)
    c["nF2im"] = f32(-F.imag)
    c["TT"] = f32(np.concatenate([Tw.real] * 4, 1))
    c["Tim"] = f32(Tw.imag)
    c["nTim"] = f32(-Tw.imag)
    c["Gc"] = f32(np.concatenate([F.real, -F.imag], 1))
    c["Gd"] = f32(np.concatenate([F.imag, F.real], 1))
    c["iF1re"] = f32(F.real[:, :64] / 16384.0)
    c["iF1im"] = f32(F.imag[:, :64] / 16384.0)
    c["niF1im"] = f32(-F.imag[:, :64] / 16384.0)
    return c


HY_CONST_SHAPES = {"F1a": [64, 256], "F1b": [64, 256], "F2re": [128, 128], "F2im": [128, 128], "nF2im": [128, 128],
                   "TT": [128, 512], "Tim": [128, 128], "nTim": [128, 128], "Gc": [128, 256], "Gd": [128, 256],
                   "iF1re": [128, 64], "iF1im": [128, 64], "niF1im": [128, 64]}


def build_HY():
    fw = Fw()
    G, NG, CH = HG, HNG, HCH
    W = 2 * G * 128
    U = fw.dram_in("U", [NG, 3, 3, 64, W])
    featsT = fw.dram_in("featsT", [33, 8192])
    w1 = fw.dram_in("w1", [33, 64])
    w2 = fw.dram_in("w2", [64, 64])
    w3 = fw.dram_in("w3", [NG, 64, 4 * G])
    b1 = fw.dram_in("b1", [64, 1])
    b2 = fw.dram_in("b2", [64, 1])
    fr = fw.dram_in("fr", [64, 1])
    decay = fw.dram_in("decay", [NG, 64, G * 128])
    sw = fw.dram_in("sw", [64, 9 * CH])
    skip = fw.dram_in("skip", [64, 2 * CH])
    o = fw.dram_out("o", [NG, 64, W])
    cd = {k: fw.dram_in("c_" + k, shp) for k, shp in HY_CONST_SHAPES.items()}
    c = consts_common(fw)
    T1 = [fw.sb(f"T1_{i}", [128, 512], F32) for i in range(4)]
    T2 = [fw.sb(f"T2_{i}", [128, 512], F32) for i in range(4)]
    K = {}
    BF_CONSTS = ("F2re", "F2im", "nF2im", "Gc", "Gd", "iF1re", "iF1im", "niF1im")
    for n_, (k, shp) in enumerate(HY_CONST_SHAPES.items()):
        if k in BF_CONSTS:
            stg = T1[n_ % 4] if n_ % 2 == 0 else T2[n_ % 4]
            sv = stg[0:shp[0], 0:shp[1]]
            fw.dma("sp", sv, cd[k])
            K[k] = fw.sb("k_" + k, shp, BF16)
            fw.copy("dve", K[k][:], sv)
        else:
            K[k] = fw.sb("k_" + k, shp, F32)
            fw.dma("sp", K[k][:], cd[k])
    w1s = fw.sb("w1s", [33, 64]); fw.dma("sp", w1s[:], w1)
    w2s = fw.sb("w2s", [64, 64]); fw.dma("sp", w2s[:], w2)
    b1s = fw.sb("b1s", [64, 1]); fw.dma("sp", b1s[:], b1)
    b2s = fw.sb("b2s", [64, 1]); fw.dma("sp", b2s[:], b2)
    frs = fw.sb("frs", [64, 1]); fw.dma("sp", frs[:], fr)
    sws = fw.sb("sws", [64, 9 * CH]); fw.dma("sp", sws[:], sw)
    sks = fw.sb("sks", [64, 2 * CH]); fw.dma("sp", sks[:], skip)
    frb1 = fw.sb("frb1", [64, 1]); fw.tt("dve", frb1[:], frs[:], b1s[:], ALU.mult)
    frb2 = fw.sb("frb2", [64, 1]); fw.tt("dve", frb2[:], frs[:], b2s[:], ALU.mult)
    h2T = fw.sb("h2T", [64, 8192], F32)
    scr = fw.sb("scr", [64, 4096], F32)
    ft = [scr[0:33, 0:512], scr[0:33, 512:1024]]
    zt = scr[:, 1024:1536]
    rt = scr[:, 1536:2048]
    h1t = scr[:, 2048:2560]
    psR = [fw.ps(f"psR{i}", [128, 512], F32) for i in range(7)]
    psM = fw.ps("psM", [128, 512], F32)
    ring = [0]

    def nps():
        r = psR[ring[0] % 7]
        ring[0] += 1
        return r

    def sin_layer(ps, frb, dst):
        fw.ts("dve", zt, ps, frs[:, 0:1], frb[:, 0:1], op0=ALU.mult, op1=ALU.add)
        fw.ts("dve", rt, zt, 1.0 / TWO_PI, MAGIC, op0=ALU.mult, op1=ALU.add)
        fw.ts("dve", rt, rt, MAGIC, TWO_PI, op0=ALU.subtract, op1=ALU.mult)
        fw.tt("dve", zt, zt, rt, ALU.subtract)
        fw.act(dst, zt, AF.Sin, scale=1.0 - 1e-6)

    for ti in range(16):
        f = ft[ti % 2]
        fw.dma("sp", f, featsT[:, ti * 512:(ti + 1) * 512])
        fw.mm(psM[0:64, :], w1s[:], f)
        sin_layer(psM[0:64, :], frb1, h1t)
        fw.mm(psM[0:64, :], w2s[:], h1t)
        sin_layer(psM[0:64, :], frb2, h2T[:, ti * 512:(ti + 1) * 512])

    w3s = fw.sb("w3s", [64, 4 * G], F32)
    dec = fw.sb("dec", [64, G, 128], F32)
    hf = fw.sb("hf", [64, 4 * G, 128], F32)
    habs_v = scr[:].rearrange("p (a n) -> p a n", a=4 * G)
    rs = fw.sb("rs", [64, 4 * G], F32)
    dsum = fw.sb("dsum", [128, 4 * G], F32)
    rden = fw.sb("rden", [128, 2 * G], F32)
    kk = fw.sb("kk", [128, 2, G, 512], F32)
    Xs = [fw.sb(f"Xs{i}", [128, 512], F32) for i in range(2)]
    sd = [fw.sb(f"sd{i}", [128, 256], F32) for i in range(2)]
    ush2 = fw.sb("ush2", [64, 2, G, 128], F32)
    Ushv = [scr[:, 0:2048].rearrange("p (b c n) -> p b c n", b=2, c=G),
            scr[:, 2048:4096].rearrange("p (b c n) -> p b c n", b=2, c=G), ush2[:]]
    uc = [fw.sb(f"uc{s}", [64, 2, G, 128], F32) for s in range(3)]
    OPb = [fw.sb(f"OP{i}", [128, 512], BF16) for i in range(6)]
    gtl = [fw.sb(f"gt{i}", [64, 2, 2, 128], F32) for i in range(4)]
    cnt = {"t": 0, "op": 0, "x": 0, "g": 0}

    def vw(ap, lay):
        if lay == "crk":
            return ap.rearrange("p (c r k) -> p c r k", c=2, r=2)
        return ap.rearrange("p (r c k) -> p c r k", c=2, r=2)

    def cmul(ps, lin, mulP1, mulRe, mulIm, lout):
        a, b = T1[cnt["t"] % 4], T2[cnt["t"] % 4]
        cnt["t"] += 1
        dst = OPb[cnt["op"] % 6]
        cnt["op"] += 1
        p4 = vw(ps, lin)
        fw.tt("dve", vw(a[:], lout), p4, mulP1, ALU.mult)
        fw.tt("dve", vw(b[:], lout)[:, :, 0, :], p4[:, :, 1, :], mulRe, ALU.mult)
        fw.tt("dve", vw(b[:], lout)[:, :, 1, :], p4[:, :, 0, :], mulIm, ALU.mult)
        fw.tt("pool", dst[:], a[:], b[:], ALU.add)
        return dst

    TT4 = K["TT"][:].rearrange("p (c r k) -> p c r k", c=2, r=2)
    Tim_b = K["Tim"][:].unsqueeze(1).broadcast_to([128, 2, 128])
    nTim_b = K["nTim"][:].unsqueeze(1).broadcast_to([128, 2, 128])

    def fwd_s2(a):
        px = nps()
        fw.mm(px[:, 0:256], K["F2re"][:], a[:, 0:256], start=True, stop=False)
        fw.mm(px[:, 0:256], K["nF2im"][:], a[:, 256:512], start=False, stop=True)
        fw.mm(px[:, 256:512], K["F2re"][:], a[:, 256:512], start=True, stop=False)
        fw.mm(px[:, 256:512], K["F2im"][:], a[:, 0:256], start=False, stop=True)
        return px

    for g in range(NG):
        c0 = g * G
        fw.dma("sp", w3s[:], w3[g])
        fw.dma("sp", dec[:], decay[g].rearrange("p (c n) -> p c n", c=G))
        for nb in range(8):
            pl3 = nps()
            for i in range(16):
                n2 = nb * 16 + i
                fw.mm(pl3[0:64, i * 32:(i + 1) * 32], h2T[:, n2 * 64:(n2 + 1) * 64], w3s[:])
            pin = pl3[0:64, :].rearrange("p (n a c) -> p n a c", n=16, a=4)
            dv = dec[:, :, nb * 16:(nb + 1) * 16].rearrange("p c n -> p n c").unsqueeze(2).broadcast_to([64, 16, 4, G])
            ov = hf[:, :, nb * 16:(nb + 1) * 16].rearrange("p (a c) n -> p n a c", a=4)
            fw.tt("dve", ov, pin, dv, ALU.mult)
        fw.act(habs_v, hf[:], AF.Abs)
        fw.reduce("dve", rs[:], habs_v, ALU.add)
        fw.mm(psM[:, 0:4 * G], c["ones"][0:64, :], rs[:])
        fw.copy("act", dsum[:], psM[:, 0:4 * G])
        d4 = dsum[:].rearrange("p (o d c) -> p o d c", o=2, d=2)
        r3 = rden[:].rearrange("p (o c) -> p o c", o=2)
        fw.tt("dve", r3, d4[:, :, 0, :], d4[:, :, 1, :], ALU.add)
        fw.ts("dve", rden[:], rden[:], 1e-6, None, op0=ALU.add)
        fw.op("dve", lambda e: e.reciprocal(rden[:], rden[:]), [rden[:]], [rden[:]])
        for s in range(3):
            for j in range(3):
                fw.dma("sp", Ushv[j], U[g, s, j].rearrange("p (b c n) -> p b c n", b=2, c=G))
            def wsc(j, cc):
                col = (s * 3 + j) * CH + c0 + cc
                return sws[:, col:col + 1]
            for cc in range(G):
                fw.act(uc[s][:, :, cc, :], Ushv[0][:, :, cc, :], AF.Copy, scale=wsc(0, cc))
            for j in (1, 2):
                for cc in range(G):
                    acc = uc[s][:, :, cc, :]
                    fw.stt("dve", acc, Ushv[j][:, :, cc, :], wsc(j, cc), acc, ALU.mult, ALU.add)
        ocs = [(oo, cc) for oo in range(2) for cc in range(G)]
        for q0 in range(0, len(ocs), 4):
            wave = ocs[q0:q0 + 4]
            pas = []
            for (oo, cc) in wave:
                pa = nps()
                for d in range(2):
                    col = (oo * 2 + d) * G + cc
                    fw.mm(pa[:, d * 256:(d + 1) * 256], hf[:, col, :], K["F1a"][:])
                pas.append(pa)
            aps = [cmul(pa[:], "crk", TT4, nTim_b, Tim_b, "rck") for pa in pas]
            pxs = [fwd_s2(a) for a in aps]
            for (oo, cc), px in zip(wave, pxs):
                xs_, sd_ = Xs[cnt["x"] % 2], sd[cnt["x"] % 2]
                cnt["x"] += 1
                fw.copy("act", xs_[:], px[:])
                fw.tt("pool", sd_[:, 0:128], xs_[:, 0:128], xs_[:, 128:256], ALU.add)
                fw.tt("pool", sd_[:, 128:256], xs_[:, 256:384], xs_[:, 384:512], ALU.subtract)
                rsc = rden[:, oo * G + cc:oo * G + cc + 1]
                kv = kk[:, oo, cc, :]
                fw.ts("dve", kv[:, 0:256].rearrange("p (r k) -> p r k", r=2),
                      sd_[:, 0:128].unsqueeze(1).broadcast_to([128, 2, 128]), rsc, None, op0=ALU.mult)
                fw.ts("dve", kv[:, 384:512], sd_[:, 128:256], rsc, None, op0=ALU.mult)
                fw.ts("dve", kv[:, 256:384], sd_[:, 128:256], rsc, -1.0, op0=ALU.mult, op1=ALU.mult)
        for oo in range(2):
            zin = uc[2]
            gate = uc[0] if oo == 0 else uc[1]
            zout = uc[2]
            prs = list(range(G // 2))
            pas = []
            for p in prs:
                pa = nps()
                for ci in range(2):
                    cc = 2 * p + ci
                    fw.mm(pa[:, ci * 256:(ci + 1) * 256], zin[:, 0, cc, :], K["F1a"][:], start=True, stop=False)
                    fw.mm(pa[:, ci * 256:(ci + 1) * 256], zin[:, 1, cc, :], K["F1b"][:], start=False, stop=True)
                pas.append(pa)
            aps = [cmul(pa[:], "crk", TT4, nTim_b, Tim_b, "rck") for pa in pas]
            pxs = [fwd_s2(a) for a in aps]
            yps = []
            for p, px in zip(prs, pxs):
                kp = kk[:, oo, 2 * p:2 * p + 2, :]
                yps.append(cmul(px[:], "rck", kp[:, :, 0:256].rearrange("p c (r k) -> p c r k", r=2),
                                kp[:, :, 256:384], kp[:, :, 384:512], "crk"))
            pbs = []
            for yp in yps:
                pb = nps()
                y4 = vw(yp[:], "crk")
                for ci in range(2):
                    fw.mm(pb[:, ci * 256:(ci + 1) * 256], y4[:, ci, 0, :], K["Gc"][:], start=True, stop=False)
                    fw.mm(pb[:, ci * 256:(ci + 1) * 256], y4[:, ci, 1, :], K["Gd"][:], start=False, stop=True)
                pbs.append(pb)
            bps = [cmul(pb[:], "crk", TT4, Tim_b, nTim_b, "rck") for pb in pbs]
            pys = []
            for bp in bps:
                py = nps()
                yv = py[0:64, :].rearrange("p (b c n) -> p b c n", b=2, c=2)
                fw.mm(yv[:, 0, :, :], K["iF1re"][:], bp[:, 0:256], start=True, stop=False)
                fw.mm(yv[:, 0, :, :], K["iF1im"][:], bp[:, 256:512], start=False, stop=True)
                fw.mm(yv[:, 1, :, :], K["iF1re"][:], bp[:, 256:512], start=True, stop=False)
                fw.mm(yv[:, 1, :, :], K["niF1im"][:], bp[:, 0:256], start=False, stop=True)
                pys.append(py)
            for p, py in zip(prs, pys):
                yv = py[0:64, :].rearrange("p (b c n) -> p b c n", b=2, c=2)
                gt = gtl[cnt["g"] % 4]
                cnt["g"] += 1
                base = oo * CH + c0 + 2 * p
                for ci in range(2):
                    fw.act(gt[:, :, ci, :], zin[:, :, 2 * p + ci, :], AF.Copy, scale=sks[:, base + ci:base + ci + 1])
                fw.tt("dve", gt[:], yv, gt[:], ALU.add)
                fw.tt("pool", zout[:, :, 2 * p:2 * p + 2, :], gate[:, :, 2 * p:2 * p + 2, :], gt[:], ALU.mult)
        fw.dma("sp", o[g].rearrange("p (b c n) -> p b c n", b=2, c=G), uc[2][:], is_output=True)
    return fw


def hy_host_inputs(proj, hy_w, core):
    G, NG, CH = HG, HNG, HCH
    ch0 = core * CH
    Umat = np.empty((NG, 3, 3, 64, 2 * G * 128), np.float32)
    for s in range(3):
        a = proj[:, :, s * 768 + ch0: s * 768 + ch0 + CH]
        ap = np.pad(a, ((0, 0), (1, 1), (0, 0)))
        for j in range(3):
            sh = ap[:, j:j + T, :]
            x = sh.transpose(0, 2, 1).reshape(B, NG, G, 64, 128)
            Umat[:, s, j] = x.transpose(1, 3, 0, 2, 4).reshape(NG, 64, 2 * G * 128)
    d = {"U": Umat}
    d.update(hy_w[core])
    return d


def hy_host_weights(short_w, w1, b1, w2, b2, w3, freq, skip):
    G, NG, CH = HG, HNG, HCH
    L = T
    m = np.arange(L, dtype=np.float64)
    tt_ = m / (L - 1)
    wv = 2.0 * np.pi * m / L
    bands = np.linspace(1e-4, 15.0, 16)
    feats = np.concatenate([tt_[:, None], np.cos(bands[None] * wv[:, None]), -np.sin(bands[None] * wv[:, None])], 1)
    perm = (np.arange(64)[None, :] * 128 + np.arange(128)[:, None]).reshape(-1)
    featsT = np.ascontiguousarray(feats[perm].T, dtype=np.float32)
    max_decay = np.log(1e-2) / 0.3
    min_decay = np.log(1e-2) / 1.5
    deltas = np.abs(np.linspace(min_decay, max_decay, 768))
    dec_full = np.exp(-tt_[:, None] * deltas[None, :])
    consts = hy_host_consts()
    outs = []
    w3r = w3.reshape(64, 2, 2, 768)
    for core in range(NCORES):
        ch0 = core * CH
        d = {"featsT": featsT, "w1": np.ascontiguousarray(w1), "w2": np.ascontiguousarray(w2),
             "b1": np.ascontiguousarray(b1.reshape(64, 1)), "b2": np.ascontiguousarray(b2.reshape(64, 1)),
             "fr": np.ascontiguousarray(freq.reshape(64, 1))}
        w3c = w3r[:, :, :, ch0:ch0 + CH].reshape(64, 2, 2, NG, G)
        d["w3"] = np.ascontiguousarray(w3c.transpose(3, 0, 1, 2, 4).reshape(NG, 64, 4 * G))
        dc = dec_full[:, ch0:ch0 + CH].reshape(64, 128, NG, G)
        d["decay"] = np.ascontiguousarray(dc.transpose(2, 0, 3, 1).reshape(NG, 64, G * 128), dtype=np.float32)
        swc = short_w.reshape(3, 3, 768)[:, :, ch0:ch0 + CH]
        swl = swc.transpose(1, 0, 2).reshape(1, 9 * CH)
        d["sw"] = np.ascontiguousarray(np.broadcast_to(swl, (64, 9 * CH)))
        skl = skip[:, ch0:ch0 + CH].reshape(1, 2 * CH)
        d["skip"] = np.ascontiguousarray(np.broadcast_to(skl, (64, 2 * CH)))
        for k, v in consts.items():
            d["c_" + k] = v
        outs.append(d)
    return outs


def hy_host_gather(results):
    G, NG, CH = HG, HNG, HCH
    main = np.empty((B, T, 768), np.float32)
    for core, r in enumerate(results):
        x = r.reshape(NG, 64, B, G, 128)
        x = x.transpose(2, 1, 4, 0, 3).reshape(B, T, CH)
        main[:, :, core * CH:(core + 1) * CH] = x
    return main


def at_host_consts():
    rows = T // 64
    pos_row = np.repeat(np.arange(rows), 64).astype(np.float64)
    pos_col = np.tile(np.arange(64), rows).astype(np.float64)
    inv = 1.0 / (10000.0 ** (np.arange(0, 32, 2, dtype=np.float64) / 32))
    ang = np.stack([pos_row[:, None] * inv, pos_col[:, None] * inv], 1)
    d = np.arange(64)
    axis = d // 32
    f = d % 16
    cosT = np.cos(ang[:, axis, f]).T
    sinT = np.sin(ang[:, axis, f]).T
    R = np.zeros((64, 64))
    for a in range(2):
        for ff in range(16):
            i0 = a * 32 + ff
            i1 = a * 32 + 16 + ff
            R[i0, i1] = -1.0
            R[i1, i0] = 1.0
    f32 = lambda a: np.ascontiguousarray(a, dtype=np.float32)
    return {"cosT": f32(cosT), "sinT": f32(sinT), "rotT": f32(R.T)}


def build_AT():
    fw = Fw()
    qT = fw.dram_in("qT", [3, 64, T])
    kT = fw.dram_in("kT", [64, T])
    v = fw.dram_in("v", [T, 64])
    gq = fw.dram_in("gq", [64, 1])
    gk = fw.dram_in("gk", [64, 1])
    cosT = fw.dram_in("cosT", [64, T])
    sinT = fw.dram_in("sinT", [64, T])
    rotT = fw.dram_in("rotT", [64, 64])
    o = fw.dram_out("o", [3, 64, T])
    c = consts_common(fw)
    cs = fw.sb("cs", [64, T], F32); fw.dma("sp", cs[:], cosT)
    sn = fw.sb("sn", [64, T], F32); fw.dma("sp", sn[:], sinT)
    rot = fw.sb("rot", [64, 64], F32); fw.dma("sp", rot[:], rotT)
    gqs = fw.sb("gqs", [64, 1], F32); fw.dma("sp", gqs[:], gq)
    gks = fw.sb("gks", [64, 1], F32); fw.dma("sp", gks[:], gk)
    qb = [fw.sb(f"qb{h}", [128, T], BF16) for h in range(3)]
    kb = fw.sb("kb", [128, T], BF16)
    for t_ in qb + [kb]:
        fw.memset("pool", t_[64:128, :], 0.0)
    vst = fw.sb("vst", [128, 64, 64], F32)
    va = fw.sb("va", [128, 64, 128], BF16)
    fw.dma("sp", vst[:], v.rearrange("(c p) d -> p c d", p=128))
    fw.memset("pool", va[:, :, 64:128], 0.0)
    fw.copy("dve", va[:, :, 0:64], vst[:])
    fw.memset("dve", va[:, :, 64:65], 1.0)
    psS = [fw.ps(f"psS{i}", [128, TT], F32) for i in range(3)]
    psO = [fw.ps(f"psO{i}", [128, TT], F32) for i in range(2)]
    psA = fw.ps("psA", [128, TT], F32)
    psB = fw.ps("psB", [128, TT], F32)
    xin = [fw.sb(f"xin{i}", [64, TT], F32) for i in range(2)]
    sq = [fw.sb(f"sqa{i}", [64, TT], F32) for i in range(2)]
    rstd = [fw.sb(f"rstda{i}", [64, TT], F32) for i in range(2)]
    xn = [fw.sb(f"xna{i}", [64, TT], F32) for i in range(2)]
    ra = [fw.sb(f"ra{i}", [64, TT], F32) for i in range(2)]
    rb = [fw.sb(f"rb{i}", [64, TT], F32) for i in range(2)]
    pA = [psA[0:64, :], psS[0][0:64, :]]
    pB = [psB[0:64, :], psS[1][0:64, :]]
    jobs = [(kT, gks, kb, ti) for ti in range(T // TT)]
    for h_ in range(3):
        jobs += [(qT[h_], gqs, qb[h_], ti) for ti in range(T // TT)]
    for j0 in range(0, len(jobs), 2):
        pair = list(enumerate(jobs[j0:j0 + 2]))
        sls = [slice(ti * TT, (ti + 1) * TT) for _, (_, _, _, ti) in pair]
        for i, (src, g, dst, ti) in pair:
            fw.dma("sp", xin[i][:], src[:, sls[i]])
        for i, _ in pair:
            fw.act(sq[i][:], xin[i][:], AF.Square)
        for i, _ in pair:
            fw.mm(pA[i], c["ones"][0:64, 0:64], sq[i][:])
        for i, _ in pair:
            fw.act(rstd[i][:], pA[i], AF.Sqrt, bias=c["eps"][0:64, :], scale=1.0 / 64)
        for i, _ in pair:
            r_ = rstd[i]
            fw.op("dve", lambda e, r_=r_: e.reciprocal(r_[:], r_[:]), [r_[:]], [r_[:]])
        for i, (src, g, dst, ti) in pair:
            fw.stt("dve", xn[i][:], xin[i][:], g[:, 0:1], rstd[i][:], ALU.mult, ALU.mult)
        for i, _ in pair:
            fw.mm(pB[i], rot[:], xn[i][:])
        for i, _ in pair:
            fw.tt("dve", rb[i][:], pB[i], sn[:, sls[i]], ALU.mult)
        for i, _ in pair:
            fw.tt("pool", ra[i][:], xn[i][:], cs[:, sls[i]], ALU.mult)
        for i, (src, g, dst, ti) in pair:
            fw.tt("pool", dst[0:64, sls[i]], ra[i][:], rb[i][:], ALU.add)
    pt = [fw.sb(f"pt{i}", [128, TT], BF16) for i in range(3)]
    lsb = fw.sb("lsb", [128, TT], F32)
    rec = fw.sb("rec", [64, TT], F32)
    ost = [fw.sb(f"ost{i}", [64, TT], F32) for i in range(2)]
    it = 0
    blk = 0
    for h in range(3):
        for qi in range(T // TT):
            qs = slice(qi * TT, (qi + 1) * TT)
            po = psO[blk % 2]
            for kc in range(2):
                fw.mm(psS[(it + kc) % 3][:], kb[:, kc * 128:(kc + 1) * 128], qb[h][:, qs])
            for kc in range(64):
                ps = psS[it % 3]
                p = pt[it % 3]
                fw.act(p[:], ps[:], AF.Exp, scale=0.125)
                if kc + 2 < 64:
                    fw.mm(psS[(it + 2) % 3][:], kb[:, (kc + 2) * 128:(kc + 3) * 128], qb[h][:, qs])
                fw.mm(po[:], va[:, kc, :], p[:], start=(kc == 0), stop=(kc == 63))
                it += 1
            fw.copy("act", lsb[64:65, :], po[64:65, :])
            fw.mm(psA[0:64, :], c["ones"][64:65, 0:64], lsb[64:65, :])
            fw.op("dve", lambda e: e.reciprocal(rec[:], psA[0:64, :]), [psA[0:64, :]], [rec[:]])
            os_ = ost[blk % 2]
            fw.tt("dve", os_[:], po[0:64, :], rec[:], ALU.mult)
            fw.dma("sp", o[h, :, qs], os_[:], is_output=True)
            blk += 1
    return fw


def build_O():
    fw = Fw()
    hT = fw.dram_in("hT", [D, NT])
    mainT = fw.dram_in("mainT", [768, NT])
    cqT = fw.dram_in("cqT", [4, 64, NT])
    memT = fw.dram_in("memT", [D, 256])
    g_mem = fw.dram_in("g_mem", [128, 8])
    w_kv = fw.dram_in("w_kv", [D, 512])
    w_out = fw.dram_in("w_out", [D, D])
    g_ffn = fw.dram_in("g_ffn", [128, 8])
    w_r = fw.dram_in("w_r", [D, 16])
    ident = fw.dram_in("ident", [128, 128])
    ho = fw.dram_out("ho", [D, NT])
    affo = fw.dram_out("aff", [NT, 16])
    c = consts_common(fw)
    xT = fw.sb("xT", [128, 8, NT], F32)
    gm = fw.sb("gm", [128, 8], F32); fw.dma("sp", gm[:], g_mem)
    gf = fw.sb("gf", [128, 8], F32); fw.dma("sp", gf[:], g_ffn)
    wr = fw.sb("wr", [128, 8, 16], F32); fw.dma("sp", wr[:], w_r.rearrange("(c p) e -> p c e", p=128))
    idf = fw.sb("idf", [128, 128], F32); fw.dma("sp", idf[:], ident)
    idb = fw.sb("idb", [128, 128], BF16); fw.copy("dve", idb[:], idf[:])
    mT = fw.sb("mT", [128, 8, 256], F32); fw.dma("sp", mT[:], memT.rearrange("(c p) m -> p c m", p=128))
    wkv = fw.sb("wkv", [128, 8, 512], BF16); fw.dma("pool", wkv[:], w_kv.rearrange("(c p) f -> p c f", p=128))
    main_bf = fw.sb("main_bf", [128, 6, NT], BF16); fw.dma("pool", main_bf[:], mainT.rearrange("(c p) t -> p c t", p=128))
    cq_bf = fw.sb("cq_bf", [64, 4, NT], BF16); fw.dma("pool", cq_bf[:], cqT.rearrange("h p t -> p h t"))
    wo_bf = fw.sb("wo_bf", [128, 6, D], BF16); fw.dma("pool", wo_bf[:], w_out[0:768, :].rearrange("(c p) d -> p c d", p=128))
    woc_bf = fw.sb("woc_bf", [64, 4, D], BF16); fw.dma("pool", woc_bf[:], w_out[768:1024, :].rearrange("(h p) d -> p h d", p=64))
    cross_bf = fw.sb("cross_bf", [64, 4, NT], BF16)
    for ti in range(NT // TT):
        for ch in range(8):
            fw.dma("sp", xT[:, ch, ti * TT:(ti + 1) * TT], hT[ch * 128:(ch + 1) * 128, ti * TT:(ti + 1) * TT])
    sq = [fw.sb(f"sq{i}", [128, TT], F32) for i in range(2)]
    rstd = fw.sb("rstd", [128, TT], F32)
    ps_ss = fw.ps("ps_ss", [128, TT], F32)
    psS = [fw.ps(f"psS{i}", [128, TT], F32) for i in range(2)]
    psT = [fw.ps(f"psT{i}", [128, 128], BF16) for i in range(2)]
    psO = fw.ps("psO", [128, TT], F32)
    psP = [fw.ps(f"psP{i}", [128, TT], F32) for i in range(2)]
    memn = fw.sb("memn", [128, 8, 256], BF16)
    emit_rmsnorm(fw, c, mT, gm, memn, 0, 256, sq, ps_ss, rstd)
    mkT = fw.sb("mkT", [64, 4, 256], BF16)
    mv = fw.sb("mv", [128, 2, 256], BF16)
    for h in range(4):
        for ch in range(8):
            fw.mm(psS[0][0:64, 0:256], wkv[:, ch, h * 64:(h + 1) * 64], memn[:, ch, :], start=(ch == 0), stop=(ch == 7))
        fw.copy("act", mkT[:, h, :], psS[0][0:64, 0:256])
    for mc in range(2):
        for ch in range(8):
            fw.mm(psS[1][:, 0:256], memn[:, ch, mc * 128:(mc + 1) * 128], wkv[:, ch, 256:512], start=(ch == 0), stop=(ch == 7))
        fw.copy("act", mv[:, mc, :], psS[1][:, 0:256])
    pe_ = [fw.sb(f"pe{i}", [128, 256], F32) for i in range(2)]
    pn = [fw.sb(f"pn{i}", [128, 256], BF16) for i in range(2)]
    pT = [fw.sb(f"pT{i}", [128, 128], BF16) for i in range(4)]
    st = [fw.sb(f"st{i}", [128, 4], F32) for i in range(2)]
    it = 0
    tcnt = 0
    for tile in range(NT // TT):
        for h in range(4):
            for qp in range(0, 4, 2):
                units = [(qp + u, u) for u in range(2)]
                for qb_, i in units:
                    q0 = tile * TT + qb_ * 128
                    fw.mm(psS[i][:, 0:256], cq_bf[:, h, q0:q0 + 128], mkT[:, h, :])
                for qb_, i in units:
                    fw.reduce("dve", st[i][:, 0:1], psS[i][:, 0:256], ALU.max)
                for qb_, i in units:
                    fw.ts("dve", st[i][:, 1:2], st[i][:, 0:1], -0.125, None, op0=ALU.mult)
                for qb_, i in units:
                    fw.act(pe_[i][:], psS[i][:, 0:256], AF.Exp, bias=st[i][:, 1:2], scale=0.125)
                for qb_, i in units:
                    fw.reduce("dve", st[i][:, 2:3], pe_[i][:], ALU.add)
                for qb_, i in units:
                    s_ = st[i]
                    fw.op("dve", lambda e, s_=s_: e.reciprocal(s_[:, 3:4], s_[:, 2:3]), [s_[:, 2:3]], [s_[:, 3:4]])
                for qb_, i in units:
                    fw.ts("dve", pn[i][:], pe_[i][:], st[i][:, 3:4], None, op0=ALU.mult)
                for mc in range(2):
                    for qb_, i in units:
                        fw.transpose(psT[i][:], pn[i][:, mc * 128:(mc + 1) * 128], idb[:])
                    for qb_, i in units:
                        fw.copy("act", pT[2 * mc + i][:], psT[i][:])
                for qb_, i in units:
                    for mc in range(2):
                        fw.mm(psO[0:64, qb_ * 128:(qb_ + 1) * 128], mv[:, mc, h * 64:(h + 1) * 64], pT[2 * mc + i][:], start=(mc == 0), stop=(mc == 1))
                it += 2
            fw.copy("act", cross_bf[:, h, tile * TT:(tile + 1) * TT], psO[0:64, :])
    xn = fw.sb("xn", [128, 8, TT], F32)
    lg = fw.sb("lg", [128, 16], F32)
    affs = fw.sb("affs", [128, 16, 16], F32)
    pc = 0
    for tile in range(NT // TT):
        ts_ = slice(tile * TT, (tile + 1) * TT)
        for j in range(8):
            ps = psP[pc % 2]
            pc += 1
            js = slice(j * 128, (j + 1) * 128)
            for cc in range(6):
                fw.mm(ps[:], wo_bf[:, cc, js], main_bf[:, cc, ts_], start=(cc == 0), stop=False)
            for h in range(4):
                fw.mm(ps[:], woc_bf[:, h, js], cross_bf[:, h, ts_], start=False, stop=(h == 3))
            fw.tt("dve", xT[:, j, ts_], xT[:, j, ts_], ps[:], ALU.add)
            fw.dma("sp", ho[js, ts_], xT[:, j, ts_], is_output=True)
        emit_rmsnorm(fw, c, xT, gf, xn, tile * TT, TT, sq, ps_ss, rstd)
        for b_ in range(4):
            blk = tile * 4 + b_
            s_ = st[it % 2]
            it += 1
            pl = psO[:, 0:16]
            for ch in range(8):
                fw.mm(pl, xn[:, ch, b_:TT:4], wr[:, ch, :], start=(ch == 0), stop=(ch == 7))
            fw.reduce("dve", s_[:, 0:1], pl, ALU.max)
            fw.ts("dve", s_[:, 1:2], s_[:, 0:1], -1.0, None, op0=ALU.mult)
            fw.act(lg[:], pl, AF.Exp, bias=s_[:, 1:2], scale=1.0)
            fw.reduce("dve", s_[:, 2:3], lg[:], ALU.add)
            fw.op("dve", lambda e, s_=s_: e.reciprocal(s_[:, 3:4], s_[:, 2:3]), [s_[:, 2:3]], [s_[:, 3:4]])
            fw.ts("dve", affs[:, blk, :], lg[:], s_[:, 3:4], None, op0=ALU.mult)
        fw.dma("sp", affo[tile * TT:(tile + 1) * TT, :].rearrange("(p b) e -> p b e", b=4),
               affs[:, tile * 4:(tile + 1) * 4, :], is_output=True)
    return fw


N_BISECT = 34
CAP = 1024


def m_host_consts():
    p = np.arange(128)
    Gm = (p[:, None] // 8 == p[None, :] // 8).astype(np.float32)
    G16 = (p[:, None] // 8 == np.arange(16)[None, :]).astype(np.float32)
    selm = np.zeros((16, 16, 128), np.float32)
    for e in range(16):
        selm[e, e, :] = 1.0
    return {"Gm": Gm, "G16": G16, "selm": selm.reshape(16, 2048)}


def build_M():
    fw = Fw()
    hT = fw.dram_in("hT", [D, NT])
    g_ffn = fw.dram_in("g_ffn", [128, 8])
    affP = fw.dram_in("affP", [128, 1024])
    affT = fw.dram_in("affT", [16, NT])
    Gm_d = fw.dram_in("Gm", [128, 128])
    G16_d = fw.dram_in("G16", [128, 16])
    selm_d = fw.dram_in("selm", [16, 2048])
    wg = fw.dram_in("wg", [16, D, 768])
    wu = fw.dram_in("wu", [16, D, 768])
    wd = fw.dram_in("wd", [16, 768, D])
    ho = fw.dram_out("ho", [D, NT])
    c = consts_common(fw)
    xT = fw.sb("xT", [128, 8, NT], F32)
    gf = fw.sb("gf", [128, 8], F32); fw.dma("sp", gf[:], g_ffn)
    aP = fw.sb("aP", [128, 1024], F32); fw.dma("sp", aP[:], affP)
    wT = fw.sb("wT", [16, NT], F32); fw.dma("sp", wT[:], affT)
    Gm = fw.sb("Gm_s", [128, 128], F32); fw.dma("sp", Gm[:], Gm_d)
    selm = fw.sb("selm_s", [16, 2048], F32); fw.dma("sp", selm[:], selm_d)
    for ti in range(NT // TT):
        for ch in range(8):
            fw.dma("sp", xT[:, ch, ti * TT:(ti + 1) * TT], hT[ch * 128:(ch + 1) * 128, ti * TT:(ti + 1) * TT])
    wgb = [fw.sb(f"wgb{i}", [128, 8, 384], BF16) for i in range(2)]
    wub = [fw.sb(f"wub{i}", [128, 8, 384], BF16) for i in range(2)]
    wdb = [fw.sb(f"wdb{i}", [128, 3, D], BF16) for i in range(2)]

    def load_w(e, half, i):
        fs = slice(half * 384, (half + 1) * 384)
        fw.dma("pool", wgb[i][:], wg[e][:, fs].rearrange("(c p) f -> p c f", p=128))
        fw.dma("pool", wub[i][:], wu[e][:, fs].rearrange("(c p) f -> p c f", p=128))
        fw.dma("pool", wdb[i][:], wd[e][fs, :].rearrange("(c p) d -> p c d", p=128))

    load_w(0, 0, 0)
    load_w(0, 1, 1)
    sq = [fw.sb(f"sq{i}", [128, TT], F32) for i in range(2)]
    rstd = fw.sb("rstd", [128, TT], F32)
    ps_ss = fw.ps("ps_ss", [128, TT], F32)
    psG = [fw.ps(f"psG{i}", [128, TT], F32) for i in range(2)]
    psU = [fw.ps(f"psU{i}", [128, TT], F32) for i in range(2)]
    psY = [fw.ps(f"psY{i}", [128, TT], F32) for i in range(2)]
    psW = fw.ps("psW", [128, TT], F32)
    cmp_ = fw.sb("cmp", [128, 1024], F32)
    cn = fw.sb("cn", [128, 1], F32)
    bs = fw.sb("bs128", [128, 4], F32)
    lo, mid, ge = (bs[:, k:k + 1] for k in range(3))
    w = 0.75
    fw.memset("dve", lo, 0.0)
    fw.memset("dve", mid, w)
    xn = fw.sb("xn", [128, 8, NT], BF16)
    sg = [fw.sb(f"sg{i}", [128, TT], F32) for i in range(2)]
    pieces = []
    for tile in range(NT // TT):
        pieces += rmsnorm_nodve_pieces(fw, c, xT, gf, xn[:, :, tile * TT:(tile + 1) * TT], tile * TT, TT, sq, ps_ss, rstd, sg)
    for itn in range(N_BISECT):
        if itn % 4 == 1 and pieces:
            pieces.pop(0)()
        fw.ts("dve", cmp_[:], aP[:], mid, None, op0=ALU.is_ge)
        fw.reduce("dve", cn[:], cmp_[:], ALU.add)
        fw.mm(psW[:, 0:1], Gm[:], cn[:])
        fw.ts("dve", ge, psW[:, 0:1], float(CAP) - 0.5, None, op0=ALU.is_ge)
        fw.stt("dve", lo, ge, w, lo, ALU.mult, ALU.add)
        w = w * 0.5
        fw.ts("dve", mid, lo, w, None, op0=ALU.add)
    thr_d = fw.dram_tmp("thr_d", [128, 1])
    thr16 = fw.sb("thr16", [16, 1], F32)
    fw.dma("sp", thr_d, lo)
    fw.dma("sp", thr16[:], thr_d.rearrange("(e s) o -> e (s o)", s=8)[:, 0:1], allow_slow_non_contiguous=True)
    fw.stt("dve", wT[:], wT[:], thr16[:, 0:1], wT[:], ALU.is_ge, ALU.mult)
    while pieces:
        pieces.pop(0)()
    wbc = [fw.sb(f"wbc{i}", [128, TT], F32) for i in range(2)]
    hid = [fw.sb(f"hid{i}", [128, 3, TT], BF16) for i in range(2)]
    gi = [0]
    yi = [0]
    steps = [(k, tile) for k in range(32) for tile in range(NT // TT)]

    def gateup(sidx):
        k, tile = steps[sidx]
        e, i = k // 2, k % 2
        ts_ = slice(tile * TT, (tile + 1) * TT)
        wb = wbc[sidx % 2]
        fw.mm(psW[:], selm[:, e * 128:(e + 1) * 128], wT[:, ts_])
        fw.copy("act", wb[:], psW[:])
        hd = hid[sidx % 2]
        for f in range(3):
            pg = psG[gi[0] % 2]
            pu = psU[gi[0] % 2]
            s_ = sg[gi[0] % 2]
            gi[0] += 1
            fs = slice(f * 128, (f + 1) * 128)
            for ch in range(8):
                fw.mm(pg[:], wgb[i][:, ch, fs], xn[:, ch, ts_], start=(ch == 0), stop=(ch == 7))
            for ch in range(8):
                fw.mm(pu[:], wub[i][:, ch, fs], xn[:, ch, ts_], start=(ch == 0), stop=(ch == 7))
            fw.act(s_[:], pg[:], AF.Silu)
            fw.tt("dve", s_[:], s_[:], wb[:], ALU.mult)
            fw.tt("dve", hd[:, f, :], s_[:], pu[:], ALU.mult)

    def down(sidx):
        k, tile = steps[sidx]
        i = k % 2
        ts_ = slice(tile * TT, (tile + 1) * TT)
        hd = hid[sidx % 2]
        for j in range(8):
            py = psY[yi[0] % 2]
            yi[0] += 1
            for f in range(3):
                fw.mm(py[:], wdb[i][:, f, j * 128:(j + 1) * 128], hd[:, f, :], start=(f == 0), stop=(f == 2))
            fw.tt("dve", xT[:, j, ts_], xT[:, j, ts_], py[:], ALU.add)
        if tile == NT // TT - 1 and k + 2 < 32:
            load_w((k + 2) // 2, (k + 2) % 2, (k + 2) % 2)

    for sidx in range(len(steps)):
        gateup(sidx)
        if sidx > 0:
            down(sidx - 1)
    down(len(steps) - 1)
    for ch in range(8):
        fw.dma("sp", ho[ch * 128:(ch + 1) * 128, :], xT[:, ch, :], is_output=True)
    return fw


def build_F():
    fw = Fw()
    hT = fw.dram_in("hT", [D, NT])
    g = fw.dram_in("g", [128, 8])
    o = fw.dram_out("o", [D, NT])
    c = consts_common(fw)
    xT = fw.sb("xT", [128, 8, NT], F32)
    for ch in range(8):
        fw.dma("sp", xT[:, ch, :], hT[ch * 128:(ch + 1) * 128, :])
    gs = fw.sb("gs", [128, 8], F32); fw.dma("sp", gs[:], g)
    sq = [fw.sb(f"sq{i}", [128, TT], F32) for i in range(2)]
    rstd = fw.sb("rstd", [128, TT], F32)
    ps_ss = fw.ps("ps_ss", [128, TT], F32)
    un = [fw.sb(f"un{i}", [128, 8, TT], F32) for i in range(2)]
    for tile in range(NT // TT):
        u = un[tile % 2]
        emit_rmsnorm(fw, c, xT, gs, u, tile * TT, TT, sq, ps_ss, rstd)
        for ch in range(8):
            fw.dma("sp", o[ch * 128:(ch + 1) * 128, tile * TT:(tile + 1) * TT], u[:, ch, :], is_output=True)
    return fw


def _lay(g):
    return np.ascontiguousarray(np.asarray(g, np.float32).reshape(8, 128).T)


def _run(fw, in_maps):
    nc = fw.finish()
    res = run_bass_kernel_spmd(nc, in_maps, core_ids=list(range(NCORES)))
    return res.results


def kernel(x, mem, mix_norm_g, ffn_norm_g, mem_norm_g, final_norm_g, w_mem_kv, w_out,
           hy_w_in, hy_short_w, hy_filt_w1, hy_filt_b1, hy_filt_w2, hy_filt_b2, hy_filt_w3,
           hy_filt_freq, hy_skip, at_w_in, at_q_norm_g, at_k_norm_g,
           router_w, exp_w_gate, exp_w_up, exp_w_down):
    f32 = lambda a: np.ascontiguousarray(np.asarray(a), dtype=np.float32)
    x = f32(x); mem = f32(mem)
    h = x.reshape(B * T, D)
    hT = [np.ascontiguousarray(h[c * NT:(c + 1) * NT].T) for c in range(NCORES)]
    memT = [np.ascontiguousarray(mem[b].T) for b in range(B)]
    ident = np.eye(128, dtype=np.float32)
    mconst = m_host_consts()
    aconst = at_host_consts()
    for i in range(4):
        j = i // 2
        hyena = (i % 2 == 0)
        w_in = f32(hy_w_in[j]) if hyena else f32(at_w_in[j])
        Dp = w_in.shape[1]
        g_mix = _lay(mix_norm_g[i])
        res = _run(build_P(Dp), [{"hT": hT[c], "g": g_mix, "w": w_in} for c in range(NCORES)])
        proj = np.concatenate([r["o"].T for r in res], 0).reshape(B, T, Dp)
        if hyena:
            hw = hy_host_weights(f32(hy_short_w[j]), f32(hy_filt_w1[j]), f32(hy_filt_b1[j]), f32(hy_filt_w2[j]),
                                 f32(hy_filt_b2[j]), f32(hy_filt_w3[j]), f32(hy_filt_freq[j]), f32(hy_skip[j]))
            res = _run(build_HY(), [hy_host_inputs(proj, hw, c) for c in range(NCORES)])
            main = hy_host_gather([r["o"] for r in res])
        else:
            gq = f32(at_q_norm_g[j]).reshape(64, 1)
            gk = f32(at_k_norm_g[j]).reshape(64, 1)
            ims = []
            for c in range(NCORES):
                b, g = c // 4, c % 4
                q = proj[b, :, g * 192:(g + 1) * 192].reshape(T, 3, 64)
                d = {"qT": np.ascontiguousarray(q.transpose(1, 2, 0)),
                     "kT": np.ascontiguousarray(proj[b, :, 768 + g * 64:768 + (g + 1) * 64].T),
                     "v": np.ascontiguousarray(proj[b, :, 1024 + g * 64:1024 + (g + 1) * 64]),
                     "gq": gq, "gk": gk}
                d.update(aconst)
                ims.append(d)
            res = _run(build_AT(), ims)
            main = np.empty((B, T, 768), np.float32)
            for c in range(NCORES):
                b, g = c // 4, c % 4
                main[b, :, g * 192:(g + 1) * 192] = res[c]["o"].transpose(2, 0, 1).reshape(T, 192)
        mainf = main.reshape(B * T, 768)
        cqf = proj[:, :, Dp - 256:].reshape(B * T, 256)
        ims = []
        for c in range(NCORES):
            sl = slice(c * NT, (c + 1) * NT)
            ims.append({"hT": hT[c], "mainT": np.ascontiguousarray(mainf[sl].T),
                        "cqT": np.ascontiguousarray(cqf[sl].T.reshape(4, 64, NT)), "memT": memT[c // 4],
                        "g_mem": _lay(mem_norm_g), "w_kv": f32(w_mem_kv[i]), "w_out": f32(w_out[i]),
                        "g_ffn": _lay(ffn_norm_g[i]), "w_r": f32(router_w[i]), "ident": ident})
        res = _run(build_O(), ims)
        hT = [r["ho"] for r in res]
        aff = np.concatenate([r["aff"] for r in res], 0)
        wg_, wu_, wd_ = f32(exp_w_gate[i]), f32(exp_w_up[i]), f32(exp_w_down[i])
        ims = []
        for c in range(NCORES):
            b = c // 4
            ab = aff[b * T:(b + 1) * T]
            d = {"hT": hT[c], "g_ffn": _lay(ffn_norm_g[i]),
                 "affP": np.ascontiguousarray(ab.T.reshape(128, 1024)),
                 "affT": np.ascontiguousarray(aff[c * NT:(c + 1) * NT].T),
                 "wg": wg_, "wu": wu_, "wd": wd_}
            d.update(mconst)
            ims.append(d)
        res = _run(build_M(), ims)
        hT = [r["ho"] for r in res]
    res = _run(build_F(), [{"hT": hT[c], "g": _lay(final_norm_g)} for c in range(NCORES)])
    out = np.concatenate([r["o"].T for r in res], 0).reshape(B, T, D)
    return np.ascontiguousarray(out, dtype=np.float32)
```

```python
from contextlib import ExitStack
import numpy as np
import concourse.bass as bass
import concourse.mybir as mybir
from concourse.bass_utils import run_bass_kernel_spmd

F32 = mybir.dt.float32
BF16 = mybir.dt.bfloat16
I32 = mybir.dt.int32
ALU = mybir.AluOpType
AF = mybir.ActivationFunctionType
AX = mybir.AxisListType

SEM_LIMIT = 16000
N_DMA_SLOTS = 6


def _box(ap):
    t = ap.tensor
    name = t.name
    off = int(ap.offset)
    dims = ap.ap
    space = str(ap.space)
    if space == "DRAM":
        ext = sum((c - 1) * abs(s) for s, c in dims)
        return (name, 0, 1, off, off + ext + 1)
    shp = t.shape
    pstride = 1
    for d in shp[1:]:
        pstride *= int(d)
    p_lo = off // pstride
    f_lo = off % pstride
    pc = 1
    fext = 0
    for s, c in dims:
        if s == pstride and c > 1:
            pc = c
        elif s >= pstride and c > 1:
            pc = max(pc, (c - 1) * (s // pstride) + 1)
        else:
            fext += (c - 1) * abs(s)
    return (name, p_lo, p_lo + pc, f_lo, f_lo + fext + 1)


def _overlap(a, b):
    return a[1] < b[2] and b[1] < a[2] and a[3] < b[4] and b[3] < a[4]


class Fw:
    ENGS = ("pe", "act", "dve", "pool", "sp")

    def __init__(self, name="k"):
        self.nc = bass.Bass("TRN2", target_bir_lowering=False)
        self.stack = ExitStack()
        self.prog = {e: [] for e in self.ENGS}
        self.cur = {}
        self.waited = {e: {} for e in self.ENGS}
        self.acc = {}
        self.nsem = 0
        self.sems = {}
        self.slots = {}
        self.slot_i = {}
        self.n_instr = 0
        self.out_tokens = []

    def dram_in(self, name, shape, dt=F32):
        return self.nc.dram_tensor(name, list(shape), dt, kind="ExternalInput").ap()

    def dram_out(self, name, shape, dt=F32):
        return self.nc.dram_tensor(name, list(shape), dt, kind="ExternalOutput").ap()

    def dram_tmp(self, name, shape, dt=F32):
        return self.nc.dram_tensor(name, list(shape), dt, kind="Internal").ap()

    def sb(self, name, shape, dt=F32):
        return self.stack.enter_context(self.nc.sbuf_tensor(name, list(shape), dt))

    def ps(self, name, shape, dt=F32):
        return self.stack.enter_context(self.nc.psum_tensor(name, list(shape), dt))

    def _newsem(self):
        self.nsem += 1
        s = self.stack.enter_context(self.nc.semaphore(f"s{self.nsem}"))
        self.sems[id(s)] = s
        return s

    def _deps(self, eng, reads, writes):
        toks = []
        for ap, is_w in [(a, False) for a in reads] + [(a, True) for a in writes]:
            b = _box(ap)
            for rec in self.acc.get(b[0], ()):
                tok, rw, rb, reng = rec
                if not (is_w or rw):
                    continue
                if not _overlap(b, rb):
                    continue
                if reng == "pe" and eng == "pe":
                    continue
                toks.append(tok)
        return toks

    def _record(self, eng, tok, reads, writes):
        for ap, is_w in [(a, False) for a in reads] + [(a, True) for a in writes]:
            b = _box(ap)
            lst = self.acc.setdefault(b[0], [])
            new = []
            for rec in lst:
                _, rw, rb, reng = rec
                if rb == b and reng == eng and rw == is_w and not eng.startswith("dma"):
                    continue
                if is_w and rb[1] >= b[1] and rb[2] <= b[2] and rb[3] >= b[3] and rb[4] <= b[4]:
                    continue
                new.append(rec)
            new.append((tok, is_w, b, eng))
            self.acc[b[0]] = new

    def _waits(self, eng, toks):
        w = {}
        for sem, val in toks:
            k = id(sem)
            if self.waited[eng].get(k, 0) >= val:
                continue
            if w.get(k, (None, 0))[1] < val:
                w[k] = (sem, val)
        for k, (sem, val) in w.items():
            self.waited[eng][k] = val
        return list(w.values())

    def _next_tok(self, eng):
        c = self.cur.get(eng)
        if c is None or c[1] >= SEM_LIMIT:
            c = [self._newsem(), 0]
            self.cur[eng] = c
        c[1] += 1
        return (c[0], c[1])

    def op(self, eng, fn, reads, writes):
        toks = self._deps(eng, reads, writes)
        waits = self._waits(eng, toks)
        tok = self._next_tok(eng)
        self.prog[eng].append((waits, fn, tok[0], 1))
        self._record(eng, tok, reads, writes)
        self.n_instr += 1
        return tok

    def dma(self, q, out, in_, is_output=False, **kw):
        toks = self._deps("dma" + q, [in_], [out])
        if q not in self.slots:
            self.slots[q] = [[self._newsem(), 0] for _ in range(N_DMA_SLOTS)]
        sl = self.slots[q]
        i = self.slot_i.get(q, 0)
        self.slot_i[q] = i + 1
        s = sl[i % N_DMA_SLOTS]
        if s[1] + 16 > SEM_LIMIT:
            toks.append((s[0], s[1]))
            s = [self._newsem(), 0]
            sl[i % N_DMA_SLOTS] = s
        if s[1] > 0:
            toks.append((s[0], s[1]))
        waits = self._waits(q, toks)
        s[1] += 16
        tok = (s[0], s[1])
        self.prog[q].append((waits, lambda e: e.dma_start(out=out, in_=in_, **kw), tok[0], 16))
        self._record("dma" + q, tok, [in_], [out])
        if is_output:
            self.out_tokens.append(tok)
        self.n_instr += 1
        return tok

    def mm(self, out, lhsT, rhs, start=True, stop=True):
        return self.op("pe", lambda e: e.matmul(out, lhsT, rhs, start=start, stop=stop), [lhsT, rhs], [out])

    def transpose(self, out, in_, ident):
        return self.op("pe", lambda e: e.transpose(out, in_, ident), [in_, ident], [out])

    def act(self, out, in_, func, bias=None, scale=None, accum_out=None, eng="act"):
        kw = {}
        rd = [in_]
        wr = [out]
        if bias is not None:
            kw["bias"] = bias
            if not isinstance(bias, (int, float)):
                rd.append(bias)
        if scale is not None:
            kw["scale"] = scale
            if not isinstance(scale, (int, float)):
                rd.append(scale)
        if accum_out is not None:
            kw["accum_out"] = accum_out
            wr.append(accum_out)
        return self.op("act", lambda e: e.activation(out, in_, func, **kw), rd, wr)

    def tt(self, eng, out, a, b, op):
        return self.op(eng, lambda e: e.tensor_tensor(out, a, b, op), [a, b], [out])

    def ts(self, eng, out, a, s1, s2=None, op0=ALU.mult, op1=None, accum_out=None):
        rd = [a]
        wr = [out]
        if not isinstance(s1, (int, float)):
            rd.append(s1)
        if s2 is not None and not isinstance(s2, (int, float)):
            rd.append(s2)
        kw = {}
        if op1 is not None:
            kw["op1"] = op1
        if accum_out is not None:
            kw["accum_out"] = accum_out
            wr.append(accum_out)
        return self.op(eng, lambda e: e.tensor_scalar(out, a, s1, s2, op0, **kw), rd, wr)

    def stt(self, eng, out, a, s, b, op0, op1):
        rd = [a, b]
        if not isinstance(s, (int, float)):
            rd.append(s)
        return self.op(eng, lambda e: e.scalar_tensor_tensor(out, a, s, b, op0, op1), rd, [out])

    def copy(self, eng, out, in_):
        if eng == "act":
            return self.op("act", lambda e: e.copy(out, in_), [in_], [out])
        return self.op(eng, lambda e: e.tensor_copy(out, in_), [in_], [out])

    def memset(self, eng, out, val):
        return self.op(eng, lambda e: e.memset(out, val), [], [out])

    def reduce(self, eng, out, in_, op, axis=AX.X):
        return self.op(eng, lambda e: e.tensor_reduce(out, in_, axis, op), [in_], [out])

    def finish(self):
        if self.out_tokens:
            waits = self._waits("sp", self.out_tokens)
            self.prog["sp"].append((waits, None, None, 0))
        nc = self.nc
        prog = self.prog

        def emit(e, lst):
            for waits, fn, sem, inc in lst:
                for s, v in waits:
                    e.wait_ge(s, v)
                if fn is not None:
                    fn(e).then_inc(sem, inc)

        with nc.Block() as block:
            @block.tensor
            def _(e):
                emit(e, prog["pe"])

            @block.scalar
            def _(e):
                emit(e, prog["act"])

            @block.vector
            def _(e):
                emit(e, prog["dve"])

            @block.gpsimd
            def _(e):
                emit(e, prog["pool"])

            @block.sync
            def _(e):
                emit(e, prog["sp"])
        self.stack.close()
        return nc


def run(fw, in_maps, n=8, trace=False):
    nc = fw.finish()
    res = run_bass_kernel_spmd(nc, in_maps, core_ids=list(range(n)), trace=trace)
    return res

D = 1024
B = 2
T = 8192
NT = 2048
TT = 512
EPS = 1e-6
NCORES = 8


def consts_common(fw):
    c = {}
    c["ones"] = fw.sb("ones", [128, 128], F32)
    fw.memset("dve", c["ones"][:], 1.0)
    c["eps"] = fw.sb("eps", [128, 1], F32)
    fw.memset("dve", c["eps"][:], EPS)
    return c


def emit_rmsnorm(fw, c, xT, g_sb, uT, t0, tw, sq, ps_ss, rstd, out_dt_scale=None):
    for ch in range(8):
        s = sq[ch % 2]
        eng = "act" if ch % 2 == 0 else "pool"
        if eng == "act":
            fw.act(s[:, :tw], xT[:, ch, t0:t0 + tw], AF.Square)
        else:
            fw.tt("pool", s[:, :tw], xT[:, ch, t0:t0 + tw], xT[:, ch, t0:t0 + tw], ALU.mult)
        fw.mm(ps_ss[:, :tw], c["ones"][:], s[:, :tw], start=(ch == 0), stop=(ch == 7))
    fw.act(rstd[:, :tw], ps_ss[:, :tw], AF.Sqrt, bias=c["eps"][:], scale=1.0 / D)
    fw.op("dve", lambda e: e.reciprocal(rstd[:, :tw], rstd[:, :tw]), [rstd[:, :tw]], [rstd[:, :tw]])
    for ch in range(8):
        fw.stt("dve", uT[:, ch, :tw], xT[:, ch, t0:t0 + tw], g_sb[:, ch:ch + 1], rstd[:, :tw], ALU.mult, ALU.mult)


def rmsnorm_nodve_pieces(fw, c, xT, g_sb, uT, t0, tw, sq, ps_ss, rstd, tmp):
    def stats():
        for ch in range(8):
            s = sq[ch % 2]
            if ch % 2 == 0:
                fw.act(s[:, :tw], xT[:, ch, t0:t0 + tw], AF.Square)
            else:
                fw.tt("pool", s[:, :tw], xT[:, ch, t0:t0 + tw], xT[:, ch, t0:t0 + tw], ALU.mult)
            fw.mm(ps_ss[:, :tw], c["ones"][:], s[:, :tw], start=(ch == 0), stop=(ch == 7))
        fw.act(rstd[:, :tw], ps_ss[:, :tw], AF.Ln, bias=c["eps"][:], scale=1.0 / D)
        fw.act(rstd[:, :tw], rstd[:, :tw], AF.Exp, scale=-0.5)

    def apply():
        for ch in range(8):
            t_ = tmp[ch % 2]
            fw.tt("pool", t_[:, :tw], xT[:, ch, t0:t0 + tw], rstd[:, :tw], ALU.mult)
            fw.act(uT[:, ch, :tw], t_[:, :tw], AF.Copy, scale=g_sb[:, ch:ch + 1])
    return [stats, apply]


def emit_proj(fw, uT, w_bf, Dp, out_dram, t0, tw, ps_list, stg_list, ctr):
    for j in range(Dp // 128):
        ps = ps_list[ctr[0] % len(ps_list)]
        stg = stg_list[ctr[0] % len(stg_list)]
        for ch in range(8):
            fw.mm(ps[:, :tw], w_bf[:, ch, j * 128:(j + 1) * 128], uT[:, ch, :tw], start=(ch == 0), stop=(ch == 7))
        fw.copy("act" if ctr[0] % 2 == 0 else "dve", stg[:, :tw], ps[:, :tw])
        fw.dma("sp", out_dram[j * 128:(j + 1) * 128, t0:t0 + tw], stg[:, :tw], is_output=True)
        ctr[0] += 1


def build_P(Dp):
    fw = Fw()
    hT = fw.dram_in("hT", [D, NT])
    g = fw.dram_in("g", [128, 8])
    w = fw.dram_in("w", [D, Dp])
    o = fw.dram_out("o", [Dp, NT])
    c = consts_common(fw)
    xT = fw.sb("xT", [128, 8, NT], F32)
    g_sb = fw.sb("g_sb", [128, 8], F32)
    w_bf = fw.sb("w_bf", [128, 8, Dp], BF16)
    sq = [fw.sb(f"sq{i}", [128, TT], F32) for i in range(2)]
    rstd = fw.sb("rstd", [128, TT], F32)
    uT = [fw.sb(f"uT{i}", [128, 8, TT], BF16) for i in range(2)]
    ps_ss = fw.ps("ps_ss", [128, TT], F32)
    ps_list = [fw.ps(f"ps{i}", [128, TT], F32) for i in range(4)]
    stg_list = [fw.sb(f"stg{i}", [128, TT], F32) for i in range(4)]
    fw.dma("sp", g_sb[:], g)
    for ti in range(NT // TT):
        for ch in range(8):
            fw.dma("sp", xT[:, ch, ti * TT:(ti + 1) * TT], hT[ch * 128:(ch + 1) * 128, ti * TT:(ti + 1) * TT])
    for ch in range(8):
        fw.dma("pool", w_bf[:, ch, :], w[ch * 128:(ch + 1) * 128, :])
    ctr = [0]
    for ti in range(NT // TT):
        u = uT[ti % 2]
        emit_rmsnorm(fw, c, xT, g_sb, u, ti * TT, TT, sq, ps_ss, rstd)
        emit_proj(fw, u, w_bf, Dp, o, ti * TT, TT, ps_list, stg_list, ctr)
    return fw


HG = 8
HNG = 12
HCH = 96
TWO_PI = 6.283185307179586
MAGIC = 12582912.0


def hy_host_consts():
    n = np.arange(128)
    F = np.exp(-2j * np.pi * np.outer(n, n) / 128.0)
    Tw = np.exp(-2j * np.pi * np.outer(n, n) / 16384.0)
    f32 = lambda a: np.ascontiguousarray(a, dtype=np.float32)
    c = {}
    c["F1a"] = f32(np.concatenate([F.real, F.imag], 1)[:64])
    c["F1b"] = f32(np.concatenate([-F.imag, F.real], 1)[:64])
    c["F2re"] = f32(F.real)
    c["F2im"] = f32(F.imag)
    c["nF2im"] = f32(-F.imag)
    c["TT"] = f32(np.concatenate([Tw.real] * 4, 1))
    c["Tim"] = f32(Tw.imag)
    c["nTim"] = f32(-Tw.imag)
    c["Gc"] = f32(np.concatenate([F.real, -F.imag], 1))
    c["Gd"] = f32(np.concatenate([F.imag, F.real], 1))
    c["iF1re"] = f32(F.real[:, :64] / 16384.0)
    c["iF1im"] = f32(F.imag[:, :64] / 16384.0)
    c["niF1im"] = f32(-F.imag[:, :64] / 16384.0)
    return c


HY_CONST_SHAPES = {"F1a": [64, 256], "F1b": [64, 256], "F2re": [128, 128], "F2im": [128, 128], "nF2im": [128, 128],
                   "TT": [128, 512], "Tim": [128, 128], "nTim": [128, 128], "Gc": [128, 256], "Gd": [128, 256],
                   "iF1re": [128, 64], "iF1im": [128, 64], "niF1im": [128, 64]}


def build_HY():
    fw = Fw()
    G, NG, CH = HG, HNG, HCH
    W = 2 * G * 128
    U = fw.dram_in("U", [NG, 3, 3, 64, W])
    featsT = fw.dram_in("featsT", [33, 8192])
    w1 = fw.dram_in("w1", [33, 64])
    w2 = fw.dram_in("w2", [64, 64])
    w3 = fw.dram_in("w3", [NG, 64, 4 * G])
    b1 = fw.dram_in("b1", [64, 1])
    b2 = fw.dram_in("b2", [64, 1])
    fr = fw.dram_in("fr", [64, 1])
    decay = fw.dram_in("decay", [NG, 64, G * 128])
    sw = fw.dram_in("sw", [64, 9 * CH])
    skip = fw.dram_in("skip", [64, 2 * CH])
    o = fw.dram_out("o", [NG, 64, W])
    cd = {k: fw.dram_in("c_" + k, shp) for k, shp in HY_CONST_SHAPES.items()}
    c = consts_common(fw)
    T1 = [fw.sb(f"T1_{i}", [128, 512], F32) for i in range(4)]
    T2 = [fw.sb(f"T2_{i}", [128, 512], F32) for i in range(4)]
    K = {}
    BF_CONSTS = ("F2re", "F2im", "nF2im", "Gc", "Gd", "iF1re", "iF1im", "niF1im")
    for n_, (k, shp) in enumerate(HY_CONST_SHAPES.items()):
        if k in BF_CONSTS:
            stg = T1[n_ % 4] if n_ % 2 == 0 else T2[n_ % 4]
            sv = stg[0:shp[0], 0:shp[1]]
            fw.dma("sp", sv, cd[k])
            K[k] = fw.sb("k_" + k, shp, BF16)
            fw.copy("dve", K[k][:], sv)
        else:
            K[k] = fw.sb("k_" + k, shp, F32)
            fw.dma("sp", K[k][:], cd[k])
    w1s = fw.sb("w1s", [33, 64]); fw.dma("sp", w1s[:], w1)
    w2s = fw.sb("w2s", [64, 64]); fw.dma("sp", w2s[:], w2)
    b1s = fw.sb("b1s", [64, 1]); fw.dma("sp", b1s[:], b1)
    b2s = fw.sb("b2s", [64, 1]); fw.dma("sp", b2s[:], b2)
    frs = fw.sb("frs", [64, 1]); fw.dma("sp", frs[:], fr)
    sws = fw.sb("sws", [64, 9 * CH]); fw.dma("sp", sws[:], sw)
    sks = fw.sb("sks", [64, 2 * CH]); fw.dma("sp", sks[:], skip)
    frb1 = fw.sb("frb1", [64, 1]); fw.tt("dve", frb1[:], frs[:], b1s[:], ALU.mult)
    frb2 = fw.sb("frb2", [64, 1]); fw.tt("dve", frb2[:], frs[:], b2s[:], ALU.mult)
    h2T = fw.sb("h2T", [64, 8192], F32)
    scr = fw.sb("scr", [64, 4096], F32)
    ft = [scr[0:33, 0:512], scr[0:33, 512:1024]]
    zt = scr[:, 1024:1536]
    rt = scr[:, 1536:2048]
    h1t = scr[:, 2048:2560]
    psR = [fw.ps(f"psR{i}", [128, 512], F32) for i in range(7)]
    psM = fw.ps("psM", [128, 512], F32)
    ring = [0]

    def nps():
        r = psR[ring[0] % 7]
        ring[0] += 1
        return r

    def sin_layer(ps, frb, dst):
        fw.ts("dve", zt, ps, frs[:, 0:1], frb[:, 0:1], op0=ALU.mult, op1=ALU.add)
        fw.ts("dve", rt, zt, 1.0 / TWO_PI, MAGIC, op0=ALU.mult, op1=ALU.add)
        fw.ts("dve", rt, rt, MAGIC, TWO_PI, op0=ALU.subtract, op1=ALU.mult)
        fw.tt("dve", zt, zt, rt, ALU.subtract)
        fw.act(dst, zt, AF.Sin, scale=1.0 - 1e-6)

    for ti in range(16):
        f = ft[ti % 2]
        fw.dma("sp", f, featsT[:, ti * 512:(ti + 1) * 512])
        fw.mm(psM[0:64, :], w1s[:], f)
        sin_layer(psM[0:64, :], frb1, h1t)
        fw.mm(psM[0:64, :], w2s[:], h1t)
        sin_layer(psM[0:64, :], frb2, h2T[:, ti * 512:(ti + 1) * 512])

    w3s = fw.sb("w3s", [64, 4 * G], F32)
    dec = fw.sb("dec", [64, G, 128], F32)
    hf = fw.sb("hf", [64, 4 * G, 128], F32)
    habs_v = scr[:].rearrange("p (a n) -> p a n", a=4 * G)
    rs = fw.sb("rs", [64, 4 * G], F32)
    dsum = fw.sb("dsum", [128, 4 * G], F32)
    rden = fw.sb("rden", [128, 2 * G], F32)
    kk = fw.sb("kk", [128, 2, G, 512], F32)
    Xs = [fw.sb(f"Xs{i}", [128, 512], F32) for i in range(2)]
    sd = [fw.sb(f"sd{i}", [128, 256], F32) for i in range(2)]
    ush2 = fw.sb("ush2", [64, 2, G, 128], F32)
    Ushv = [scr[:, 0:2048].rearrange("p (b c n) -> p b c n", b=2, c=G),
            scr[:, 2048:4096].rearrange("p (b c n) -> p b c n", b=2, c=G), ush2[:]]
    uc = [fw.sb(f"uc{s}", [64, 2, G, 128], F32) for s in range(3)]
    OPb = [fw.sb(f"OP{i}", [128, 512], BF16) for i in range(6)]
    gtl = [fw.sb(f"gt{i}", [64, 2, 2, 128], F32) for i in range(4)]
    cnt = {"t": 0, "op": 0, "x": 0, "g": 0}

    def vw(ap, lay):
        if lay == "crk":
            return ap.rearrange("p (c r k) -> p c r k", c=2, r=2)
        return ap.rearrange("p (r c k) -> p c r k", c=2, r=2)

    def cmul(ps, lin, mulP1, mulRe, mulIm, lout):
        a, b = T1[cnt["t"] % 4], T2[cnt["t"] % 4]
        cnt["t"] += 1
        dst = OPb[cnt["op"] % 6]
        cnt["op"] += 1
        p4 = vw(ps, lin)
        fw.tt("dve", vw(a[:], lout), p4, mulP1, ALU.mult)
        fw.tt("dve", vw(b[:], lout)[:, :, 0, :], p4[:, :, 1, :], mulRe, ALU.mult)
        fw.tt("dve", vw(b[:], lout)[:, :, 1, :], p4[:, :, 0, :], mulIm, ALU.mult)
        fw.tt("pool", dst[:], a[:], b[:], ALU.add)
        return dst

    TT4 = K["TT"][:].rearrange("p (c r k) -> p c r k", c=2, r=2)
    Tim_b = K["Tim"][:].unsqueeze(1).broadcast_to([128, 2, 128])
    nTim_b = K["nTim"][:].unsqueeze(1).broadcast_to([128, 2, 128])

    def fwd_s2(a):
        px = nps()
        fw.mm(px[:, 0:256], K["F2re"][:], a[:, 0:256], start=True, stop=False)
        fw.mm(px[:, 0:256], K["nF2im"][:], a[:, 256:512], start=False, stop=True)
        fw.mm(px[:, 256:512], K["F2re"][:], a[:, 256:512], start=True, stop=False)
        fw.mm(px[:, 256:512], K["F2im"][:], a[:, 0:256], start=False, stop=True)
        return px

    for g in range(NG):
        c0 = g * G
        fw.dma("sp", w3s[:], w3[g])
        fw.dma("sp", dec[:], decay[g].rearrange("p (c n) -> p c n", c=G))
        for nb in range(8):
            pl3 = nps()
            for i in range(16):
                n2 = nb * 16 + i
                fw.mm(pl3[0:64, i * 32:(i + 1) * 32], h2T[:, n2 * 64:(n2 + 1) * 64], w3s[:])
            pin = pl3[0:64, :].rearrange("p (n a c) -> p n a c", n=16, a=4)
            dv = dec[:, :, nb * 16:(nb + 1) * 16].rearrange("p c n -> p n c").unsqueeze(2).broadcast_to([64, 16, 4, G])
            ov = hf[:, :, nb * 16:(nb + 1) * 16].rearrange("p (a c) n -> p n a c", a=4)
            fw.tt("dve", ov, pin, dv, ALU.mult)
        fw.act(habs_v, hf[:], AF.Abs)
        fw.reduce("dve", rs[:], habs_v, ALU.add)
        fw.mm(psM[:, 0:4 * G], c["ones"][0:64, :], rs[:])
        fw.copy("act", dsum[:], psM[:, 0:4 * G])
        d4 = dsum[:].rearrange("p (o d c) -> p o d c", o=2, d=2)
        r3 = rden[:].rearrange("p (o c) -> p o c", o=2)
        fw.tt("dve", r3, d4[:, :, 0, :], d4[:, :, 1, :], ALU.add)
        fw.ts("dve", rden[:], rden[:], 1e-6, None, op0=ALU.add)
        fw.op("dve", lambda e: e.reciprocal(rden[:], rden[:]), [rden[:]], [rden[:]])
        for s in range(3):
            for j in range(3):
                fw.dma("sp", Ushv[j], U[g, s, j].rearrange("p (b c n) -> p b c n", b=2, c=G))
            def wsc(j, cc):
                col = (s * 3 + j) * CH + c0 + cc
                return sws[:, col:col + 1]
            for cc in range(G):
                fw.act(uc[s][:, :, cc, :], Ushv[0][:, :, cc, :], AF.Copy, scale=wsc(0, cc))
            for j in (1, 2):
                for cc in range(G):
                    acc = uc[s][:, :, cc, :]
                    fw.stt("dve", acc, Ushv[j][:, :, cc, :], wsc(j, cc), acc, ALU.mult, ALU.add)
        ocs = [(oo, cc) for oo in range(2) for cc in range(G)]
        for q0 in range(0, len(ocs), 4):
            wave = ocs[q0:q0 + 4]
            pas = []
            for (oo, cc) in wave:
                pa = nps()
                for d in range(2):
                    col = (oo * 2 + d) * G + cc
                    fw.mm(pa[:, d * 256:(d + 1) * 256], hf[:, col, :], K["F1a"][:])
                pas.append(pa)
            aps = [cmul(pa[:], "crk", TT4, nTim_b, Tim_b, "rck") for pa in pas]
            pxs = [fwd_s2(a) for a in aps]
            for (oo, cc), px in zip(wave, pxs):
                xs_, sd_ = Xs[cnt["x"] % 2], sd[cnt["x"] % 2]
                cnt["x"] += 1
                fw.copy("act", xs_[:], px[:])
                fw.tt("pool", sd_[:, 0:128], xs_[:, 0:128], xs_[:, 128:256], ALU.add)
                fw.tt("pool", sd_[:, 128:256], xs_[:, 256:384], xs_[:, 384:512], ALU.subtract)
                rsc = rden[:, oo * G + cc:oo * G + cc + 1]
                kv = kk[:, oo, cc, :]
                fw.ts("dve", kv[:, 0:256].rearrange("p (r k) -> p r k", r=2),
                      sd_[:, 0:128].unsqueeze(1).broadcast_to([128, 2, 128]), rsc, None, op0=ALU.mult)
                fw.ts("dve", kv[:, 384:512], sd_[:, 128:256], rsc, None, op0=ALU.mult)
                fw.ts("dve", kv[:, 256:384], sd_[:, 128:256], rsc, -1.0, op0=ALU.mult, op1=ALU.mult)
        for oo in range(2):
            zin = uc[2]
            gate = uc[0] if oo == 0 else uc[1]
            zout = uc[2]
            prs = list(range(G // 2))
            pas = []
            for p in prs:
                pa = nps()
                for ci in range(2):
                    cc = 2 * p + ci
                    fw.mm(pa[:, ci * 256:(ci + 1) * 256], zin[:, 0, cc, :], K["F1a"][:], start=True, stop=False)
                    fw.mm(pa[:, ci * 256:(ci + 1) * 256], zin[:, 1, cc, :], K["F1b"][:], start=False, stop=True)
                pas.append(pa)
            aps = [cmul(pa[:], "crk", TT4, nTim_b, Tim_b, "rck") for pa in pas]
            pxs = [fwd_s2(a) for a in aps]
            yps = []
            for p, px in zip(prs, pxs):
                kp = kk[:, oo, 2 * p:2 * p + 2, :]
                yps.append(cmul(px[:], "rck", kp[:, :, 0:256].rearrange("p c (r k) -> p c r k", r=2),
                                kp[:, :, 256:384], kp[:, :, 384:512], "crk"))
            pbs = []
            for yp in yps:
                pb = nps()
                y4 = vw(yp[:], "crk")
                for ci in range(2):
                    fw.mm(pb[:, ci * 256:(ci + 1) * 256], y4[:, ci, 0, :], K["Gc"][:], start=True, stop=False)
                    fw.mm(pb[:, ci * 256:(ci + 1) * 256], y4[:, ci, 1, :], K["Gd"][:], start=False, stop=True)
                pbs.append(pb)
            bps = [cmul(pb[:], "crk", TT4, Tim_b, nTim_b, "rck") for pb in pbs]
            pys = []
            for bp in bps:
                py = nps()
                yv = py[0:64, :].rearrange("p (b c n) -> p b c n", b=2, c=2)
                fw.mm(yv[:, 0, :, :], K["iF1re"][:], bp[:, 0:256], start=True, stop=False)
                fw.mm(yv[:, 0, :, :], K["iF1im"][:], bp[:, 256:512], start=False, stop=True)
                fw.mm(yv[:, 1, :, :], K["iF1re"][:], bp[:, 256:512], start=True, stop=False)
                fw.mm(yv[:, 1, :, :], K["niF1im"][:], bp[:, 0:256], start=False, stop=True)
                pys.append(py)
            for p, py in zip(prs, pys):
                yv = py[0:64, :].rearrange("p (b c n) -> p b c n", b=2, c=2)
                gt = gtl[cnt["g"] % 4]
                cnt["g"] += 1
                base = oo * CH + c0 + 2 * p
                for ci in range(2):
                    fw.act(gt[:, :, ci, :], zin[:, :, 2 * p + ci, :], AF.Copy, scale=sks[:, base + ci:base + ci + 1])
                fw.tt("dve", gt[:], yv, gt[:], ALU.add)
                fw.tt("pool", zout[:, :, 2 * p:2 * p + 2, :], gate[:, :, 2 * p:2 * p + 2, :], gt[:], ALU.mult)
        fw.dma("sp", o[g].rearrange("p (b c n) -> p b c n", b=2, c=G), uc[2][:], is_output=True)
    return fw


def hy_host_inputs(proj, hy_w, core):
    G, NG, CH = HG, HNG, HCH
    ch0 = core * CH
    Umat = np.empty((NG, 3, 3, 64, 2 * G * 128), np.float32)
    for s in range(3):
        a = proj[:, :, s * 768 + ch0: s * 768 + ch0 + CH]
        ap = np.pad(a, ((0, 0), (1, 1), (0, 0)))
        for j in range(3):
            sh = ap[:, j:j + T, :]
            x = sh.transpose(0, 2, 1).reshape(B, NG, G, 64, 128)
            Umat[:, s, j] = x.transpose(1, 3, 0, 2, 4).reshape(NG, 64, 2 * G * 128)
    d = {"U": Umat}
    d.update(hy_w[core])
    return d


def hy_host_weights(short_w, w1, b1, w2, b2, w3, freq, skip):
    G, NG, CH = HG, HNG, HCH
    L = T
    m = np.arange(L, dtype=np.float64)
    tt_ = m / (L - 1)
    wv = 2.0 * np.pi * m / L
    bands = np.linspace(1e-4, 15.0, 16)
    feats = np.concatenate([tt_[:, None], np.cos(bands[None] * wv[:, None]), -np.sin(bands[None] * wv[:, None])], 1)
    perm = (np.arange(64)[None, :] * 128 + np.arange(128)[:, None]).reshape(-1)
    featsT = np.ascontiguousarray(feats[perm].T, dtype=np.float32)
    max_decay = np.log(1e-2) / 0.3
    min_decay = np.log(1e-2) / 1.5
    deltas = np.abs(np.linspace(min_decay, max_decay, 768))
    dec_full = np.exp(-tt_[:, None] * deltas[None, :])
    consts = hy_host_consts()
    outs = []
    w3r = w3.reshape(64, 2, 2, 768)
    for core in range(NCORES):
        ch0 = core * CH
        d = {"featsT": featsT, "w1": np.ascontiguousarray(w1), "w2": np.ascontiguousarray(w2),
             "b1": np.ascontiguousarray(b1.reshape(64, 1)), "b2": np.ascontiguousarray(b2.reshape(64, 1)),
             "fr": np.ascontiguousarray(freq.reshape(64, 1))}
        w3c = w3r[:, :, :, ch0:ch0 + CH].reshape(64, 2, 2, NG, G)
        d["w3"] = np.ascontiguousarray(w3c.transpose(3, 0, 1, 2, 4).reshape(NG, 64, 4 * G))
        dc = dec_full[:, ch0:ch0 + CH].reshape(64, 128, NG, G)
        d["decay"] = np.ascontiguousarray(dc.transpose(2, 0, 3, 1).reshape(NG, 64, G * 128), dtype=np.float32)
        swc = short_w.reshape(3, 3, 768)[:, :, ch0:ch0 + CH]
        swl = swc.transpose(1, 0, 2).reshape(1, 9 * CH)
        d["sw"] = np.ascontiguousarray(np.broadcast_to(swl, (64, 9 * CH)))
        skl = skip[:, ch0:ch0 + CH].reshape(1, 2 * CH)
        d["skip"] = np.ascontiguousarray(np.broadcast_to(skl, (64, 2 * CH)))
        for k, v in consts.items():
            d["c_" + k] = v
        outs.append(d)
    return outs


def hy_host_gather(results):
    G, NG, CH = HG, HNG, HCH
    main = np.empty((B, T, 768), np.float32)
    for core, r in enumerate(results):
        x = r.reshape(NG, 64, B, G, 128)
        x = x.transpose(2, 1, 4, 0, 3).reshape(B, T, CH)
        main[:, :, core * CH:(core + 1) * CH] = x
    return main


def at_host_consts():
    rows = T // 64
    pos_row = np.repeat(np.arange(rows), 64).astype(np.float64)
    pos_col = np.tile(np.arange(64), rows).astype(np.float64)
    inv = 1.0 / (10000.0 ** (np.arange(0, 32, 2, dtype=np.float64) / 32))
    ang = np.stack([pos_row[:, None] * inv, pos_col[:, None] * inv], 1)
    d = np.arange(64)
    axis = d // 32
    f = d % 16
    cosT = np.cos(ang[:, axis, f]).T
    sinT = np.sin(ang[:, axis, f]).T
    R = np.zeros((64, 64))
    for a in range(2):
        for ff in range(16):
            i0 = a * 32 + ff
            i1 = a * 32 + 16 + ff
            R[i0, i1] = -1.0
            R[i1, i0] = 1.0
    f32 = lambda a: np.ascontiguousarray(a, dtype=np.float32)
    return {"cosT": f32(cosT), "sinT": f32(sinT), "rotT": f32(R.T)}


def build_AT():
    fw = Fw()
    qT = fw.dram_in("qT", [3, 64, T])
    kT = fw.dram_in("kT", [64, T])
    v = fw.dram_in("v", [T, 64])
    gq = fw.dram_in("gq", [64, 1])
    gk = fw.dram_in("gk", [64, 1])
    cosT = fw.dram_in("cosT", [64, T])
    sinT = fw.dram_in("sinT", [64, T])
    rotT = fw.dram_in("rotT", [64, 64])
    o = fw.dram_out("o", [3, 64, T])
    c = consts_common(fw)
    cs = fw.sb("cs", [64, T], F32); fw.dma("sp", cs[:], cosT)
    sn = fw.sb("sn", [64, T], F32); fw.dma("sp", sn[:], sinT)
    rot = fw.sb("rot", [64, 64], F32); fw.dma("sp", rot[:], rotT)
    gqs = fw.sb("gqs", [64, 1], F32); fw.dma("sp", gqs[:], gq)
    gks = fw.sb("gks", [64, 1], F32); fw.dma("sp", gks[:], gk)
    qb = [fw.sb(f"qb{h}", [128, T], BF16) for h in range(3)]
    kb = fw.sb("kb", [128, T], BF16)
    for t_ in qb + [kb]:
        fw.memset("pool", t_[64:128, :], 0.0)
    vst = fw.sb("vst", [128, 64, 64], F32)
    va = fw.sb("va", [128, 64, 128], BF16)
    fw.dma("sp", vst[:], v.rearrange("(c p) d -> p c d", p=128))
    fw.memset("pool", va[:, :, 64:128], 0.0)
    fw.copy("dve", va[:, :, 0:64], vst[:])
    fw.memset("dve", va[:, :, 64:65], 1.0)
    psS = [fw.ps(f"psS{i}", [128, TT], F32) for i in range(3)]
    psO = [fw.ps(f"psO{i}", [128, TT], F32) for i in range(2)]
    psA = fw.ps("psA", [128, TT], F32)
    psB = fw.ps("psB", [128, TT], F32)
    xin = [fw.sb(f"xin{i}", [64, TT], F32) for i in range(2)]
    sq = [fw.sb(f"sqa{i}", [64, TT], F32) for i in range(2)]
    rstd = [fw.sb(f"rstda{i}", [64, TT], F32) for i in range(2)]
    xn = [fw.sb(f"xna{i}", [64, TT], F32) for i in range(2)]
    ra = [fw.sb(f"ra{i}", [64, TT], F32) for i in range(2)]
    rb = [fw.sb(f"rb{i}", [64, TT], F32) for i in range(2)]
    pA = [psA[0:64, :], psS[0][0:64, :]]
    pB = [psB[0:64, :], psS[1][0:64, :]]
    jobs = [(kT, gks, kb, ti) for ti in range(T // TT)]
    for h_ in range(3):
        jobs += [(qT[h_], gqs, qb[h_], ti) for ti in range(T // TT)]
    for j0 in range(0, len(jobs), 2):
        pair = list(enumerate(jobs[j0:j0 + 2]))
        sls = [slice(ti * TT, (ti + 1) * TT) for _, (_, _, _, ti) in pair]
        for i, (src, g, dst, ti) in pair:
            fw.dma("sp", xin[i][:], src[:, sls[i]])
        for i, _ in pair:
            fw.act(sq[i][:], xin[i][:], AF.Square)
        for i, _ in pair:
            fw.mm(pA[i], c["ones"][0:64, 0:64], sq[i][:])
        for i, _ in pair:
            fw.act(rstd[i][:], pA[i], AF.Sqrt, bias=c["eps"][0:64, :], scale=1.0 / 64)
        for i, _ in pair:
            r_ = rstd[i]
            fw.op("dve", lambda e, r_=r_: e.reciprocal(r_[:], r_[:]), [r_[:]], [r_[:]])
        for i, (src, g, dst, ti) in pair:
            fw.stt("dve", xn[i][:], xin[i][:], g[:, 0:1], rstd[i][:], ALU.mult, ALU.mult)
        for i, _ in pair:
            fw.mm(pB[i], rot[:], xn[i][:])
        for i, _ in pair:
            fw.tt("dve", rb[i][:], pB[i], sn[:, sls[i]], ALU.mult)
        for i, _ in pair:
            fw.tt("pool", ra[i][:], xn[i][:], cs[:, sls[i]], ALU.mult)
        for i, (src, g, dst, ti) in pair:
            fw.tt("pool", dst[0:64, sls[i]], ra[i][:], rb[i][:], ALU.add)
    pt = [fw.sb(f"pt{i}", [128, TT], BF16) for i in range(3)]
    lsb = fw.sb("lsb", [128, TT], F32)
    rec = fw.sb("rec", [64, TT], F32)
    ost = [fw.sb(f"ost{i}", [64, TT], F32) for i in range(2)]
    it = 0
    blk = 0
    for h in range(3):
        for qi in range(T // TT):
            qs = slice(qi * TT, (qi + 1) * TT)
            po = psO[blk % 2]
            for kc in range(2):
                fw.mm(psS[(it + kc) % 3][:], kb[:, kc * 128:(kc + 1) * 128], qb[h][:, qs])
            for kc in range(64):
                ps = psS[it % 3]
                p = pt[it % 3]
                fw.act(p[:], ps[:], AF.Exp, scale=0.125)
                if kc + 2 < 64:
                    fw.mm(psS[(it + 2) % 3][:], kb[:, (kc + 2) * 128:(kc + 3) * 128], qb[h][:, qs])
                fw.mm(po[:], va[:, kc, :], p[:], start=(kc == 0), stop=(kc == 63))
                it += 1
            fw.copy("act", lsb[64:65, :], po[64:65, :])
            fw.mm(psA[0:64, :], c["ones"][64:65, 0:64], lsb[64:65, :])
            fw.op("dve", lambda e: e.reciprocal(rec[:], psA[0:64, :]), [psA[0:64, :]], [rec[:]])
            os_ = ost[blk % 2]
            fw.tt("dve", os_[:], po[0:64, :], rec[:], ALU.mult)
            fw.dma("sp", o[h, :, qs], os_[:], is_output=True)
            blk += 1
    return fw


def build_O():
    fw = Fw()
    hT = fw.dram_in("hT", [D, NT])
    mainT = fw.dram_in("mainT", [768, NT])
    cqT = fw.dram_in("cqT", [4, 64, NT])
    memT = fw.dram_in("memT", [D, 256])
    g_mem = fw.dram_in("g_mem", [128, 8])
    w_kv = fw.dram_in("w_kv", [D, 512])
    w_out = fw.dram_in("w_out", [D, D])
    g_ffn = fw.dram_in("g_ffn", [128, 8])
    w_r = fw.dram_in("w_r", [D, 16])
    ident = fw.dram_in("ident", [128, 128])
    ho = fw.dram_out("ho", [D, NT])
    affo = fw.dram_out("aff", [NT, 16])
    c = consts_common(fw)
    xT = fw.sb("xT", [128, 8, NT], F32)
    gm = fw.sb("gm", [128, 8], F32); fw.dma("sp", gm[:], g_mem)
    gf = fw.sb("gf", [128, 8], F32); fw.dma("sp", gf[:], g_ffn)
    wr = fw.sb("wr", [128, 8, 16], F32); fw.dma("sp", wr[:], w_r.rearrange("(c p) e -> p c e", p=128))
    idf = fw.sb("idf", [128, 128], F32); fw.dma("sp", idf[:], ident)
    idb = fw.sb("idb", [128, 128], BF16); fw.copy("dve", idb[:], idf[:])
    mT = fw.sb("mT", [128, 8, 256], F32); fw.dma("sp", mT[:], memT.rearrange("(c p) m -> p c m", p=128))
    wkv = fw.sb("wkv", [128, 8, 512], BF16); fw.dma("pool", wkv[:], w_kv.rearrange("(c p) f -> p c f", p=128))
    main_bf = fw.sb("main_bf", [128, 6, NT], BF16); fw.dma("pool", main_bf[:], mainT.rearrange("(c p) t -> p c t", p=128))
    cq_bf = fw.sb("cq_bf", [64, 4, NT], BF16); fw.dma("pool", cq_bf[:], cqT.rearrange("h p t -> p h t"))
    wo_bf = fw.sb("wo_bf", [128, 6, D], BF16); fw.dma("pool", wo_bf[:], w_out[0:768, :].rearrange("(c p) d -> p c d", p=128))
    woc_bf = fw.sb("woc_bf", [128, 2, D], BF16); fw.dma("pool", woc_bf[:], w_out[768:1024, :].rearrange("(c p) d -> p c d", p=128))
    cross_bf = fw.sb("cross_bf", [128, 2, NT], BF16)
    for ti in range(NT // TT):
        for ch in range(8):
            fw.dma("sp", xT[:, ch, ti * TT:(ti + 1) * TT], hT[ch * 128:(ch + 1) * 128, ti * TT:(ti + 1) * TT])
    sq = [fw.sb(f"sq{i}", [128, TT], F32) for i in range(2)]
    rstd = fw.sb("rstd", [128, TT], F32)
    ps_ss = fw.ps("ps_ss", [128, TT], F32)
    psS = [fw.ps(f"psS{i}", [128, TT], F32) for i in range(2)]
    psT = [fw.ps(f"psT{i}", [128, 128], BF16) for i in range(2)]
    psO = fw.ps("psO", [128, TT], F32)
    psP = [fw.ps(f"psP{i}", [128, TT], F32) for i in range(2)]
    memn = fw.sb("memn", [128, 8, 256], BF16)
    emit_rmsnorm(fw, c, mT, gm, memn, 0, 256, sq, ps_ss, rstd)
    mkT = fw.sb("mkT", [64, 4, 256], BF16)
    mv = fw.sb("mv", [128, 2, 256], BF16)
    mvp = fw.sb("mvp", [128, 4, 2, 128], BF16)
    fw.memset("pool", mvp[:], 0.0)
    for h in range(4):
        for ch in range(8):
            fw.mm(psS[0][0:64, 0:256], wkv[:, ch, h * 64:(h + 1) * 64], memn[:, ch, :], start=(ch == 0), stop=(ch == 7))
        fw.copy("act", mkT[:, h, :], psS[0][0:64, 0:256])
    for mc in range(2):
        for ch in range(8):
            fw.mm(psS[1][:, 0:256], memn[:, ch, mc * 128:(mc + 1) * 128], wkv[:, ch, 256:512], start=(ch == 0), stop=(ch == 7))
        fw.copy("act", mv[:, mc, :], psS[1][:, 0:256])
        for h in range(4):
            fw.copy("dve", mvp[:, h, mc, (h % 2) * 64:(h % 2) * 64 + 64], mv[:, mc, h * 64:(h + 1) * 64])
    pe_ = [fw.sb(f"pe{i}", [128, 256], F32) for i in range(4)]
    pn = [fw.sb(f"pn{i}", [128, 256], BF16) for i in range(4)]
    pT = [fw.sb(f"pT{i}", [128, 128], BF16) for i in range(8)]
    st = [fw.sb(f"st{i}", [128, 4], F32) for i in range(4)]
    it = 0
    for tile in range(NT // TT):
        for hp in range(2):
            for qp in range(0, 4, 2):
                units = [(qp + u, 2 * hp + hh, 2 * u + hh) for u in range(2) for hh in range(2)]
                sc = {i: psS[i // 2][:, (i % 2) * 256:(i % 2) * 256 + 256] for _, _, i in units}
                for qb_, h, i in units:
                    q0 = tile * TT + qb_ * 128
                    fw.mm(sc[i], cq_bf[:, h, q0:q0 + 128], mkT[:, h, :])
                for qb_, h, i in units:
                    fw.reduce("dve", st[i][:, 0:1], sc[i], ALU.max)
                for qb_, h, i in units:
                    fw.ts("dve", st[i][:, 1:2], st[i][:, 0:1], -0.125, None, op0=ALU.mult)
                for qb_, h, i in units:
                    fw.act(pe_[i][:], sc[i], AF.Exp, bias=st[i][:, 1:2], scale=0.125)
                for qb_, h, i in units:
                    fw.reduce("dve", st[i][:, 2:3], pe_[i][:], ALU.add)
                for qb_, h, i in units:
                    s_ = st[i]
                    fw.op("dve", lambda e, s_=s_: e.reciprocal(s_[:, 3:4], s_[:, 2:3]), [s_[:, 2:3]], [s_[:, 3:4]])
                for qb_, h, i in units:
                    fw.ts("dve", pn[i][:], pe_[i][:], st[i][:, 3:4], None, op0=ALU.mult)
                tc = 0
                for mc in range(2):
                    for qb_, h, i in units:
                        fw.transpose(psT[tc % 2][:], pn[i][:, mc * 128:(mc + 1) * 128], idb[:])
                        fw.copy("act", pT[2 * i + mc][:], psT[tc % 2][:])
                        tc += 1
                for u in range(2):
                    qb_ = qp + u
                    k = 0
                    for hh in range(2):
                        h = 2 * hp + hh
                        i = 2 * u + hh
                        for mc in range(2):
                            fw.mm(psO[:, qb_ * 128:(qb_ + 1) * 128], mvp[:, h, mc, :], pT[2 * i + mc][:],
                                  start=(k == 0), stop=(k == 3))
                            k += 1
                it += 4
            fw.copy("act", cross_bf[:, hp, tile * TT:(tile + 1) * TT], psO[:, :])
    xn = fw.sb("xn", [128, 8, TT], F32)
    lg = fw.sb("lg", [128, 16], F32)
    affs = fw.sb("affs", [128, 16, 16], F32)
    pc = 0
    for tile in range(NT // TT):
        ts_ = slice(tile * TT, (tile + 1) * TT)
        for j in range(8):
            ps = psP[pc % 2]
            pc += 1
            js = slice(j * 128, (j + 1) * 128)
            for cc in range(6):
                fw.mm(ps[:], wo_bf[:, cc, js], main_bf[:, cc, ts_], start=(cc == 0), stop=False)
            for c2 in range(2):
                fw.mm(ps[:], woc_bf[:, c2, js], cross_bf[:, c2, ts_], start=False, stop=(c2 == 1))
            fw.tt("dve", xT[:, j, ts_], xT[:, j, ts_], ps[:], ALU.add)
            fw.dma("sp", ho[js, ts_], xT[:, j, ts_], is_output=True)
        emit_rmsnorm(fw, c, xT, gf, xn, tile * TT, TT, sq, ps_ss, rstd)
        for b_ in range(4):
            blk = tile * 4 + b_
            s_ = st[it % 2]
            it += 1
            pl = psO[:, 0:16]
            for ch in range(8):
                fw.mm(pl, xn[:, ch, b_:TT:4], wr[:, ch, :], start=(ch == 0), stop=(ch == 7))
            fw.reduce("dve", s_[:, 0:1], pl, ALU.max)
            fw.ts("dve", s_[:, 1:2], s_[:, 0:1], -1.0, None, op0=ALU.mult)
            fw.act(lg[:], pl, AF.Exp, bias=s_[:, 1:2], scale=1.0)
            fw.reduce("dve", s_[:, 2:3], lg[:], ALU.add)
            fw.op("dve", lambda e, s_=s_: e.reciprocal(s_[:, 3:4], s_[:, 2:3]), [s_[:, 2:3]], [s_[:, 3:4]])
            fw.ts("dve", affs[:, blk, :], lg[:], s_[:, 3:4], None, op0=ALU.mult)
        fw.dma("sp", affo[tile * TT:(tile + 1) * TT, :].rearrange("(p b) e -> p b e", b=4),
               affs[:, tile * 4:(tile + 1) * 4, :], is_output=True)
    return fw


N_BISECT = 34
CAP = 1024


def m_host_consts():
    p = np.arange(128)
    Gm = (p[:, None] // 8 == p[None, :] // 8).astype(np.float32)
    G16 = (p[:, None] // 8 == np.arange(16)[None, :]).astype(np.float32)
    selm = np.zeros((16, 16, 128), np.float32)
    for e in range(16):
        selm[e, e, :] = 1.0
    return {"Gm": Gm, "G16": G16, "selm": selm.reshape(16, 2048)}


def build_M(Dp_next=None, final=False):
    fw = Fw()
    hT = fw.dram_in("hT", [D, NT])
    g_ffn = fw.dram_in("g_ffn", [128, 8])
    affP = fw.dram_in("affP", [128, 1024])
    affT = fw.dram_in("affT", [16, NT])
    Gm_d = fw.dram_in("Gm", [128, 128])
    G16_d = fw.dram_in("G16", [128, 16])
    selm_d = fw.dram_in("selm", [16, 2048])
    wg = fw.dram_in("wg", [16, D, 768])
    wu = fw.dram_in("wu", [16, D, 768])
    wd = fw.dram_in("wd", [16, 768, D])
    if final:
        g_nx = fw.dram_in("g_nx", [128, 8])
        of = fw.dram_out("of", [D, NT])
    else:
        ho = fw.dram_out("ho", [D, NT])
        g_nx = fw.dram_in("g_nx", [128, 8])
        w_nx = fw.dram_in("w_nx", [D, Dp_next])
        po = fw.dram_out("o", [Dp_next, NT])
    c = consts_common(fw)
    xT = fw.sb("xT", [128, 8, NT], F32)
    gf = fw.sb("gf", [128, 8], F32); fw.dma("sp", gf[:], g_ffn)
    aP = fw.sb("aP", [128, 1024], F32); fw.dma("sp", aP[:], affP)
    wT = fw.sb("wT", [16, NT], F32); fw.dma("sp", wT[:], affT)
    Gm = fw.sb("Gm_s", [128, 128], F32); fw.dma("sp", Gm[:], Gm_d)
    selm = fw.sb("selm_s", [16, 2048], F32); fw.dma("sp", selm[:], selm_d)
    for ti in range(NT // TT):
        for ch in range(8):
            fw.dma("sp", xT[:, ch, ti * TT:(ti + 1) * TT], hT[ch * 128:(ch + 1) * 128, ti * TT:(ti + 1) * TT])
    wbig = fw.sb("wbig", [128, 20480], BF16)
    gnx = fw.sb("gnx", [128, 8], F32); fw.dma("sp", gnx[:], g_nx)
    wgb = [wbig[:, i * 9216:i * 9216 + 3072].rearrange("p (c f) -> p c f", c=8) for i in range(2)]
    wub = [wbig[:, i * 9216 + 3072:i * 9216 + 6144].rearrange("p (c f) -> p c f", c=8) for i in range(2)]
    wdb = [wbig[:, i * 9216 + 6144:i * 9216 + 9216].rearrange("p (c d) -> p c d", c=3) for i in range(2)]

    def load_w(e, half, i):
        fs = slice(half * 384, (half + 1) * 384)
        fw.dma("pool", wgb[i], wg[e][:, fs].rearrange("(c p) f -> p c f", p=128))
        fw.dma("pool", wub[i], wu[e][:, fs].rearrange("(c p) f -> p c f", p=128))
        fw.dma("pool", wdb[i], wd[e][fs, :].rearrange("(c p) d -> p c d", p=128))

    load_w(0, 0, 0)
    load_w(0, 1, 1)
    sq = [fw.sb(f"sq{i}", [128, TT], F32) for i in range(2)]
    rstd = fw.sb("rstd", [128, TT], F32)
    ps_ss = fw.ps("ps_ss", [128, TT], F32)
    psG = [fw.ps(f"psG{i}", [128, TT], F32) for i in range(2)]
    psU = [fw.ps(f"psU{i}", [128, TT], F32) for i in range(2)]
    psY = [fw.ps(f"psY{i}", [128, TT], F32) for i in range(2)]
    psW = fw.ps("psW", [128, TT], F32)
    cmp_ = fw.sb("cmp", [128, 1024], F32)
    cn = fw.sb("cn", [128, 1], F32)
    bs = fw.sb("bs128", [128, 4], F32)
    lo, mid, ge = (bs[:, k:k + 1] for k in range(3))
    w = 0.75
    fw.memset("dve", lo, 0.0)
    fw.memset("dve", mid, w)
    xn = fw.sb("xn", [128, 8, NT], BF16)
    sg = [fw.sb(f"sg{i}", [128, TT], F32) for i in range(2)]
    pieces = []
    for tile in range(NT // TT):
        pieces += rmsnorm_nodve_pieces(fw, c, xT, gf, xn[:, :, tile * TT:(tile + 1) * TT], tile * TT, TT, sq, ps_ss, rstd, sg)
    for itn in range(N_BISECT):
        if itn % 4 == 1 and pieces:
            pieces.pop(0)()
        fw.ts("dve", cmp_[:], aP[:], mid, None, op0=ALU.is_ge)
        fw.reduce("dve", cn[:], cmp_[:], ALU.add)
        fw.mm(psW[:, 0:1], Gm[:], cn[:])
        fw.ts("dve", ge, psW[:, 0:1], float(CAP) - 0.5, None, op0=ALU.is_ge)
        fw.stt("dve", lo, ge, w, lo, ALU.mult, ALU.add)
        w = w * 0.5
        fw.ts("dve", mid, lo, w, None, op0=ALU.add)
    thr_d = fw.dram_tmp("thr_d", [128, 1])
    thr16 = fw.sb("thr16", [16, 1], F32)
    fw.dma("sp", thr_d, lo)
    fw.dma("sp", thr16[:], thr_d.rearrange("(e s) o -> e (s o)", s=8)[:, 0:1], allow_slow_non_contiguous=True)
    fw.stt("dve", wT[:], wT[:], thr16[:, 0:1], wT[:], ALU.is_ge, ALU.mult)
    while pieces:
        pieces.pop(0)()
    wbc = [fw.sb(f"wbc{i}", [128, TT], F32) for i in range(2)]
    hid = [fw.sb(f"hid{i}", [128, 3, TT], BF16) for i in range(2)]
    gi = [0]
    yi = [0]
    steps = [(k, tile) for k in range(32) for tile in range(NT // TT)]

    def gateup(sidx):
        k, tile = steps[sidx]
        e, i = k // 2, k % 2
        ts_ = slice(tile * TT, (tile + 1) * TT)
        wb = wbc[sidx % 2]
        fw.mm(psW[:], selm[:, e * 128:(e + 1) * 128], wT[:, ts_])
        fw.copy("act", wb[:], psW[:])
        hd = hid[sidx % 2]
        for f in range(3):
            pg = psG[gi[0] % 2]
            pu = psU[gi[0] % 2]
            s_ = sg[gi[0] % 2]
            gi[0] += 1
            fs = slice(f * 128, (f + 1) * 128)
            for ch in range(8):
                fw.mm(pg[:], wgb[i][:, ch, fs], xn[:, ch, ts_], start=(ch == 0), stop=(ch == 7))
            for ch in range(8):
                fw.mm(pu[:], wub[i][:, ch, fs], xn[:, ch, ts_], start=(ch == 0), stop=(ch == 7))
            fw.act(s_[:], pg[:], AF.Silu)
            fw.tt("dve", s_[:], s_[:], wb[:], ALU.mult)
            fw.tt("dve", hd[:, f, :], s_[:], pu[:], ALU.mult)

    def down(sidx):
        k, tile = steps[sidx]
        i = k % 2
        ts_ = slice(tile * TT, (tile + 1) * TT)
        hd = hid[sidx % 2]
        for j in range(8):
            py = psY[yi[0] % 2]
            yi[0] += 1
            for f in range(3):
                fw.mm(py[:], wdb[i][:, f, j * 128:(j + 1) * 128], hd[:, f, :], start=(f == 0), stop=(f == 2))
            fw.tt("dve", xT[:, j, ts_], xT[:, j, ts_], py[:], ALU.add)
        if tile == NT // TT - 1 and k + 2 < 32:
            load_w((k + 2) // 2, (k + 2) % 2, (k + 2) % 2)

    for sidx in range(len(steps)):
        gateup(sidx)
        if sidx > 0:
            down(sidx - 1)
    down(len(steps) - 1)
    if final:
        un = fw.sb("un", [128, 8, TT], F32)
        for tile in range(NT // TT):
            emit_rmsnorm(fw, c, xT, gnx, un, tile * TT, TT, sq, ps_ss, rstd)
            for ch in range(8):
                fw.dma("sp", of[ch * 128:(ch + 1) * 128, tile * TT:(tile + 1) * TT], un[:, ch, :], is_output=True)
        return fw
    for ch in range(8):
        fw.dma("sp", ho[ch * 128:(ch + 1) * 128, :], xT[:, ch, :], is_output=True)
    w_bf = wbig[:, 0:8 * Dp_next].rearrange("p (c f) -> p c f", c=8)
    for ch in range(8):
        fw.dma("pool", w_bf[:, ch, :], w_nx[ch * 128:(ch + 1) * 128, :])
    ctr = [0]
    for tile in range(NT // TT):
        u = xn[:, :, tile * TT:(tile + 1) * TT]
        emit_rmsnorm(fw, c, xT, gnx, u, tile * TT, TT, sq, ps_ss, rstd)
        emit_proj(fw, u, w_bf, Dp_next, po, tile * TT, TT, psG + psU, sg + wbc, ctr)
    return fw


def build_F():
    fw = Fw()
    hT = fw.dram_in("hT", [D, NT])
    g = fw.dram_in("g", [128, 8])
    o = fw.dram_out("o", [D, NT])
    c = consts_common(fw)
    xT = fw.sb("xT", [128, 8, NT], F32)
    for ch in range(8):
        fw.dma("sp", xT[:, ch, :], hT[ch * 128:(ch + 1) * 128, :])
    gs = fw.sb("gs", [128, 8], F32); fw.dma("sp", gs[:], g)
    sq = [fw.sb(f"sq{i}", [128, TT], F32) for i in range(2)]
    rstd = fw.sb("rstd", [128, TT], F32)
    ps_ss = fw.ps("ps_ss", [128, TT], F32)
    un = [fw.sb(f"un{i}", [128, 8, TT], F32) for i in range(2)]
    for tile in range(NT // TT):
        u = un[tile % 2]
        emit_rmsnorm(fw, c, xT, gs, u, tile * TT, TT, sq, ps_ss, rstd)
        for ch in range(8):
            fw.dma("sp", o[ch * 128:(ch + 1) * 128, tile * TT:(tile + 1) * TT], u[:, ch, :], is_output=True)
    return fw


def _lay(g):
    return np.ascontiguousarray(np.asarray(g, np.float32).reshape(8, 128).T)


def _run(fw, in_maps):
    nc = fw.finish()
    res = run_bass_kernel_spmd(nc, in_maps, core_ids=list(range(NCORES)))
    return res.results


def kernel(x, mem, mix_norm_g, ffn_norm_g, mem_norm_g, final_norm_g, w_mem_kv, w_out,
           hy_w_in, hy_short_w, hy_filt_w1, hy_filt_b1, hy_filt_w2, hy_filt_b2, hy_filt_w3,
           hy_filt_freq, hy_skip, at_w_in, at_q_norm_g, at_k_norm_g,
           router_w, exp_w_gate, exp_w_up, exp_w_down):
    f32 = lambda a: np.ascontiguousarray(np.asarray(a), dtype=np.float32)
    x = f32(x); mem = f32(mem)
    h = x.reshape(B * T, D)
    hT = [np.ascontiguousarray(h[c * NT:(c + 1) * NT].T) for c in range(NCORES)]
    memT = [np.ascontiguousarray(mem[b].T) for b in range(B)]
    ident = np.eye(128, dtype=np.float32)
    mconst = m_host_consts()
    aconst = at_host_consts()
    for i in range(4):
        j = i // 2
        hyena = (i % 2 == 0)
        w_in = f32(hy_w_in[j]) if hyena else f32(at_w_in[j])
        Dp = w_in.shape[1]
        g_mix = _lay(mix_norm_g[i])
        if i == 0:
            res = _run(build_P(Dp), [{"hT": hT[c], "g": g_mix, "w": w_in} for c in range(NCORES)])
            projT = [r["o"] for r in res]
        proj = np.concatenate([p_.T for p_ in projT], 0).reshape(B, T, Dp)
        if hyena:
            hw = hy_host_weights(f32(hy_short_w[j]), f32(hy_filt_w1[j]), f32(hy_filt_b1[j]), f32(hy_filt_w2[j]),
                                 f32(hy_filt_b2[j]), f32(hy_filt_w3[j]), f32(hy_filt_freq[j]), f32(hy_skip[j]))
            res = _run(build_HY(), [hy_host_inputs(proj, hw, c) for c in range(NCORES)])
            main = hy_host_gather([r["o"] for r in res])
        else:
            gq = f32(at_q_norm_g[j]).reshape(64, 1)
            gk = f32(at_k_norm_g[j]).reshape(64, 1)
            ims = []
            for c in range(NCORES):
                b, g = c // 4, c % 4
                q = proj[b, :, g * 192:(g + 1) * 192].reshape(T, 3, 64)
                d = {"qT": np.ascontiguousarray(q.transpose(1, 2, 0)),
                     "kT": np.ascontiguousarray(proj[b, :, 768 + g * 64:768 + (g + 1) * 64].T),
                     "v": np.ascontiguousarray(proj[b, :, 1024 + g * 64:1024 + (g + 1) * 64]),
                     "gq": gq, "gk": gk}
                d.update(aconst)
                ims.append(d)
            res = _run(build_AT(), ims)
            main = np.empty((B, T, 768), np.float32)
            for c in range(NCORES):
                b, g = c // 4, c % 4
                main[b, :, g * 192:(g + 1) * 192] = res[c]["o"].transpose(2, 0, 1).reshape(T, 192)
        mainf = main.reshape(B * T, 768)
        cqf = proj[:, :, Dp - 256:].reshape(B * T, 256)
        ims = []
        for c in range(NCORES):
            sl = slice(c * NT, (c + 1) * NT)
            ims.append({"hT": hT[c], "mainT": np.ascontiguousarray(mainf[sl].T),
                        "cqT": np.ascontiguousarray(cqf[sl].T.reshape(4, 64, NT)), "memT": memT[c // 4],
                        "g_mem": _lay(mem_norm_g), "w_kv": f32(w_mem_kv[i]), "w_out": f32(w_out[i]),
                        "g_ffn": _lay(ffn_norm_g[i]), "w_r": f32(router_w[i]), "ident": ident})
        res = _run(build_O(), ims)
        hT = [r["ho"] for r in res]
        aff = np.concatenate([r["aff"] for r in res], 0)
        wg_, wu_, wd_ = f32(exp_w_gate[i]), f32(exp_w_up[i]), f32(exp_w_down[i])
        ims = []
        for c in range(NCORES):
            b = c // 4
            ab = aff[b * T:(b + 1) * T]
            d = {"hT": hT[c], "g_ffn": _lay(ffn_norm_g[i]),
                 "affP": np.ascontiguousarray(ab.T.reshape(128, 1024)),
                 "affT": np.ascontiguousarray(aff[c * NT:(c + 1) * NT].T),
                 "wg": wg_, "wu": wu_, "wd": wd_}
            if i < 3:
                jn = (i + 1) // 2
                d["g_nx"] = _lay(mix_norm_g[i + 1])
                d["w_nx"] = f32(hy_w_in[jn]) if (i + 1) % 2 == 0 else f32(at_w_in[jn])
            else:
                d["g_nx"] = _lay(final_norm_g)
            d.update(mconst)
            ims.append(d)
        if i < 3:
            res = _run(build_M(Dp_next=ims[0]["w_nx"].shape[1]), ims)
            hT = [r["ho"] for r in res]
            projT = [r["o"] for r in res]
        else:
            res = _run(build_M(final=True), ims)
    out = np.concatenate([r["of"].T for r in res], 0).reshape(B, T, D)
    return np.ascontiguousarray(out, dtype=np.float32)
```
